# Optimizing a Trainium2 kernel written in Bass

```python
import math
import jax
import jax.numpy as jnp
from jax import lax
import numpy as np

D_MODEL = 2048
BATCH = 16
SEQ = 2048
DEPTH = 4

GRID_W = 64
CTX_LEN = 256
N_DIFF_HEADS = 8
DIFF_HD = 64
DIFF_VD = 2 * DIFF_HD
ATTN_W = N_DIFF_HEADS * DIFF_VD
QK_W = N_DIFF_HEADS * 2 * DIFF_HD
Q_BLOCK = 128
ROPE_THETA = 10000.0
ROPE_FREQS = DIFF_HD // 4
CONV_W = 512
CONV_K = 31
FOURIER_HEADS = 4
FOURIER_HD = 128
FOURIER_W = FOURIER_HEADS * FOURIER_HD
MIX_W = ATTN_W + CONV_W + FOURIER_W
K0 = QK_W
V0 = 2 * QK_W
G0 = V0 + ATTN_W
F0 = G0 + 2 * CONV_W
IN_W = F0 + FOURIER_W
N_EXPERTS = 16
EXPERT_FF = 1024
CAPACITY_FACTOR = 2
EPS = 1e-6

kernel_name = 'hybrid_diffattn_conformer_fnet_ecmoe_dit'


def rms_norm(x, g):
    x32 = x.astype(jnp.float32)
    y = x32 * lax.rsqrt(jnp.mean(x32 * x32, axis=-1, keepdims=True) + EPS)
    return (y * g.astype(jnp.float32)).astype(x.dtype)


def layer_norm(x, g, b):
    x32 = x.astype(jnp.float32)
    mu = jnp.mean(x32, axis=-1, keepdims=True)
    var = jnp.mean(jnp.square(x32 - mu), axis=-1, keepdims=True)
    y = (x32 - mu) * lax.rsqrt(var + EPS)
    return (y * g.astype(jnp.float32) + b.astype(jnp.float32)).astype(x.dtype)


def modulate(h, shift, scale):
    return h * (1 + scale) + shift


def rope_tables(n, dtype):
    rows = n // GRID_W
    row = jnp.repeat(jnp.arange(rows), GRID_W)
    col = jnp.tile(jnp.arange(GRID_W), rows)
    pos = jnp.stack([row, col], axis=-1).astype(jnp.float32)
    inv_freq = ROPE_THETA ** (-jnp.arange(ROPE_FREQS, dtype=jnp.float32) / ROPE_FREQS)
    ang = pos[:, :, None, None] * inv_freq
    ang = jnp.broadcast_to(ang, (n, 2, 2, ROPE_FREQS)).reshape(n, DIFF_HD)
    return jnp.cos(ang).astype(dtype), jnp.sin(ang).astype(dtype)


def apply_rope_2d(t, cos, sin):
    ts = t.reshape(t.shape[:-1] + (2, 2, ROPE_FREQS))
    rot = jnp.stack([-ts[..., 1, :], ts[..., 0, :]], axis=-2).reshape(t.shape)
    return t * cos[None, :, None, None, :] + rot * sin[None, :, None, None, :]


def split_mix(u):
    b, n = u.shape[:2]
    q = u[..., :K0].reshape(b, n, N_DIFF_HEADS, 2, DIFF_HD)
    k = u[..., K0:V0].reshape(b, n, N_DIFF_HEADS, 2, DIFF_HD)
    v = u[..., V0:G0].reshape(b, n, N_DIFF_HEADS, DIFF_VD)
    return q, k, v, u[..., G0:F0], u[..., F0:]


def diff_lambda_value(lv, lam_init):
    lv = lv.astype(jnp.float32)
    return jnp.exp(jnp.sum(lv[0] * lv[1])) - jnp.exp(jnp.sum(lv[2] * lv[3])) + lam_init


def diff_attn_core(q, k, v, lam):
    s = jnp.einsum('bqhmd,bkhmd->bhmqk', q, k).astype(jnp.float32) * (DIFF_HD ** -0.5)
    p = jax.nn.softmax(s, axis=-1)
    a = p[:, :, 0] - lam * p[:, :, 1]
    return jnp.einsum('bhqk,bkhe->bqhe', a.astype(v.dtype), v)


def diff_head_out(o, g_sub, lam_init):
    b, n = o.shape[:2]
    return (rms_norm(o, g_sub) * (1.0 - lam_init)).reshape(b, n, ATTN_W)


def diff_attention_latent(q, k, v, kc, vc, lam):
    b, n = q.shape[:2]
    k_all = jnp.concatenate([kc, k], axis=1)
    v_all = jnp.concatenate([vc, v], axis=1)
    nb = n // Q_BLOCK
    qb = jnp.swapaxes(q.reshape((b, nb, Q_BLOCK) + q.shape[2:]), 0, 1)
    o = lax.map(lambda blk: diff_attn_core(blk, k_all, v_all, lam), qb)
    return jnp.swapaxes(o, 0, 1).reshape(b, n, N_DIFF_HEADS, DIFF_VD)


def conformer_conv(u, w, bias, ln_g, ln_b):
    a, g = jnp.split(u, 2, axis=-1)
    z = a * jax.nn.sigmoid(g)
    z = lax.conv_general_dilated(z, w[:, None, :], window_strides=(1,),
                                 padding=[(CONV_K // 2, CONV_K // 2)],
                                 dimension_numbers=('NWC', 'WIO', 'NWC'),
                                 feature_group_count=CONV_W) + bias
    return jax.nn.silu(layer_norm(z, ln_g, ln_b))


def fourier_mix(u):
    b, n = u.shape[:2]
    z = u.astype(jnp.float32).reshape(b, n, FOURIER_HEADS, FOURIER_HD)
    z = jnp.fft.fftn(z, axes=(1, 3), norm='ortho').real
    return z.reshape(b, n, FOURIER_W).astype(u.dtype)


def expert_choice_ffn(h, w_r, w_g, w_u, w_d):
    b, n, d = h.shape
    cap = (CAPACITY_FACTOR * n) // N_EXPERTS
    aff = jax.nn.softmax((h @ w_r).astype(jnp.float32), axis=-1)
    gates, idx = lax.top_k(jnp.swapaxes(aff, 1, 2), cap)
    xs = jax.vmap(lambda hb, ib: hb[ib])(h, idx)
    hid = jax.nn.silu(jnp.einsum('becd,edf->becf', xs, w_g)) * jnp.einsum('becd,edf->becf', xs, w_u)
    y = jnp.einsum('becf,efd->becd', hid, w_d) * gates[..., None].astype(h.dtype)
    scatter = lambda ib, yb: jnp.zeros((n, d), yb.dtype).at[ib.reshape(-1)].add(yb.reshape(-1, d))
    return jax.vmap(scatter)(idx, y)


def setup_inputs(seed: int = 0) -> dict:
    key = jax.random.key(seed)
    ks = jax.random.split(key, 24)
    nrm = lambda k, shape, s: jax.random.normal(k, shape, jnp.float32) * s
    return {
        'x': nrm(ks[0], (BATCH, SEQ, D_MODEL), 1.0),
        'c': nrm(ks[1], (BATCH, D_MODEL), 1.0),
        'ctx': nrm(ks[2], (BATCH, CTX_LEN, D_MODEL), 1.0),
        'c_ctx': nrm(ks[3], (D_MODEL,), 1.0),
        'w_ada': nrm(ks[4], (DEPTH, D_MODEL, 6 * D_MODEL), 0.5 * D_MODEL ** -0.5),
        'b_ada': nrm(ks[5], (DEPTH, 6 * D_MODEL), 0.02),
        'g_norm1': 1.0 + nrm(ks[6], (DEPTH, D_MODEL), 0.02),
        'w_in': nrm(ks[7], (DEPTH, D_MODEL, IN_W), D_MODEL ** -0.5),
        'diff_lambda': nrm(ks[8], (DEPTH, 4, DIFF_HD), 0.1),
        'g_sub': 1.0 + nrm(ks[9], (DEPTH, DIFF_VD), 0.02),
        'conv_w': nrm(ks[10], (DEPTH, CONV_K, CONV_W), CONV_K ** -0.5),
        'conv_b': nrm(ks[11], (DEPTH, CONV_W), 0.02),
        'conv_ln_g': 1.0 + nrm(ks[12], (DEPTH, CONV_W), 0.02),
        'conv_ln_b': nrm(ks[13], (DEPTH, CONV_W), 0.02),
        'w_out': nrm(ks[14], (DEPTH, MIX_W, D_MODEL), MIX_W ** -0.5),
        'g_norm2': 1.0 + nrm(ks[15], (DEPTH, D_MODEL), 0.02),
        'w_router': nrm(ks[16], (DEPTH, D_MODEL, N_EXPERTS), D_MODEL ** -0.5),
        'w_gate': nrm(ks[17], (DEPTH, N_EXPERTS, D_MODEL, EXPERT_FF), D_MODEL ** -0.5),
        'w_up': nrm(ks[18], (DEPTH, N_EXPERTS, D_MODEL, EXPERT_FF), D_MODEL ** -0.5),
        'w_down': nrm(ks[19], (DEPTH, N_EXPERTS, EXPERT_FF, D_MODEL), EXPERT_FF ** -0.5),
        'g_final': 1.0 + nrm(ks[20], (D_MODEL,), 0.02),
    }


def reference(x, c, ctx, c_ctx, w_ada, b_ada, g_norm1, w_in, diff_lambda, g_sub, conv_w, conv_b,
              conv_ln_g, conv_ln_b, w_out, g_norm2, w_router, w_gate, w_up, w_down, g_final):
    n = x.shape[1]
    cos, sin = rope_tables(n, x.dtype)
    c_act = jax.nn.silu(c)
    cc_act = jax.nn.silu(c_ctx)
    xc = ctx
    for l in range(DEPTH):
        last = l == DEPTH - 1
        mod = (c_act @ w_ada[l] + b_ada[l])[:, None, :]
        modc = cc_act @ w_ada[l] + b_ada[l]
        sh1, sc1, gt1, sh2, sc2, gt2 = jnp.split(mod, 6, axis=-1)
        csh1, csc1, cgt1, csh2, csc2, cgt2 = jnp.split(modc, 6, axis=-1)
        lam_init = 0.8 - 0.6 * math.exp(-0.3 * l)
        lam = diff_lambda_value(diff_lambda[l], lam_init)

        h = modulate(rms_norm(x, g_norm1[l]), sh1, sc1)
        hc = modulate(rms_norm(xc, g_norm1[l]), csh1, csc1)
        q, k, v, u_glu, u_four = split_mix(h @ w_in[l])
        if last:
            ukv = hc @ w_in[l][:, K0:G0]
            kc = ukv[..., :QK_W].reshape(ukv.shape[:2] + (N_DIFF_HEADS, 2, DIFF_HD))
            vc = ukv[..., QK_W:].reshape(ukv.shape[:2] + (N_DIFF_HEADS, DIFF_VD))
        else:
            qc, kc, vc, uc_glu, uc_four = split_mix(hc @ w_in[l])
        q = apply_rope_2d(q, cos, sin)
        k = apply_rope_2d(k, cos, sin)
        attn = diff_head_out(diff_attention_latent(q, k, v, kc, vc, lam), g_sub[l], lam_init)
        conv = conformer_conv(u_glu, conv_w[l], conv_b[l], conv_ln_g[l], conv_ln_b[l])
        four = fourier_mix(u_four)
        y = jnp.concatenate([attn, conv, four], axis=-1) @ w_out[l]
        if not last:
            attn_c = diff_head_out(diff_attn_core(qc, kc, vc, lam), g_sub[l], lam_init)
            conv_c = conformer_conv(uc_glu, conv_w[l], conv_b[l], conv_ln_g[l], conv_ln_b[l])
            four_c = fourier_mix(uc_four)
            yc = jnp.concatenate([attn_c, conv_c, four_c], axis=-1) @ w_out[l]
            xc = xc + cgt1 * yc
        x = x + gt1 * y

        h2 = modulate(rms_norm(x, g_norm2[l]), sh2, sc2)
        x = x + gt2 * expert_choice_ffn(h2, w_router[l], w_gate[l], w_up[l], w_down[l])
        if not last:
            h2c = modulate(rms_norm(xc, g_norm2[l]), csh2, csc2)
            xc = xc + cgt2 * expert_choice_ffn(h2c, w_router[l], w_gate[l], w_up[l], w_down[l])
    return rms_norm(x, g_final)
```

```python
import numpy as np
import concourse.bass as bass
import concourse.mybir as mybir
from concourse.bass_utils import run_bass_kernel_spmd

F32 = mybir.dt.float32
BF16 = mybir.dt.bfloat16
I32 = mybir.dt.int32
U32 = mybir.dt.uint32
AF = mybir.ActivationFunctionType
ALU = mybir.AluOpType
AX = mybir.AxisListType

SEM_CAP = 30000
N_DMA_SEMS = 24


class Buf:
    __slots__ = ("name", "t", "last_write", "readers")

    def __init__(self, name, t=None):
        self.name = name
        self.t = t
        self.last_write = None
        self.readers = []


class _Op:
    __slots__ = ("waits", "fn", "marked", "dma_sem", "dma_val", "final")

    def __init__(self, waits, fn):
        self.waits = waits
        self.fn = fn
        self.marked = False
        self.dma_sem = None
        self.dma_val = 0
        self.final = False


class Prog:
    ENGS = ("pe", "act", "dve", "pool", "sp")

    def __init__(self, nc):
        self.nc = nc
        self.ops = {e: [] for e in self.ENGS}
        self.known = {e: {} for e in self.ENGS}
        self.ctx = []
        self.dma_sems = []
        self.dma_cnt = [0] * N_DMA_SEMS
        self.dma_last_issuer = [None] * N_DMA_SEMS
        self.dma_rr = 0
        self.finals = []
        self._n = 0
        self.n_psum = 0
        self.scopes = []

    def _enter(self, cm):
        self.ctx.append(cm)
        return cm.__enter__()

    def sbuf(self, name, shape, dtype):
        self._n += 1
        name = "%s_%d" % (name, self._n)
        t = self._enter(self.nc.sbuf_tensor(name, list(shape), dtype))
        return Buf(name, t)

    def psum(self, name, shape=(128, 512), dtype=F32):
        t = self._enter(self.nc.psum_tensor(name, list(shape), dtype))
        return Buf(name, t)

    def dram(self, name, shape, dtype, addr_space="Local"):
        t = self.nc.dram_tensor(name, list(shape), dtype, kind="Internal", addr_space=addr_space).ap()
        return Buf(name, t)

    def _deps(self, eng, reads, writes):
        deps = []
        for b in reads:
            if b.last_write is not None:
                deps.append(b.last_write)
        for b in writes:
            if b.last_write is not None:
                deps.append(b.last_write)
            deps.extend(b.readers)
        waits = []
        kn = self.known[eng]
        for tok in deps:
            kind, key, val = tok
            if kind == "c":
                if key == "pe" and eng == "pe":
                    continue
                if kn.get(key, 0) >= val:
                    continue
                kn[key] = val
                self.ops[key][val - 1].marked = True
                waits.append(tok)
            else:
                k2 = ("d", key)
                if kn.get(k2, 0) >= val:
                    continue
                kn[k2] = val
                waits.append(tok)
        return waits

    def _commit(self, tok, reads, writes):
        for b in writes:
            b.last_write = tok
            b.readers = []
        for b in reads:
            if b in writes:
                continue
            if tok[0] == "c":
                b.readers = [r for r in b.readers if not (r[0] == "c" and r[1] == tok[1])]
            b.readers.append(tok)

    def op(self, eng, fn, reads=(), writes=()):
        waits = self._deps(eng, reads, writes)
        o = _Op(waits, fn)
        self.ops[eng].append(o)
        tok = ("c", eng, len(self.ops[eng]))
        self._commit(tok, reads, writes)
        return tok

    def dma(self, eng, out, in_, reads=(), writes=(), final=False, **kw):
        def fn(e, out=out, in_=in_, kw=kw):
            return e.dma_start(out=out, in_=in_, **kw)
        return self.dma_fn(eng, fn, reads, writes, final)

    def dma_fn(self, eng, fn, reads=(), writes=(), final=False):
        waits = self._deps(eng, reads, writes)
        j = self.dma_rr
        self.dma_rr = (self.dma_rr + 1) % N_DMA_SEMS
        prev = self.dma_cnt[j]
        kn = self.known[eng]
        if prev > 0 and kn.get(("d", j), 0) < prev:
            kn[("d", j)] = prev
            waits.append(("d", j, prev))
        o = _Op(waits, fn)
        o.dma_sem = j
        self.dma_cnt[j] = prev + 16
        o.dma_val = prev + 16
        self.ops[eng].append(o)
        tok = ("d", j, prev + 16)
        self._commit(tok, reads, writes)
        if final:
            self.finals.append(tok)
        return tok

    def barrier(self):
        toks = []
        for e in self.ENGS:
            n = len(self.ops[e])
            while n > 0 and (self.ops[e][n - 1].dma_sem is not None or self.ops[e][n - 1].fn is None):
                n -= 1
            if n > 0:
                toks.append(("c", e, n))
        for j in range(N_DMA_SEMS):
            if self.dma_cnt[j] > 0:
                toks.append(("d", j, self.dma_cnt[j]))
        for e in self.ENGS:
            kn = self.known[e]
            waits = []
            for tok in toks:
                kind, key, val = tok
                if kind == "c":
                    if key == e:
                        continue
                    if kn.get(key, 0) >= val:
                        continue
                    kn[key] = val
                    self.ops[key][val - 1].marked = True
                    waits.append(tok)
                else:
                    k2 = ("d", key)
                    if kn.get(k2, 0) >= val:
                        continue
                    kn[k2] = val
                    waits.append(tok)
            if waits:
                self.ops[e].append(_Op(waits, None))

    def push_scope(self):
        self.scopes.append(len(self.ctx))

    def pop_scope(self):
        self.barrier()
        n = self.scopes.pop()
        while len(self.ctx) > n:
            self.ctx.pop().__exit__(None, None, None)

    def barrier_tokens(self):
        toks = []
        for e in self.ENGS:
            if self.ops[e]:
                toks.append(("c", e, len(self.ops[e])))
        return toks

    def emit(self):
        nc = self.nc
        fw = []
        for tok in self.finals:
            fw.append(tok)
        if fw:
            self.ops["sp"].append(_Op(fw, None))
        rank = {}
        nsem = {}
        for e in self.ENGS:
            r = 0
            for i, o in enumerate(self.ops[e]):
                if o.dma_sem is None and o.marked:
                    r += 1
                    rank[(e, i + 1)] = r
            nsem[e] = (r + SEM_CAP - 1) // SEM_CAP
        csems = {e: [self._enter(nc.semaphore("s_%s_%d" % (e, k))) for k in range(max(1, nsem[e]))]
                 for e in self.ENGS}
        dsems = [self._enter(nc.semaphore("s_dma_%d" % k)) for k in range(N_DMA_SEMS)]
        block = self._enter(nc.Block())

        def run(ename, eng):
            for i, o in enumerate(self.ops[ename]):
                for tok in o.waits:
                    if tok[0] == "c":
                        r = rank[(tok[1], tok[2])] - 1
                        eng.wait_ge(csems[tok[1]][r // SEM_CAP], (r % SEM_CAP) + 1)
                    else:
                        eng.wait_ge(dsems[tok[1]], tok[2])
                if o.fn is None:
                    continue
                ins = o.fn(eng)
                if o.dma_sem is not None:
                    ins.then_inc(dsems[o.dma_sem], 16)
                elif o.marked:
                    r = rank[(ename, i + 1)] - 1
                    ins.then_inc(csems[ename][r // SEM_CAP], 1)

        @block.tensor
        def _(e):
            run("pe", e)

        @block.scalar
        def _(e):
            run("act", e)

        @block.vector
        def _(e):
            run("dve", e)

        @block.gpsimd
        def _(e):
            run("pool", e)

        @block.sync
        def _(e):
            run("sp", e)

        while self.ctx:
            self.ctx.pop().__exit__(None, None, None)


D = 2048
LAT = 2048
CTX = 256
SEG = LAT + CTX
NSMP = 2
NT = NSMP * SEG
H = 8
INW = 4608
NE = 16
FF = 1024
CAPL = 256
CAPC = 32
EPS = 1e-6
NSP = 425
SP_G1, SP_G2, SP_CW, SP_CB, SP_LG, SP_LB, SP_GS, SP_DL = 0, 16, 32, 156, 160, 164, 168, 169


def build_program(depth=4, stop_after=None, dump=()):
    nc = bass.Bass("TRN2", target_bir_lowering=False)
    P = Prog(nc)

    def ein(name, shape, dt=F32):
        return nc.dram_tensor(name, list(shape), dt, kind="ExternalInput").ap()

    x_in = ein("x", [NSMP, LAT, D])
    ctx_in = ein("ctx", [NSMP, CTX, D])
    cvec_in = ein("cvec", [128, 16, 3])
    w_ada = ein("w_ada", [depth, D, 6 * D])
    b_ada = ein("b_ada", [depth, 6 * D])
    w_in = ein("w_in", [depth, D, INW])
    w_out = ein("w_out", [depth, D, D])
    w_router = ein("w_router", [depth, D, NE])
    w_gate = ein("w_gate", [depth, NE, D, FF])
    w_up = ein("w_up", [depth, NE, D, FF])
    w_down = ein("w_down", [depth, NE, FF, D])
    smallp_in = ein("smallp", [depth, 128, NSP])
    gfin_in = ein("gfin", [128, D])
    ident_in = ein("ident", [128, 128])
    rperm_in = ein("rperm", [128, 128])
    rope_in = ein("ropeT", [128, 2, LAT])
    dftc_in = ein("dftc", [128, 256])
    dftn_in = ein("dftn", [2, LAT, LAT], BF16)
    dft256_in = ein("dft256", [2, CTX, CTX], BF16)
    rowbase_in = ein("rowbase", [32, 2])
    out_d = nc.dram_tensor("out", [NSMP, LAT, D], F32, kind="ExternalOutput").ap()

    xs = P.dram("xs", [NT, D], F32).t
    qT = P.dram("qT", [1024, NT], BF16).t
    kT = P.dram("kT", [1024, NT], BF16).t
    vv = P.dram("vv", [NT, 1024], BF16).t
    zT = P.dram("zT", [512, NT], F32).t
    fT = P.dram("fT", [512, NT], BF16).t
    mixT = P.dram("mixT", [D, NT], BF16).t
    xn_d = P.dram("xn_d", [NT, D], F32).t
    gt_d = P.dram("gt_d", [2, 3, D], F32).t
    scratch = {"xs": (xs, [NT, D], F32), "qT": (qT, [1024, NT], BF16), "kT": (kT, [1024, NT], BF16),
               "vv": (vv, [NT, 1024], BF16), "zT": (zT, [512, NT], F32), "fT": (fT, [512, NT], BF16),
               "mixT": (mixT, [D, NT], BF16), "xn_d": (xn_d, [NT, D], F32), "gt_d": (gt_d, [2, 3, D], F32)}

    ps = [P.psum("ps%d" % i) for i in range(8)]

    ident = P.sbuf("ident", [128, 128], F32)
    rperm = P.sbuf("rperm", [128, 128], F32)
    onesb = P.sbuf("onesb", [128, 128], BF16)
    onesf = P.sbuf("onesf", [128, 128], F32)
    epst = P.sbuf("epst", [128, 1], F32)
    csil = P.sbuf("csil", [128, 16, 3], F32)
    cT3 = P.sbuf("cT3", [128, 16, 3], BF16)
    modT = P.sbuf("modT", [128, 96, 3], F32)
    smallp = P.sbuf("smallp", [128, NSP], F32)
    a1 = P.sbuf("a1", [128, 16, 3], F32)
    a2 = P.sbuf("a2", [128, 16, 3], F32)
    neglam = P.sbuf("neglam", [128, 1], F32)
    gsubs = P.sbuf("gsubs", [128, 1], F32)
    rowbase = P.sbuf("rowbase", [32, 2], F32)
    mhalf_g = P.sbuf("mhalf_g", [128, 1], F32)

    def MM(psb, out_ap, pairs, reads):
        n = len(pairs)
        for i, (l, r) in enumerate(pairs):
            P.op("pe", lambda e, l=l, r=r, i=i: e.matmul(out_ap, l, r, start=(i == 0), stop=(i == n - 1)),
                 reads=reads, writes=[psb])

    def TR(psb, out_ap, in_ap, rows, reads):
        P.op("pe", lambda e: e.transpose(out_ap, in_ap, ident.t[0:rows, 0:rows]), reads=list(reads) + [ident],
             writes=[psb])

    def ACT(out, in_, func, reads, writes, **kw):
        P.op("act", lambda e: e.activation(out, in_, func, **kw), reads=reads, writes=writes)

    def TS(eng, out, in0, s1, s2, op0, op1, reads, writes):
        P.op(eng, lambda e: e.tensor_scalar(out, in0, s1, s2, op0, op1), reads=reads, writes=writes)

    def TT(eng, out, in0, in1, op, reads, writes):
        P.op(eng, lambda e: e.tensor_tensor(out, in0, in1, op), reads=reads, writes=writes)

    def STT(out, in0, scalar, in1, op0, op1, reads, writes):
        P.op("dve", lambda e: e.scalar_tensor_tensor(out, in0, scalar, in1, op0, op1), reads=reads, writes=writes)

    def RECIP(out, in_, reads, writes):
        P.op("dve", lambda e: e.reciprocal(out, in_), reads=reads, writes=writes)

    def MEMSET(eng, ap, val, writes):
        P.op(eng, lambda e: e.memset(ap, val), reads=[], writes=writes)

    def variant(s, is_ctx):
        return 2 if is_ctx else s

    P.dma("sp", ident.t[:, :], ident_in[:, :], writes=[ident])
    P.dma("sp", rperm.t[:, :], rperm_in[:, :], writes=[rperm])
    P.dma("sp", csil.t[:, :, :], cvec_in[:, :, :], writes=[csil])
    P.dma("sp", rowbase.t[:, :], rowbase_in[:, :], writes=[rowbase])
    for s in range(NSMP):
        P.dma("sp", xs[s * SEG:s * SEG + CTX, :], ctx_in[s, :, :])
        P.dma("sp", xs[s * SEG + CTX:(s + 1) * SEG, :], x_in[s, :, :])
    MEMSET("dve", onesb.t[:, :], 1.0, [onesb])
    MEMSET("dve", onesf.t[:, :], 1.0, [onesf])
    MEMSET("dve", epst.t[:, :], EPS, [epst])
    MEMSET("dve", mhalf_g.t[:, :], -0.5, [mhalf_g])
    ACT(csil.t[:, :, :], csil.t[:, :, :], AF.Silu, [csil], [csil])
    P.op("dve", lambda e: e.tensor_copy(cT3.t[:, :, :], csil.t[:, :, :]), reads=[csil], writes=[cT3])
    P.barrier()

    def norm_block(xt, xnb, junk, ssb, col, row0, dst_store=None):
        P.dma("sp", xt.t[:, :], xs[row0:row0 + 128, :], writes=[xt])
        ACT(junk.t[:, :], xt.t[:, :], AF.Square, [xt], [junk, ssb], accum_out=ssb.t[:, col:col + 1])
        TS("dve", ssb.t[:, col + 64:col + 65], ssb.t[:, col:col + 1], 1.0 / D, EPS, ALU.mult, ALU.add, [ssb], [ssb])
        TT("pool", ssb.t[:, col + 128:col + 129], ssb.t[:, col + 64:col + 65], mhalf_g.t[:, 0:1], ALU.pow,
           [ssb, mhalf_g], [ssb])
        TS("pool", xnb.t[:, :], xt.t[:, :], ssb.t[:, col + 128:col + 129], 1.0, ALU.mult, ALU.mult,
           [xt, ssb], [xnb])

    psrr = [0]

    def nextps(lo=0, hi=8):
        i = lo + (psrr[0] % (hi - lo))
        psrr[0] += 1
        return ps[i]

    evrr = [0]

    def evac_mod(out_ap, in_ap, sc_ap, bi_ap, reads, writes):
        evrr[0] += 1
        if evrr[0] % 2 == 0:
            ACT(out_ap, in_ap, AF.Identity, reads, writes, scale=sc_ap, bias=bi_ap)
        else:
            TS("dve", out_ap, in_ap, sc_ap, bi_ap, ALU.mult, ALU.add, reads, writes)

    def transpose_mod(xnb, rows, dst, dst_col0, amod, shoff, v, lo=0, hi=4):
        for g in range(4):
            pb = nextps(lo, hi)
            for i in range(4):
                c = g * 4 + i
                TR(pb, pb.t[:, i * 128:i * 128 + rows], xnb.t[0:rows, c * 128:(c + 1) * 128], rows, [xnb])
            for i in range(4):
                c = g * 4 + i
                evac_mod(dst.t[:, c, dst_col0:dst_col0 + rows], pb.t[:, i * 128:i * 128 + rows],
                         amod.t[:, c, v:v + 1], modT.t[:, shoff + c, v:v + 1], [pb, amod, modT], [dst])

    TT_TILES = [(0, 256, True), (256, 512, False), (768, 512, False), (1280, 512, False), (1792, 512, False)]

    for l in range(depth):
        lam_init = 0.8 - 0.6 * float(np.exp(-0.3 * l))
        P.push_scope()
        P.dma("sp", smallp.t[:, :], smallp_in[l, :, :], writes=[smallp])
        wsl = [P.sbuf("m_wsl%d" % i, [128, 16, 512], BF16) for i in range(2)]
        bsl = [P.sbuf("m_bsl%d" % i, [1, 512], F32) for i in range(2)]
        gtmp = [P.sbuf("m_gtmp%d" % i, [128, 512], F32) for i in range(2)]
        crep = P.sbuf("m_crep", [128, 16, 3, 128], BF16)
        for j in range(16):
            for v in range(3):
                TS("dve", crep.t[:, j, v, :], onesf.t[:, :], csil.t[:, j, v:v + 1], None, ALU.mult, ALU.bypass,
                   [onesf, csil], [crep])
        gi = 0
        for k in range(24):
            w = wsl[k % 2]
            b = bsl[k % 2]
            P.dma("pool", w.t[:, :, :], w_ada[l, :, k * 512:(k + 1) * 512].rearrange("(c p) n -> p c n", p=128),
                  writes=[w])
            P.dma("sp", b.t[:, :], b_ada[l:l + 1, k * 512:(k + 1) * 512], writes=[b])
            for sub in range(4):
                pb = nextps(0, 4)
                pairs = [(w.t[:, dc, sub * 128:(sub + 1) * 128], cT3.t[:, dc, :]) for dc in range(16)]
                pairs.append((b.t[0:1, sub * 128:(sub + 1) * 128], onesf.t[0:1, 0:3]))
                MM(pb, pb.t[:, 0:3], pairs, [w, b, cT3, onesf])
                ACT(modT.t[:, k * 4 + sub, :], pb.t[:, 0:3], AF.Copy, [pb], [modT])
            if k in (8, 9, 10, 11, 20, 21, 22, 23):
                g = 0 if k < 12 else 1
                ct = k % 4
                for v in range(3):
                    pb = nextps(4, 8)
                    pairs = [(crep.t[:, dc, v, :], w.t[:, dc, :]) for dc in range(16)]
                    pairs.append((onesf.t[0:1, 0:128], b.t[0:1, :]))
                    MM(pb, pb.t[:, :], pairs, [w, b, crep, onesf])
                    gt = gtmp[gi % 2]
                    gi += 1
                    ACT(gt.t[:, :], pb.t[:, :], AF.Copy, [pb], [gt])
                    P.dma("sp", gt_d[g, v:v + 1, ct * 512:(ct + 1) * 512], gt.t[0:1, :], reads=[gt])
        for v in range(3):
            STT(a1.t[:, :, v], modT.t[:, 16:32, v], 1.0, smallp.t[:, SP_G1:SP_G1 + 16], ALU.add, ALU.mult,
                [modT, smallp], [a1])
            STT(a2.t[:, :, v], modT.t[:, 64:80, v], 1.0, smallp.t[:, SP_G2:SP_G2 + 16], ALU.add, ALU.mult,
                [modT, smallp], [a2])
        lt = P.sbuf("m_lt", [128, 2, 64], F32)
        ls = P.sbuf("m_ls", [128, 4], F32)
        dl = smallp.t[:, SP_DL:SP_DL + 256].rearrange("p (a d) -> p a d", a=4)
        TT("dve", lt.t[:, 0, :], dl[:, 0, :], dl[:, 1, :], ALU.mult, [smallp], [lt])
        TT("dve", lt.t[:, 1, :], dl[:, 2, :], dl[:, 3, :], ALU.mult, [smallp], [lt])
        P.op("dve", lambda e, ls=ls, lt=lt: e.reduce_sum(ls.t[:, 0:2], lt.t[:, :, :], AX.X), reads=[lt], writes=[ls])
        ACT(ls.t[:, 2:4], ls.t[:, 0:2], AF.Exp, [ls], [ls])
        TT("dve", neglam.t[:, :], ls.t[:, 3:4], ls.t[:, 2:3], ALU.subtract, [ls], [neglam])
        TS("dve", neglam.t[:, :], neglam.t[:, :], -lam_init, None, ALU.add, ALU.bypass, [neglam], [neglam])
        TS("dve", gsubs.t[:, :], smallp.t[:, SP_GS:SP_GS + 1], 1.0 - lam_init, None, ALU.mult, ALU.bypass,
           [smallp], [gsubs])
        P.pop_scope()
        if stop_after == ("M", l):
            break

        for s in range(NSMP):
            P.push_scope()
            hT = P.sbuf("a_hT", [128, 16, SEG], BF16)
            xt = [P.sbuf("a_xt%d" % i, [128, D], F32) for i in range(2)]
            xnb = [P.sbuf("a_xn%d" % i, [128, D], F32) for i in range(2)]
            junk = P.sbuf("a_junk", [128, D], BF16)
            ssb = P.sbuf("a_ss", [128, 192], F32)
            wsl = [P.sbuf("a_wsl%d" % i, [128, 16, 512], BF16) for i in range(2)]
            rope = P.sbuf("a_rope", [128, 2, LAT], F32)
            qf = [P.sbuf("a_qf%d" % i, [128, 512], F32) for i in range(2)]
            sgb = [P.sbuf("a_sg%d" % i, [128, 512], F32) for i in range(2)]
            ob = [P.sbuf("a_ob%d" % i, [128, 512], BF16) for i in range(3)]
            zf = [P.sbuf("a_zf%d" % i, [128, 512], F32) for i in range(2)]
            P.dma("sp", rope.t[:, :, :], rope_in[:, :, :], writes=[rope])
            for bb in range(18):
                row0 = s * SEG + bb * 128
                norm_block(xt[bb % 2], xnb[bb % 2], junk, ssb, bb, row0)
                transpose_mod(xnb[bb % 2], 128, hT, bb * 128, a1, 0, variant(s, bb < 2), 0, 4)
            slabs = [("q", 0, 0), ("q", 512, 4), ("k", 1024, 0), ("k", 1536, 4), ("v", 2048, 0), ("v", 2560, 512),
                     ("glu", 0, 0), ("glu", 256, 256), ("f", 4096, 0)]
            cnt = 0
            for si, (kind, c0, aux) in enumerate(slabs):
                w = wsl[si % 2]
                if kind == "glu":
                    P.dma("pool", w.t[:, :, 0:256],
                          w_in[l, :, 3072 + c0:3072 + c0 + 256].rearrange("(c p) n -> p c n", p=128), writes=[w])
                    P.dma("pool", w.t[:, :, 256:512],
                          w_in[l, :, 3584 + c0:3584 + c0 + 256].rearrange("(c p) n -> p c n", p=128), writes=[w])
                else:
                    P.dma("pool", w.t[:, :, :], w_in[l, :, c0:c0 + 512].rearrange("(c p) n -> p c n", p=128),
                          writes=[w])
                if kind in ("q", "k"):
                    dst = qT if kind == "q" else kT
                    for sub in range(4):
                        r0 = (aux + sub) * 128
                        for (t0, tw, isc) in TT_TILES:
                            pb = nextps(0, 5)
                            MM(pb, pb.t[:, 0:tw], [(w.t[:, dc, sub * 128:(sub + 1) * 128], hT.t[:, dc, t0:t0 + tw])
                                                   for dc in range(16)], [w, hT])
                            o = ob[cnt % 3]
                            cnt += 1
                            if isc:
                                ACT(o.t[:, 0:tw], pb.t[:, 0:tw], AF.Copy, [pb], [o])
                            else:
                                q = qf[cnt % 2]
                                sg = sgb[cnt % 2]
                                p0 = t0 - CTX
                                ACT(q.t[:, 0:tw], pb.t[:, 0:tw], AF.Copy, [pb], [q])
                                pr = nextps(5, 8)
                                MM(pr, pr.t[:, 0:tw], [(rperm.t[:, :], q.t[:, 0:tw])], [rperm, q])
                                TT("dve", sg.t[:, 0:tw], pr.t[:, 0:tw], rope.t[:, 1, p0:p0 + tw], ALU.mult,
                                   [pr, rope], [sg])
                                TT("pool", q.t[:, 0:tw], q.t[:, 0:tw], rope.t[:, 0, p0:p0 + tw], ALU.mult,
                                   [q, rope], [q])
                                TT("dve", o.t[:, 0:tw], q.t[:, 0:tw], sg.t[:, 0:tw], ALU.add, [q, sg], [o])
                            P.dma("sp", dst[r0:r0 + 128, s * SEG + t0:s * SEG + t0 + tw], o.t[:, 0:tw], reads=[o])
                elif kind == "v":
                    for tb in range(18):
                        pb = nextps(0, 5)
                        MM(pb, pb.t[:, :], [(hT.t[:, dc, tb * 128:(tb + 1) * 128], w.t[:, dc, :]) for dc in range(16)],
                           [w, hT])
                        o = ob[cnt % 3]
                        cnt += 1
                        ACT(o.t[:, :], pb.t[:, :], AF.Copy, [pb], [o])
                        P.dma("sp", vv[s * SEG + tb * 128:s * SEG + (tb + 1) * 128, aux:aux + 512], o.t[:, :],
                              reads=[o])
                elif kind == "glu":
                    for sub in range(2):
                        ch0 = aux + sub * 128
                        for (t0, tw, isc) in TT_TILES:
                            pa = nextps(0, 5)
                            MM(pa, pa.t[:, 0:tw], [(w.t[:, dc, sub * 128:(sub + 1) * 128], hT.t[:, dc, t0:t0 + tw])
                                                   for dc in range(16)], [w, hT])
                            pg = nextps(5, 8)
                            MM(pg, pg.t[:, 0:tw], [(w.t[:, dc, 256 + sub * 128:256 + (sub + 1) * 128],
                                                    hT.t[:, dc, t0:t0 + tw]) for dc in range(16)], [w, hT])
                            sg = sgb[cnt % 2]
                            z = zf[cnt % 2]
                            cnt += 1
                            ACT(sg.t[:, 0:tw], pg.t[:, 0:tw], AF.Sigmoid, [pg], [sg])
                            TT("dve", z.t[:, 0:tw], pa.t[:, 0:tw], sg.t[:, 0:tw], ALU.mult, [pa, sg], [z])
                            P.dma("sp", zT[ch0:ch0 + 128, s * SEG + t0:s * SEG + t0 + tw], z.t[:, 0:tw], reads=[z])
                else:
                    for sub in range(4):
                        for (t0, tw, isc) in TT_TILES:
                            pb = nextps(0, 5)
                            MM(pb, pb.t[:, 0:tw], [(w.t[:, dc, sub * 128:(sub + 1) * 128], hT.t[:, dc, t0:t0 + tw])
                                                   for dc in range(16)], [w, hT])
                            o = ob[cnt % 3]
                            cnt += 1
                            ACT(o.t[:, 0:tw], pb.t[:, 0:tw], AF.Copy, [pb], [o])
                            P.dma("sp", fT[sub * 128:(sub + 1) * 128, s * SEG + t0:s * SEG + t0 + tw], o.t[:, 0:tw],
                                  reads=[o])
            P.pop_scope()
        if stop_after == ("A", l):
            break

        for s in range(NSMP):
            P.push_scope()
            kh = [P.sbuf("b_kh%d" % i, [128, SEG], BF16) for i in range(2)]
            qh = [P.sbuf("b_qh%d" % i, [128, SEG], BF16) for i in range(2)]
            vh = [P.sbuf("b_vh%d" % i, [128, 18, 132], BF16) for i in range(2)]
            Eb = [P.sbuf("b_E%d" % i, [128, 18, 512], BF16) for i in range(2)]
            osb = [P.sbuf("b_o%d" % i, [128, 4, 128], F32) for i in range(2)]
            onb = [P.sbuf("b_on%d" % i, [128, 128], F32) for i in range(2)]
            rr = P.sbuf("b_rr", [128, 64], F32)
            rq = P.sbuf("b_rq", [128, 64], F32)
            mhalf = P.sbuf("b_mhalf", [128, 1], F32)
            junk = P.sbuf("b_junk", [128, 128], F32)
            mo = [P.sbuf("b_mo%d" % i, [128, 512], BF16) for i in range(2)]
            zp = [P.sbuf("c_zp%d" % i, [128, LAT + 30], F32) for i in range(2)]
            accL = P.sbuf("c_accL", [128, 4, LAT], F32)
            accC = P.sbuf("c_accC", [128, 4, CTX], F32)
            MEMSET("pool", mhalf.t[:, :], -0.5, [mhalf])
            for i in range(2):
                MEMSET("pool", vh[i].t[:, :, 128:132], 1.0, [vh[i]])
            rcs = [0, 0]

            def rcols(n, which=0):
                if rcs[which] + n > 64:
                    rcs[which] = 0
                c = rcs[which]
                rcs[which] += n
                return c

            cw = smallp.t[:, SP_CW:SP_CW + 124].rearrange("p (c k) -> p c k", c=4)

            def conv_gen():
                zc = 0
                for (g0, n, acc) in ((0, CTX, accC), (CTX, LAT, accL)):
                    for cc in range(4):
                        z = zp[zc % 2]
                        zc += 1
                        MEMSET("pool", z.t[:, 0:15], 0.0, [z])
                        MEMSET("pool", z.t[:, 15 + n:30 + n], 0.0, [z])
                        P.dma("sp", z.t[:, 15:15 + n], zT[cc * 128:(cc + 1) * 128, s * SEG + g0:s * SEG + g0 + n],
                              writes=[z])
                        TS("dve", acc.t[:, cc, 0:n], z.t[:, 0:n], cw[:, cc, 0:1],
                           smallp.t[:, SP_CB + cc:SP_CB + cc + 1], ALU.mult, ALU.add, [z, smallp], [acc])
                        yield
                        for k in range(1, 31):
                            STT(acc.t[:, cc, 0:n], z.t[:, k:k + n], cw[:, cc, k:k + 1], acc.t[:, cc, 0:n], ALU.mult,
                                ALU.add, [z, smallp, acc], [acc])
                            yield

            units = [(h, ti, m) for h in range(H) for ti in range(len(TT_TILES)) for m in range(2)]
            loaded = set()

            def load_head(h):
                if h in loaded:
                    return
                loaded.add(h)
                k_, q_, v_ = kh[h % 2], qh[h % 2], vh[h % 2]
                P.dma("sp", k_.t[:, :], kT[h * 128:(h + 1) * 128, s * SEG:(s + 1) * SEG], writes=[k_])
                P.dma("sp", q_.t[:, :], qT[h * 128:(h + 1) * 128, s * SEG:(s + 1) * SEG], writes=[q_])
                P.dma("sp", v_.t[:, :, 0:128],
                      vv[s * SEG:(s + 1) * SEG, h * 128:(h + 1) * 128].rearrange("(c p) e -> p c e", p=128),
                      writes=[v_])

            def post_fn(h, ti):
                q0, qw, isc = TT_TILES[ti]
                nj = qw // 128
                qtc = h * len(TT_TILES) + ti
                o_ = osb[qtc % 2]
                m_o = mo[qtc % 2]

                def post():
                    pT = nextps(6, 8)
                    for j in range(nj):
                        c0 = rcols(3, 1)
                        on = onb[j % 2]
                        P.op("dve", lambda e, j=j, c0=c0, junk=junk, rq=rq, o_=o_: e.scalar_tensor_tensor(
                            junk.t[:, :], o_.t[:, j, :], 1.0, o_.t[:, j, :], ALU.mult, ALU.mult,
                            accum_out=rq.t[:, c0:c0 + 1]), reads=[o_], writes=[junk, rq])
                        TS("dve", rq.t[:, c0 + 1:c0 + 2], rq.t[:, c0:c0 + 1], 1.0 / 128, EPS, ALU.mult, ALU.add,
                           [rq], [rq])
                        TT("pool", rq.t[:, c0 + 2:c0 + 3], rq.t[:, c0 + 1:c0 + 2], mhalf.t[:, 0:1], ALU.pow,
                           [rq, mhalf], [rq])
                        TS("dve", on.t[:, :], o_.t[:, j, :], rq.t[:, c0 + 2:c0 + 3], None, ALU.mult, ALU.bypass,
                           [o_, rq], [on])
                        TR(pT, pT.t[:, j * 128:(j + 1) * 128], on.t[:, :], 128, [on])
                    TS("dve", m_o.t[:, 0:qw], pT.t[:, 0:qw], gsubs.t[:, 0:1], None, ALU.mult, ALU.bypass,
                       [pT, gsubs], [m_o])
                    P.dma("sp", mixT[h * 128:(h + 1) * 128, s * SEG + q0:s * SEG + q0 + qw], m_o.t[:, 0:qw],
                          reads=[m_o])
                return post

            def pv_gen(u):
                h, ti, m = u
                v_ = vh[h % 2]
                q0, qw, isc = TT_TILES[ti]
                nkc = 2 if isc else 18
                nj = qw // 128
                qtc = h * len(TT_TILES) + ti
                o_ = osb[qtc % 2]
                E = Eb[m]
                for j in range(nj):
                    pO = nextps(3, 6)
                    for kc in range(nkc):
                        P.op("pe", lambda e, pO=pO, E=E, v_=v_, j=j, kc=kc, nkc=nkc: e.matmul(
                            pO.t[:, 0:129], E.t[:, kc, j * 128:(j + 1) * 128], v_.t[:, kc, 0:129],
                            start=(kc == 0), stop=(kc == nkc - 1)), reads=[E, v_], writes=[pO])
                        yield
                    c1 = rcols(2, 0)
                    r1 = rr.t[:, c1:c1 + 1]
                    RECIP(r1, pO.t[:, 128:129], [pO], [rr])
                    if m == 0:
                        TS("dve", o_.t[:, j, :], pO.t[:, 0:128], r1, None, ALU.mult, ALU.bypass, [pO, rr], [o_])
                    else:
                        r2 = rr.t[:, c1 + 1:c1 + 2]
                        TT("dve", r2, r1, neglam.t[:, 0:1], ALU.mult, [rr, neglam], [rr])
                        STT(o_.t[:, j, :], pO.t[:, 0:128], r2, o_.t[:, j, :], ALU.mult, ALU.add,
                            [pO, rr, o_], [o_])

            def block(u_next, u_cur):
                pv = pv_gen(u_cur) if u_cur is not None else None
                npv = 0
                if u_cur is not None:
                    _, qw_c, isc_c = TT_TILES[u_cur[1]]
                    npv = (qw_c // 128) * (2 if isc_c else 18)
                if u_next is not None:
                    h, ti, m = u_next
                    load_head(h)
                    k_, q_ = kh[h % 2], qh[h % 2]
                    q0, qw, isc = TT_TILES[ti]
                    nkc = 2 if isc else 18
                    E = Eb[m]
                    per = -(-npv // nkc)
                    for kc in range(nkc):
                        pS = nextps(0, 3)
                        MM(pS, pS.t[:, 0:qw], [(k_.t[m * 64:(m + 1) * 64, kc * 128:(kc + 1) * 128],
                                                q_.t[m * 64:(m + 1) * 64, q0:q0 + qw])], [k_, q_])
                        ACT(E.t[:, kc, 0:qw], pS.t[:, 0:qw], AF.Exp, [pS], [E], scale=0.125)
                        if pv is not None:
                            for _ in range(per):
                                next(pv, None)
                if pv is not None:
                    for _ in pv:
                        pass

            cg = conv_gen()
            pending = None
            block(units[0], None)
            for ui, u in enumerate(units):
                block(units[ui + 1] if ui + 1 < len(units) else None, u)
                if pending is not None:
                    pending()
                    pending = None
                if u[2] == 1:
                    pending = post_fn(u[0], u[1])
                for _ in range(4):
                    next(cg, None)
            if pending is not None:
                pending()
            for _ in cg:
                pass

            cb16 = P.sbuf("c_cb", [128, 4, 512], BF16)
            sq16 = P.sbuf("c_sq", [128, 4, 512], BF16)
            mean = P.sbuf("c_mean", [128, 512], F32)
            rstd = P.sbuf("c_rstd", [128, 512], F32)
            tmp = [P.sbuf("c_tmp%d" % i, [128, 512], F32) for i in range(2)]
            co = [P.sbuf("c_o%d" % i, [128, 512], BF16) for i in range(2)]
            for (g0, n, acc) in ((0, CTX, accC), (CTX, LAT, accL)):
                for t0 in range(0, n, 512):
                    tw = min(512, n - t0)
                    for cc in range(4):
                        ACT(cb16.t[:, cc, 0:tw], acc.t[:, cc, t0:t0 + tw], AF.Copy, [acc], [cb16])
                        ACT(sq16.t[:, cc, 0:tw], acc.t[:, cc, t0:t0 + tw], AF.Square, [acc], [sq16])
                    pM = nextps(0, 4)
                    MM(pM, pM.t[:, 0:tw], [(onesb.t[:, :], cb16.t[:, cc, 0:tw]) for cc in range(4)], [onesb, cb16])
                    pQ = nextps(4, 8)
                    MM(pQ, pQ.t[:, 0:tw], [(onesb.t[:, :], sq16.t[:, cc, 0:tw]) for cc in range(4)], [onesb, sq16])
                    TS("dve", mean.t[:, 0:tw], pM.t[:, 0:tw], 1.0 / 512, None, ALU.mult, ALU.bypass, [pM], [mean])
                    TT("dve", rstd.t[:, 0:tw], mean.t[:, 0:tw], mean.t[:, 0:tw], ALU.mult, [mean], [rstd])
                    STT(rstd.t[:, 0:tw], pQ.t[:, 0:tw], 1.0 / 512, rstd.t[:, 0:tw], ALU.mult, ALU.subtract,
                        [pQ, rstd], [rstd])
                    ACT(rstd.t[:, 0:tw], rstd.t[:, 0:tw], AF.Sqrt, [rstd, epst], [rstd], bias=epst.t[:, 0:1])
                    RECIP(rstd.t[:, 0:tw], rstd.t[:, 0:tw], [rstd], [rstd])
                    for cc in range(4):
                        t_ = tmp[cc % 2]
                        o = co[cc % 2]
                        TT("dve", t_.t[:, 0:tw], acc.t[:, cc, t0:t0 + tw], mean.t[:, 0:tw], ALU.subtract,
                           [acc, mean], [t_])
                        TT("pool", t_.t[:, 0:tw], t_.t[:, 0:tw], rstd.t[:, 0:tw], ALU.mult, [t_, rstd], [t_])
                        ACT(o.t[:, 0:tw], t_.t[:, 0:tw], AF.Silu, [t_, smallp], [o],
                            scale=smallp.t[:, SP_LG + cc:SP_LG + cc + 1], bias=smallp.t[:, SP_LB + cc:SP_LB + cc + 1])
                        P.dma("sp", mixT[1024 + cc * 128:1024 + (cc + 1) * 128,
                                         s * SEG + g0 + t0:s * SEG + g0 + t0 + tw], o.t[:, 0:tw], reads=[o])
            P.pop_scope()
        if stop_after in (("B", l), ("C", l)):
            break

        P.push_scope()
        Aall = [P.sbuf("d_A%d" % i, [128, 16, 256], BF16) for i in range(8)]
        Ac = [P.sbuf("d_Ac%d" % i, [128, 2, 256], BF16) for i in range(8)]
        uT = [P.sbuf("d_u%d" % i, [128, LAT], BF16) for i in range(2)]
        uC = [P.sbuf("d_uc%d" % i, [128, CTX], BF16) for i in range(2)]
        csc = P.sbuf("d_csc", [128, 256], BF16)
        tabs = [P.sbuf("d_tab%d" % i, [128, 16, 2, 512], BF16) for i in range(2)]
        tabc = P.sbuf("d_tabc", [128, 2, 2, 256], BF16)
        do = [P.sbuf("d_o%d" % i, [128, 512], BF16) for i in range(3)]
        P.dma("pool", csc.t[:, :], dftc_in[:, :], writes=[csc])
        for cs in range(2):
            P.dma("sp", tabc.t[:, :, cs, :], dft256_in[cs, :, :].rearrange("(c p) n -> p c n", p=128), writes=[tabc])
        dc_ = 0
        for s in range(NSMP):
            for fh in range(4):
                u = uT[(s * 4 + fh) % 2]
                uc = uC[(s * 4 + fh) % 2]
                A = Aall[s * 4 + fh]
                A2 = Ac[s * 4 + fh]
                P.dma("sp", u.t[:, :], fT[fh * 128:(fh + 1) * 128, s * SEG + CTX:(s + 1) * SEG], writes=[u])
                P.dma("sp", uc.t[:, :], fT[fh * 128:(fh + 1) * 128, s * SEG:s * SEG + CTX], writes=[uc])
                for ch in range(16):
                    pb = nextps(0, 4)
                    MM(pb, pb.t[:, 0:256], [(u.t[:, ch * 128:(ch + 1) * 128], csc.t[:, :])], [u, csc])
                    evrr[0] += 1
                    if evrr[0] % 2 == 0:
                        ACT(A.t[:, ch, :], pb.t[:, 0:256], AF.Copy, [pb], [A])
                    else:
                        P.op("dve", lambda e, A=A, pb=pb, ch=ch: e.tensor_copy(A.t[:, ch, :], pb.t[:, 0:256]),
                             reads=[pb], writes=[A])
                for ch in range(2):
                    pb = nextps(0, 4)
                    MM(pb, pb.t[:, 0:256], [(uc.t[:, ch * 128:(ch + 1) * 128], csc.t[:, :])], [uc, csc])
                    ACT(A2.t[:, ch, :], pb.t[:, 0:256], AF.Copy, [pb], [A2])
                pb = nextps(4, 8)
                pairs = []
                for ch in range(2):
                    pairs.append((A2.t[:, ch, 0:128], tabc.t[:, ch, 0, :]))
                    pairs.append((A2.t[:, ch, 128:256], tabc.t[:, ch, 1, :]))
                MM(pb, pb.t[:, 0:256], pairs, [A2, tabc])
                o = do[dc_ % 3]
                dc_ += 1
                ACT(o.t[:, 0:256], pb.t[:, 0:256], AF.Copy, [pb], [o])
                P.dma("sp", mixT[1536 + fh * 128:1536 + (fh + 1) * 128, s * SEG:s * SEG + CTX], o.t[:, 0:256],
                      reads=[o])
        for nt in range(4):
            tb = tabs[nt % 2]
            for cs in range(2):
                P.dma("sp", tb.t[:, :, cs, :],
                      dftn_in[cs, :, nt * 512:(nt + 1) * 512].rearrange("(c p) n -> p c n", p=128), writes=[tb])
            for s in range(NSMP):
                for fh in range(4):
                    A = Aall[s * 4 + fh]
                    pb = nextps(4, 8)
                    pairs = []
                    for ch in range(16):
                        pairs.append((A.t[:, ch, 0:128], tb.t[:, ch, 0, :]))
                        pairs.append((A.t[:, ch, 128:256], tb.t[:, ch, 1, :]))
                    MM(pb, pb.t[:, :], pairs, [A, tb])
                    o = do[dc_ % 3]
                    dc_ += 1
                    ACT(o.t[:, :], pb.t[:, :], AF.Copy, [pb], [o])
                    P.dma("sp", mixT[1536 + fh * 128:1536 + (fh + 1) * 128,
                                     s * SEG + CTX + nt * 512:s * SEG + CTX + (nt + 1) * 512], o.t[:, :], reads=[o])
        P.pop_scope()
        if stop_after == ("D", l):
            break

        P.push_scope()
        wout = P.sbuf("e_wout", [128, 16, D], BF16)
        gtb = [P.sbuf("e_gtb%d" % v, [128, D], F32) for v in range(3)]
        mx = [P.sbuf("e_mx%d" % i, [128, 16, 128], BF16) for i in range(2)]
        xt = [P.sbuf("e_xt%d" % i, [128, D], F32) for i in range(2)]
        tmp = [P.sbuf("e_tmp%d" % i, [128, 512], F32) for i in range(2)]
        for ct in range(4):
            P.dma("pool", wout.t[:, :, ct * 512:(ct + 1) * 512],
                  w_out[l, :, ct * 512:(ct + 1) * 512].rearrange("(c p) n -> p c n", p=128), writes=[wout])
        for v in range(3):
            P.dma("sp", gtb[v].t[:, :], gt_d[0, v, :].partition_broadcast(128), writes=[gtb[v]])
        tc_ = 0
        for b in range(NSMP * 18):
            s, bb = divmod(b, 18)
            v = variant(s, bb < 2)
            m_ = mx[b % 2]
            x_ = xt[b % 2]
            P.dma("sp", m_.t[:, :, :], mixT[:, b * 128:(b + 1) * 128].rearrange("(c p) t -> p c t", p=128),
                  writes=[m_])
            P.dma("sp", x_.t[:, :], xs[b * 128:(b + 1) * 128, :], writes=[x_])
            for dt_ in range(4):
                pb = nextps(0, 8)
                MM(pb, pb.t[:, :], [(m_.t[:, fc, :], wout.t[:, fc, dt_ * 512:(dt_ + 1) * 512]) for fc in range(16)],
                   [m_, wout])
                t_ = tmp[tc_ % 2]
                tc_ += 1
                TT("dve", t_.t[:, :], pb.t[:, :], gtb[v].t[:, dt_ * 512:(dt_ + 1) * 512], ALU.mult, [pb, gtb[v]], [t_])
                TT("pool", x_.t[:, dt_ * 512:(dt_ + 1) * 512], x_.t[:, dt_ * 512:(dt_ + 1) * 512], t_.t[:, :],
                   ALU.add, [x_, t_], [x_])
            P.dma("sp", xs[b * 128:(b + 1) * 128, :], x_.t[:, :], reads=[x_])
        P.pop_scope()
        if stop_after == ("E", l):
            break

        P.push_scope()
        wgu = [P.sbuf("f_wgu%d" % i, [128, 2, 16, 512], BF16) for i in range(2)]
        wd = P.sbuf("f_wd", [128, 8, D], BF16)
        for hf in range(2):
            P.dma("pool", wgu[hf].t[:, 0, :, :],
                  w_gate[l, 0, :, hf * 512:(hf + 1) * 512].rearrange("(c p) n -> p c n", p=128), writes=[wgu[hf]])
            P.dma("pool", wgu[hf].t[:, 1, :, :],
                  w_up[l, 0, :, hf * 512:(hf + 1) * 512].rearrange("(c p) n -> p c n", p=128), writes=[wgu[hf]])
        for hh in range(2):
            P.dma("pool", wd.t[:, hh * 4:(hh + 1) * 4, :],
                  w_down[l, 0, hh * 512:(hh + 1) * 512, :].rearrange("(c p) n -> p c n", p=128), writes=[wd])
        valsTL = P.sbuf("f_valsTL", [128, 2, 32], F32)
        valsTC = P.sbuf("f_valsTC", [32, 32], F32)
        idxTL = P.sbuf("f_idxTL", [128, 2, 32], U32)
        idxTC = P.sbuf("f_idxTC", [32, 32], U32)
        P.push_scope()
        affp = P.sbuf("f_affp", [128, 18, 32], F32)
        affL = P.sbuf("f_affL", [32, LAT], F32)
        affC = P.sbuf("f_affC", [32, CTX], F32)
        valsL = P.sbuf("f_valsL", [32, CAPL], F32)
        valsC = P.sbuf("f_valsC", [32, CAPC], F32)
        idxL = P.sbuf("f_idxL", [32, CAPL], U32)
        idxC = P.sbuf("f_idxC", [32, CAPC], U32)
        idxLf = P.sbuf("f_idxLf", [32, CAPL], F32)
        idxCf = P.sbuf("f_idxCf", [32, CAPC], F32)
        idxTLf = P.sbuf("f_idxTLf", [128, 2, 32], F32)
        idxTCf = P.sbuf("f_idxTCf", [32, 32], F32)
        xt = [P.sbuf("f_xt%d" % i, [128, D], F32) for i in range(2)]
        xnb = [P.sbuf("f_xn%d" % i, [128, D], F32) for i in range(2)]
        junk = P.sbuf("f_junk", [128, D], BF16)
        ssb = P.sbuf("f_ss", [128, 192], F32)
        h2T = [P.sbuf("f_h2T%d" % i, [128, 16, 128], BF16) for i in range(2)]
        wr = P.sbuf("f_wr", [128, 16, NE], BF16)
        sm = P.sbuf("f_sm", [128, 4 * 36], F32)
        ex = [P.sbuf("f_ex%d" % i, [128, NE], F32) for i in range(2)]
        P.dma("pool", wr.t[:, :, :], w_router[l, :, :].rearrange("(c p) e -> p c e", p=128), writes=[wr])
        for s in range(NSMP):
            for bb in range(18):
                b = s * 18 + bb
                x_ = xt[b % 2]
                xn_ = xnb[b % 2]
                h_ = h2T[b % 2]
                norm_block(x_, xn_, junk, ssb, bb, b * 128)
                P.dma("sp", xn_d[b * 128:(b + 1) * 128, :], xn_.t[:, :], reads=[xn_])
                transpose_mod(xn_, 128, h_, 0, a2, 48, variant(s, bb < 2), 0, 4)
                pb = nextps(4, 8)
                MM(pb, pb.t[:, 0:NE], [(h_.t[:, c, :], wr.t[:, c, :]) for c in range(16)], [h_, wr])
                c0 = b * 4
                P.op("dve", lambda e, pb=pb, c0=c0, sm=sm: e.reduce_max(sm.t[:, c0:c0 + 1], pb.t[:, 0:NE], AX.X),
                     reads=[pb], writes=[sm])
                TS("dve", sm.t[:, c0 + 1:c0 + 2], sm.t[:, c0:c0 + 1], -1.0, None, ALU.mult, ALU.bypass, [sm], [sm])
                e_ = ex[b % 2]
                ACT(e_.t[:, :], pb.t[:, 0:NE], AF.Exp, [pb, sm], [e_, sm], bias=sm.t[:, c0 + 1:c0 + 2],
                    accum_out=sm.t[:, c0 + 2:c0 + 3])
                RECIP(sm.t[:, c0 + 3:c0 + 4], sm.t[:, c0 + 2:c0 + 3], [sm], [sm])
                TS("dve", affp.t[:, bb, s * 16:(s + 1) * 16], e_.t[:, :], sm.t[:, c0 + 3:c0 + 4], None, ALU.mult,
                   ALU.bypass, [e_, sm], [affp])
        for bb in range(18):
            pb = nextps(0, 4)
            TR(pb, pb.t[0:32, 0:128], affp.t[:, bb, :], 128, [affp])
            if bb < 2:
                ACT(affC.t[:, bb * 128:(bb + 1) * 128], pb.t[0:32, 0:128], AF.Copy, [pb], [affC])
            else:
                ACT(affL.t[:, (bb - 2) * 128:(bb - 1) * 128], pb.t[0:32, 0:128], AF.Copy, [pb], [affL])
        for (aff, vals, idx, cap) in ((affL, valsL, idxL, CAPL), (affC, valsC, idxC, CAPC)):
            for r in range(cap // 8):
                P.op("dve", lambda e, aff=aff, vals=vals, r=r: e.max(out=vals.t[:, r * 8:(r + 1) * 8], in_=aff.t[:, :]),
                     reads=[aff], writes=[vals])
                P.op("dve", lambda e, aff=aff, vals=vals, idx=idx, r=r: e.max_index(
                    out=idx.t[:, r * 8:(r + 1) * 8], in_max=vals.t[:, r * 8:(r + 1) * 8], in_values=aff.t[:, :]),
                    reads=[aff, vals], writes=[idx])
                P.op("dve", lambda e, aff=aff, vals=vals, r=r: e.match_replace(
                    out=aff.t[:, :], in_to_replace=vals.t[:, r * 8:(r + 1) * 8], in_values=aff.t[:, :],
                    imm_value=-1.0), reads=[aff, vals], writes=[aff])
        for (idx, idxf, col) in ((idxL, idxLf, 0), (idxC, idxCf, 1)):
            P.op("dve", lambda e, idx=idx, idxf=idxf: e.tensor_copy(idxf.t[:, :], idx.t[:, :]), reads=[idx],
                 writes=[idxf])
            TS("dve", idxf.t[:, :], idxf.t[:, :], rowbase.t[:, col:col + 1], None, ALU.add, ALU.bypass,
               [idxf, rowbase], [idxf])
        for blk in range(2):
            pb = nextps(0, 4)
            TR(pb, pb.t[:, 0:32], valsL.t[:, blk * 128:(blk + 1) * 128], 32, [valsL])
            ACT(valsTL.t[:, blk, :], pb.t[:, 0:32], AF.Copy, [pb], [valsTL])
            pb = nextps(0, 4)
            TR(pb, pb.t[:, 0:32], idxLf.t[:, blk * 128:(blk + 1) * 128], 32, [idxLf])
            ACT(idxTLf.t[:, blk, :], pb.t[:, 0:32], AF.Copy, [pb], [idxTLf])
        pb = nextps(0, 4)
        TR(pb, pb.t[0:32, 0:32], valsC.t[:, :], 32, [valsC])
        ACT(valsTC.t[:, :], pb.t[0:32, 0:32], AF.Copy, [pb], [valsTC])
        pb = nextps(0, 4)
        TR(pb, pb.t[0:32, 0:32], idxCf.t[:, :], 32, [idxCf])
        ACT(idxTCf.t[:, :], pb.t[0:32, 0:32], AF.Copy, [pb], [idxTCf])
        P.op("dve", lambda e, a=idxTL, b_=idxTLf: e.tensor_copy(a.t[:, :, :], b_.t[:, :, :]), reads=[idxTLf],
             writes=[idxTL])
        P.op("dve", lambda e, a=idxTC, b_=idxTCf: e.tensor_copy(a.t[:, :], b_.t[:, :]), reads=[idxTCf],
             writes=[idxTC])
        P.pop_scope()

        xg = [P.sbuf("f_xg%d" % i, [128, D], F32) for i in range(2)]
        xsT = P.sbuf("f_xsT", [128, 16, 576], BF16)
        hidT = P.sbuf("f_hidT", [128, 8, 576], BF16)
        sgt = [P.sbuf("f_sgt%d" % i, [128, 288], F32) for i in range(2)]
        yo = [P.sbuf("f_yo%d" % i, [128, D], F32) for i in range(2)]
        gt2b = [P.sbuf("f_gt2b%d" % v, [128, D], F32) for v in range(3)]
        for v in range(3):
            P.dma("sp", gt2b[v].t[:, :], gt_d[1, v, :].partition_broadcast(128), writes=[gt2b[v]])
        xacc = [[Buf("xacc%d%d" % (s, g)) for g in range(2)] for s in range(NSMP)]
        blocks = []
        for s in range(NSMP):
            blocks.append((s, 0, 0, 128, s * 288))
            blocks.append((s, 0, 1, 128, s * 288 + 128))
            blocks.append((s, 1, 0, 32, s * 288 + 256))
        gcs = [0, 0, 0]

        def load_wgu(e_, hf):
            P.dma("pool", wgu[hf].t[:, 0, :, :],
                  w_gate[l, e_, :, hf * 512:(hf + 1) * 512].rearrange("(c p) n -> p c n", p=128), writes=[wgu[hf]])
            P.dma("pool", wgu[hf].t[:, 1, :, :],
                  w_up[l, e_, :, hf * 512:(hf + 1) * 512].rearrange("(c p) n -> p c n", p=128), writes=[wgu[hf]])

        def load_wd(e_):
            for hh in range(2):
                P.dma("pool", wd.t[:, hh * 4:(hh + 1) * 4, :],
                      w_down[l, e_, hh * 512:(hh + 1) * 512, :].rearrange("(c p) n -> p c n", p=128), writes=[wd])

        def gather(e_, bi):
            (s, isc, blk, rows, cb) = blocks[bi]
            g_ = xg[gcs[0] % 2]
            gcs[0] += 1
            col = s * 16 + e_
            iap = idxTC.t[0:32, col:col + 1] if isc else idxTL.t[:, blk, col:col + 1]
            ibuf = idxTC if isc else idxTL
            P.dma_fn("pool", lambda e, g_=g_, rows=rows, iap=iap: e.indirect_dma_start(
                out=g_.t[0:rows, :], out_offset=None, in_=xn_d[:, :],
                in_offset=bass.IndirectOffsetOnAxis(ap=iap, axis=0)), reads=[ibuf], writes=[g_])
            return g_

        def gate_up(hf):
            for fc in range(4):
                for sg_ in range(2):
                    cs_ = slice(sg_ * 288, (sg_ + 1) * 288)
                    pg = nextps(3, 6)
                    MM(pg, pg.t[:, 0:288], [(wgu[hf].t[:, 0, dc, fc * 128:(fc + 1) * 128], xsT.t[:, dc, cs_])
                                            for dc in range(16)], [wgu[hf], xsT])
                    pu = nextps(3, 6)
                    MM(pu, pu.t[:, 0:288], [(wgu[hf].t[:, 1, dc, fc * 128:(fc + 1) * 128], xsT.t[:, dc, cs_])
                                            for dc in range(16)], [wgu[hf], xsT])
                    sg = sgt[gcs[1] % 2]
                    gcs[1] += 1
                    ACT(sg.t[:, :], pg.t[:, 0:288], AF.Silu, [pg], [sg])
                    TT("dve", hidT.t[:, hf * 4 + fc, cs_], sg.t[:, :], pu.t[:, 0:288], ALU.mult, [sg, pu], [hidT])

        pre = {0: gather(0, 0), 1: gather(0, 1)}
        for ex_ in range(NE):
            for bi, (s, isc, blk, rows, cb) in enumerate(blocks):
                g_ = pre[bi] if bi in pre else gather(ex_, bi)
                transpose_mod(g_, rows, xsT, cb, a2, 48, variant(s, isc), 0, 3)
            pre = {}
            gate_up(0)
            if ex_ + 1 < NE:
                load_wgu(ex_ + 1, 0)
            gate_up(1)
            if ex_ + 1 < NE:
                load_wgu(ex_ + 1, 1)
                pre = {0: gather(ex_ + 1, 0), 1: gather(ex_ + 1, 1)}
            for (s, isc, blk, rows, cb) in blocks:
                y_ = yo[gcs[2] % 2]
                gcs[2] += 1
                col = s * 16 + ex_
                v = variant(s, isc)
                gap = valsTC.t[0:32, col:col + 1] if isc else valsTL.t[:, blk, col:col + 1]
                gbuf = valsTC if isc else valsTL
                iap = idxTC.t[0:32, col:col + 1] if isc else idxTL.t[:, blk, col:col + 1]
                ibuf = idxTC if isc else idxTL
                for dt_ in range(4):
                    pb = nextps(6, 8)
                    MM(pb, pb.t[0:rows, :], [(hidT.t[:, fc, cb:cb + rows], wd.t[:, fc, dt_ * 512:(dt_ + 1) * 512])
                                             for fc in range(8)], [hidT, wd])
                    STT(y_.t[0:rows, dt_ * 512:(dt_ + 1) * 512], pb.t[0:rows, :], gap,
                        gt2b[v].t[0:rows, dt_ * 512:(dt_ + 1) * 512], ALU.mult, ALU.mult, [pb, gbuf, gt2b[v]], [y_])
                xa = xacc[s][isc]
                P.dma_fn("pool", lambda e, y_=y_, rows=rows, iap=iap: e.indirect_dma_start(
                    out=xs[:, :], out_offset=bass.IndirectOffsetOnAxis(ap=iap, axis=0), in_=y_.t[0:rows, :],
                    in_offset=None, compute_op=ALU.add), reads=[y_, ibuf, xa], writes=[xa])
            if ex_ + 1 < NE:
                load_wd(ex_ + 1)
        P.pop_scope()
        if stop_after == ("F", l):
            break

    if stop_after is None:
        P.push_scope()
        gfin = P.sbuf("z_gfin", [128, D], F32)
        xt = [P.sbuf("z_xt%d" % i, [128, D], F32) for i in range(2)]
        xo = [P.sbuf("z_xo%d" % i, [128, D], F32) for i in range(2)]
        junk = P.sbuf("z_junk", [128, D], BF16)
        ssb = P.sbuf("z_ss", [128, 192], F32)
        P.dma("sp", gfin.t[:, :], gfin_in[:, :], writes=[gfin])
        for s in range(NSMP):
            for bb in range(16):
                i = s * 16 + bb
                x_ = xt[i % 2]
                o_ = xo[i % 2]
                row0 = s * SEG + CTX + bb * 128
                P.dma("sp", x_.t[:, :], xs[row0:row0 + 128, :], writes=[x_])
                col = i % 64
                ACT(junk.t[:, :], x_.t[:, :], AF.Square, [x_], [junk, ssb], accum_out=ssb.t[:, col:col + 1])
                TS("dve", ssb.t[:, col + 64:col + 65], ssb.t[:, col:col + 1], 1.0 / D, EPS, ALU.mult, ALU.add,
                   [ssb], [ssb])
                TT("pool", ssb.t[:, col + 128:col + 129], ssb.t[:, col + 64:col + 65], mhalf_g.t[:, 0:1], ALU.pow,
                   [ssb, mhalf_g], [ssb])
                STT(o_.t[:, :], x_.t[:, :], ssb.t[:, col + 128:col + 129], gfin.t[:, :], ALU.mult, ALU.mult,
                    [x_, ssb, gfin], [o_])
                P.dma("sp", out_d[s, bb * 128:(bb + 1) * 128, :], o_.t[:, :], reads=[o_], final=True)
        P.pop_scope()
    else:
        P.barrier()

    for name in dump:
        ap_, shp, dt_ = scratch[name]
        o = nc.dram_tensor("dump_" + name, list(shp), dt_, kind="ExternalOutput").ap()
        P.dma("sp", o, ap_, final=True)
    P.emit()
    return nc


def _const_tables():
    import ml_dtypes
    bf = ml_dtypes.bfloat16
    ident = np.eye(128, dtype=np.float32)
    rperm = np.zeros((128, 128), np.float32)
    for dest in range(128):
        if dest % 32 < 16:
            rperm[dest + 16, dest] = -1.0
        else:
            rperm[dest - 16, dest] = 1.0
    t = np.arange(LAT)
    pos = np.stack([t // 64, t % 64], axis=-1).astype(np.float32)
    inv_freq = (10000.0 ** (-np.arange(16, dtype=np.float32) / 16)).astype(np.float32)
    dd = np.arange(64)
    ang = (pos[:, dd // 32] * inv_freq[dd % 16][None, :]).astype(np.float32)
    ropeT = np.zeros((128, 2, LAT), np.float32)
    ropeT[:, 0, :] = np.cos(ang).astype(np.float32).T[np.arange(128) % 64]
    ropeT[:, 1, :] = np.sin(ang).astype(np.float32).T[np.arange(128) % 64]

    def dft(nn):
        j = np.arange(nn, dtype=np.int64)
        m = (j[:, None] * j[None, :]) % nn
        a = 2.0 * np.pi * m.astype(np.float64) / nn
        return np.cos(a) / np.sqrt(nn), np.sin(a) / np.sqrt(nn)

    c128, s128 = dft(128)
    dftc = np.concatenate([c128, s128], axis=1).astype(np.float32)
    cn, sn = dft(LAT)
    dftn = np.stack([cn, -sn]).astype(np.float32).astype(bf)
    cc, sc = dft(CTX)
    dft256 = np.stack([cc, -sc]).astype(np.float32).astype(bf)
    rowbase = np.zeros((32, 2), np.float32)
    for s in range(NSMP):
        rowbase[s * 16:(s + 1) * 16, 0] = s * SEG + CTX
        rowbase[s * 16:(s + 1) * 16, 1] = s * SEG
    return dict(ident=ident, rperm=rperm, ropeT=ropeT, dftc=dftc, dftn=dftn, dft256=dft256, rowbase=rowbase)


def _pack_small(g_norm1, g_norm2, conv_w, conv_b, conv_ln_g, conv_ln_b, g_sub, diff_lambda):
    depth = g_norm1.shape[0]
    sp = np.zeros((depth, 128, NSP), np.float32)
    for l in range(depth):
        sp[l, :, SP_G1:SP_G1 + 16] = g_norm1[l].reshape(16, 128).T
        sp[l, :, SP_G2:SP_G2 + 16] = g_norm2[l].reshape(16, 128).T
        cw = conv_w[l].reshape(31, 4, 128)
        sp[l, :, SP_CW:SP_CW + 124] = np.transpose(cw, (2, 1, 0)).reshape(128, 124)
        sp[l, :, SP_CB:SP_CB + 4] = conv_b[l].reshape(4, 128).T
        sp[l, :, SP_LG:SP_LG + 4] = conv_ln_g[l].reshape(4, 128).T
        sp[l, :, SP_LB:SP_LB + 4] = conv_ln_b[l].reshape(4, 128).T
        sp[l, :, SP_GS] = g_sub[l]
        sp[l, :, SP_DL:SP_DL + 256] = diff_lambda[l].reshape(1, 256)
    return sp


def make_in_maps(inputs, n_cores, depth):
    f32 = lambda a: np.ascontiguousarray(np.asarray(a, dtype=np.float32))
    consts = _const_tables()
    sp = _pack_small(*(f32(inputs[k])[:depth] for k in ("g_norm1", "g_norm2", "conv_w", "conv_b", "conv_ln_g",
                                                         "conv_ln_b", "g_sub", "diff_lambda")))
    gfin = np.ascontiguousarray(np.broadcast_to(f32(inputs["g_final"])[None, :], (128, D)))
    shared = dict(consts)
    shared["smallp"] = sp
    shared["gfin"] = gfin
    for k in ("w_ada", "b_ada", "w_in", "w_out", "w_router", "w_gate", "w_up", "w_down"):
        a = inputs[k]
        shared[k] = np.asarray(a)[:depth] if depth != np.asarray(a).shape[0] else np.asarray(a)
    x = np.asarray(inputs["x"])
    ctx = np.asarray(inputs["ctx"])
    c = f32(inputs["c"])
    c_ctx = f32(inputs["c_ctx"])
    maps = []
    for i in range(n_cores):
        m = dict(shared)
        m["x"] = x[NSMP * i:NSMP * (i + 1)]
        m["ctx"] = ctx[NSMP * i:NSMP * (i + 1)]
        cv = np.stack([c[NSMP * i], c[NSMP * i + 1], c_ctx], axis=-1)
        m["cvec"] = np.ascontiguousarray(cv.reshape(16, 128, 3).transpose(1, 0, 2))
        maps.append(m)
    return maps


def kernel(**inputs):
    n_cores = 8
    depth = 4
    nc = build_program(depth=depth)
    maps = make_in_maps(inputs, n_cores, depth)
    res = run_bass_kernel_spmd(nc, maps, core_ids=list(range(n_cores)))
    out = np.concatenate([np.asarray(r["out"]) for r in res.results], axis=0)
    return out.astype(np.float32, copy=False)
```

```python
import numpy as np
import concourse.bass as bass
import concourse.mybir as mybir
from concourse.bass_utils import run_bass_kernel_spmd

F32 = mybir.dt.float32
BF16 = mybir.dt.bfloat16
I32 = mybir.dt.int32
U32 = mybir.dt.uint32
AF = mybir.ActivationFunctionType
ALU = mybir.AluOpType
AX = mybir.AxisListType

SEM_CAP = 30000
N_DMA_SEMS = 24


class Buf:
    __slots__ = ("name", "t", "last_write", "readers")

    def __init__(self, name, t=None):
        self.name = name
        self.t = t
        self.last_write = None
        self.readers = []


class _Op:
    __slots__ = ("waits", "fn", "marked", "dma_sem", "dma_val", "final")

    def __init__(self, waits, fn):
        self.waits = waits
        self.fn = fn
        self.marked = False
        self.dma_sem = None
        self.dma_val = 0
        self.final = False


class Prog:
    ENGS = ("pe", "act", "dve", "pool", "sp")

    def __init__(self, nc):
        self.nc = nc
        self.ops = {e: [] for e in self.ENGS}
        self.known = {e: {} for e in self.ENGS}
        self.ctx = []
        self.dma_sems = []
        self.dma_cnt = [0] * N_DMA_SEMS
        self.dma_last_issuer = [None] * N_DMA_SEMS
        self.dma_rr = 0
        self.finals = []
        self._n = 0
        self.n_psum = 0
        self.scopes = []

    def _enter(self, cm):
        self.ctx.append(cm)
        return cm.__enter__()

    def sbuf(self, name, shape, dtype):
        self._n += 1
        name = "%s_%d" % (name, self._n)
        t = self._enter(self.nc.sbuf_tensor(name, list(shape), dtype))
        return Buf(name, t)

    def psum(self, name, shape=(128, 512), dtype=F32):
        t = self._enter(self.nc.psum_tensor(name, list(shape), dtype))
        return Buf(name, t)

    def dram(self, name, shape, dtype, addr_space="Local"):
        t = self.nc.dram_tensor(name, list(shape), dtype, kind="Internal", addr_space=addr_space).ap()
        return Buf(name, t)

    def _deps(self, eng, reads, writes):
        deps = []
        for b in reads:
            if b.last_write is not None:
                deps.append(b.last_write)
        for b in writes:
            if b.last_write is not None:
                deps.append(b.last_write)
            deps.extend(b.readers)
        waits = []
        kn = self.known[eng]
        for tok in deps:
            kind, key, val = tok
            if kind == "c":
                if key == "pe" and eng == "pe":
                    continue
                if kn.get(key, 0) >= val:
                    continue
                kn[key] = val
                self.ops[key][val - 1].marked = True
                waits.append(tok)
            else:
                k2 = ("d", key)
                if kn.get(k2, 0) >= val:
                    continue
                kn[k2] = val
                waits.append(tok)
        return waits

    def _commit(self, tok, reads, writes):
        for b in writes:
            b.last_write = tok
            b.readers = []
        for b in reads:
            if b in writes:
                continue
            if tok[0] == "c":
                b.readers = [r for r in b.readers if not (r[0] == "c" and r[1] == tok[1])]
            b.readers.append(tok)

    def op(self, eng, fn, reads=(), writes=()):
        waits = self._deps(eng, reads, writes)
        o = _Op(waits, fn)
        self.ops[eng].append(o)
        tok = ("c", eng, len(self.ops[eng]))
        self._commit(tok, reads, writes)
        return tok

    def dma(self, eng, out, in_, reads=(), writes=(), final=False, **kw):
        def fn(e, out=out, in_=in_, kw=kw):
            return e.dma_start(out=out, in_=in_, **kw)
        return self.dma_fn(eng, fn, reads, writes, final)

    def dma_fn(self, eng, fn, reads=(), writes=(), final=False):
        waits = self._deps(eng, reads, writes)
        j = self.dma_rr
        self.dma_rr = (self.dma_rr + 1) % N_DMA_SEMS
        prev = self.dma_cnt[j]
        kn = self.known[eng]
        if prev > 0 and kn.get(("d", j), 0) < prev:
            kn[("d", j)] = prev
            waits.append(("d", j, prev))
        o = _Op(waits, fn)
        o.dma_sem = j
        self.dma_cnt[j] = prev + 16
        o.dma_val = prev + 16
        self.ops[eng].append(o)
        tok = ("d", j, prev + 16)
        self._commit(tok, reads, writes)
        if final:
            self.finals.append(tok)
        return tok

    def barrier(self):
        toks = []
        for e in self.ENGS:
            n = len(self.ops[e])
            while n > 0 and (self.ops[e][n - 1].dma_sem is not None or self.ops[e][n - 1].fn is None):
                n -= 1
            if n > 0:
                toks.append(("c", e, n))
        for j in range(N_DMA_SEMS):
            if self.dma_cnt[j] > 0:
                toks.append(("d", j, self.dma_cnt[j]))
        for e in self.ENGS:
            kn = self.known[e]
            waits = []
            for tok in toks:
                kind, key, val = tok
                if kind == "c":
                    if key == e:
                        continue
                    if kn.get(key, 0) >= val:
                        continue
                    kn[key] = val
                    self.ops[key][val - 1].marked = True
                    waits.append(tok)
                else:
                    k2 = ("d", key)
                    if kn.get(k2, 0) >= val:
                        continue
                    kn[k2] = val
                    waits.append(tok)
            if waits:
                self.ops[e].append(_Op(waits, None))

    def push_scope(self):
        self.scopes.append(len(self.ctx))

    def pop_scope(self):
        self.barrier()
        n = self.scopes.pop()
        while len(self.ctx) > n:
            self.ctx.pop().__exit__(None, None, None)

    def barrier_tokens(self):
        toks = []
        for e in self.ENGS:
            if self.ops[e]:
                toks.append(("c", e, len(self.ops[e])))
        return toks

    def emit(self):
        nc = self.nc
        fw = []
        for tok in self.finals:
            fw.append(tok)
        if fw:
            self.ops["sp"].append(_Op(fw, None))
        rank = {}
        nsem = {}
        for e in self.ENGS:
            r = 0
            for i, o in enumerate(self.ops[e]):
                if o.dma_sem is None and o.marked:
                    r += 1
                    rank[(e, i + 1)] = r
            nsem[e] = (r + SEM_CAP - 1) // SEM_CAP
        csems = {e: [self._enter(nc.semaphore("s_%s_%d" % (e, k))) for k in range(max(1, nsem[e]))]
                 for e in self.ENGS}
        dsems = [self._enter(nc.semaphore("s_dma_%d" % k)) for k in range(N_DMA_SEMS)]
        block = self._enter(nc.Block())

        def run(ename, eng):
            for i, o in enumerate(self.ops[ename]):
                for tok in o.waits:
                    if tok[0] == "c":
                        r = rank[(tok[1], tok[2])] - 1
                        eng.wait_ge(csems[tok[1]][r // SEM_CAP], (r % SEM_CAP) + 1)
                    else:
                        eng.wait_ge(dsems[tok[1]], tok[2])
                if o.fn is None:
                    continue
                ins = o.fn(eng)
                if o.dma_sem is not None:
                    ins.then_inc(dsems[o.dma_sem], 16)
                elif o.marked:
                    r = rank[(ename, i + 1)] - 1
                    ins.then_inc(csems[ename][r // SEM_CAP], 1)

        @block.tensor
        def _(e):
            run("pe", e)

        @block.scalar
        def _(e):
            run("act", e)

        @block.vector
        def _(e):
            run("dve", e)

        @block.gpsimd
        def _(e):
            run("pool", e)

        @block.sync
        def _(e):
            run("sp", e)

        while self.ctx:
            self.ctx.pop().__exit__(None, None, None)


D = 2048
LAT = 2048
CTX = 256
SEG = LAT + CTX
NSMP = 2
NT = NSMP * SEG
H = 8
INW = 4608
NE = 16
FF = 1024
CAPL = 256
CAPC = 32
EPS = 1e-6
NSP = 425
SP_G1, SP_G2, SP_CW, SP_CB, SP_LG, SP_LB, SP_GS, SP_DL = 0, 16, 32, 156, 160, 164, 168, 169


def build_program(depth=4, stop_after=None, dump=()):
    nc = bass.Bass("TRN2", target_bir_lowering=False)
    P = Prog(nc)

    def ein(name, shape, dt=F32):
        return nc.dram_tensor(name, list(shape), dt, kind="ExternalInput").ap()

    x_in = ein("x", [NSMP, LAT, D])
    ctx_in = ein("ctx", [NSMP, CTX, D])
    cvec_in = ein("cvec", [128, 16, 3])
    w_ada = ein("w_ada", [depth, D, 6 * D])
    b_ada = ein("b_ada", [depth, 6 * D])
    w_in = ein("w_in", [depth, D, INW])
    w_out = ein("w_out", [depth, D, D])
    w_router = ein("w_router", [depth, D, NE])
    w_gate = ein("w_gate", [depth, NE, D, FF])
    w_up = ein("w_up", [depth, NE, D, FF])
    w_down = ein("w_down", [depth, NE, FF, D])
    smallp_in = ein("smallp", [depth, 128, NSP])
    gfin_in = ein("gfin", [128, D])
    ident_in = ein("ident", [128, 128])
    rperm_in = ein("rperm", [128, 128])
    rope_in = ein("ropeT", [128, 2, LAT])
    dftc_in = ein("dftc", [128, 256])
    dftn_in = ein("dftn", [2, LAT, LAT], BF16)
    dft256_in = ein("dft256", [2, CTX, CTX], BF16)
    rowbase_in = ein("rowbase", [32, 2])
    out_d = nc.dram_tensor("out", [NSMP, LAT, D], F32, kind="ExternalOutput").ap()

    xs = P.dram("xs", [NT, D], F32).t
    qT = P.dram("qT", [1024, NT], BF16).t
    kT = P.dram("kT", [1024, NT], BF16).t
    vv = P.dram("vv", [NT, 1024], BF16).t
    zT = P.dram("zT", [512, NT], F32).t
    fT = P.dram("fT", [512, NT], BF16).t
    mixT = P.dram("mixT", [D, NT], BF16).t
    xn_d = P.dram("xn_d", [NT, D], F32).t
    gt_d = P.dram("gt_d", [2, 3, D], F32).t
    scratch = {"xs": (xs, [NT, D], F32), "qT": (qT, [1024, NT], BF16), "kT": (kT, [1024, NT], BF16),
               "vv": (vv, [NT, 1024], BF16), "zT": (zT, [512, NT], F32), "fT": (fT, [512, NT], BF16),
               "mixT": (mixT, [D, NT], BF16), "xn_d": (xn_d, [NT, D], F32), "gt_d": (gt_d, [2, 3, D], F32)}

    ps = [P.psum("ps%d" % i) for i in range(8)]

    ident = P.sbuf("ident", [128, 128], F32)
    rperm = P.sbuf("rperm", [128, 128], F32)
    onesb = P.sbuf("onesb", [128, 128], BF16)
    onesf = P.sbuf("onesf", [128, 128], F32)
    epst = P.sbuf("epst", [128, 1], F32)
    csil = P.sbuf("csil", [128, 16, 3], F32)
    cT3 = P.sbuf("cT3", [128, 16, 3], BF16)
    modT = P.sbuf("modT", [128, 96, 3], F32)
    smallp = P.sbuf("smallp", [128, NSP], F32)
    a1 = P.sbuf("a1", [128, 16, 3], F32)
    a2 = P.sbuf("a2", [128, 16, 3], F32)
    neglam = P.sbuf("neglam", [128, 1], F32)
    gsubs = P.sbuf("gsubs", [128, 1], F32)
    rowbase = P.sbuf("rowbase", [32, 2], F32)
    mhalf_g = P.sbuf("mhalf_g", [128, 1], F32)

    def MM(psb, out_ap, pairs, reads):
        n = len(pairs)
        for i, (l, r) in enumerate(pairs):
            P.op("pe", lambda e, l=l, r=r, i=i: e.matmul(out_ap, l, r, start=(i == 0), stop=(i == n - 1)),
                 reads=reads, writes=[psb])

    def TR(psb, out_ap, in_ap, rows, reads):
        P.op("pe", lambda e: e.transpose(out_ap, in_ap, ident.t[0:rows, 0:rows]), reads=list(reads) + [ident],
             writes=[psb])

    def ACT(out, in_, func, reads, writes, **kw):
        P.op("act", lambda e: e.activation(out, in_, func, **kw), reads=reads, writes=writes)

    def TS(eng, out, in0, s1, s2, op0, op1, reads, writes):
        P.op(eng, lambda e: e.tensor_scalar(out, in0, s1, s2, op0, op1), reads=reads, writes=writes)

    def TT(eng, out, in0, in1, op, reads, writes):
        P.op(eng, lambda e: e.tensor_tensor(out, in0, in1, op), reads=reads, writes=writes)

    def STT(out, in0, scalar, in1, op0, op1, reads, writes):
        P.op("dve", lambda e: e.scalar_tensor_tensor(out, in0, scalar, in1, op0, op1), reads=reads, writes=writes)

    def RECIP(out, in_, reads, writes):
        P.op("dve", lambda e: e.reciprocal(out, in_), reads=reads, writes=writes)

    def MEMSET(eng, ap, val, writes):
        P.op(eng, lambda e: e.memset(ap, val), reads=[], writes=writes)

    def variant(s, is_ctx):
        return 2 if is_ctx else s

    P.dma("sp", ident.t[:, :], ident_in[:, :], writes=[ident])
    P.dma("sp", rperm.t[:, :], rperm_in[:, :], writes=[rperm])
    P.dma("sp", csil.t[:, :, :], cvec_in[:, :, :], writes=[csil])
    P.dma("sp", rowbase.t[:, :], rowbase_in[:, :], writes=[rowbase])
    for s in range(NSMP):
        P.dma("sp", xs[s * SEG:s * SEG + CTX, :], ctx_in[s, :, :])
        P.dma("sp", xs[s * SEG + CTX:(s + 1) * SEG, :], x_in[s, :, :])
    MEMSET("dve", onesb.t[:, :], 1.0, [onesb])
    MEMSET("dve", onesf.t[:, :], 1.0, [onesf])
    MEMSET("dve", epst.t[:, :], EPS, [epst])
    MEMSET("dve", mhalf_g.t[:, :], -0.5, [mhalf_g])
    ACT(csil.t[:, :, :], csil.t[:, :, :], AF.Silu, [csil], [csil])
    P.op("dve", lambda e: e.tensor_copy(cT3.t[:, :, :], csil.t[:, :, :]), reads=[csil], writes=[cT3])
    P.barrier()

    def norm_block(xt, xnb, junk, ssb, col, row0, dst_store=None):
        P.dma("sp", xt.t[:, :], xs[row0:row0 + 128, :], writes=[xt])
        ACT(junk.t[:, :], xt.t[:, :], AF.Square, [xt], [junk, ssb], accum_out=ssb.t[:, col:col + 1])
        TS("dve", ssb.t[:, col + 64:col + 65], ssb.t[:, col:col + 1], 1.0 / D, EPS, ALU.mult, ALU.add, [ssb], [ssb])
        TT("pool", ssb.t[:, col + 128:col + 129], ssb.t[:, col + 64:col + 65], mhalf_g.t[:, 0:1], ALU.pow,
           [ssb, mhalf_g], [ssb])
        TS("pool", xnb.t[:, :], xt.t[:, :], ssb.t[:, col + 128:col + 129], 1.0, ALU.mult, ALU.mult,
           [xt, ssb], [xnb])

    psrr = [0]

    def nextps(lo=0, hi=8):
        i = lo + (psrr[0] % (hi - lo))
        psrr[0] += 1
        return ps[i]

    evrr = [0]

    def evac_mod(out_ap, in_ap, sc_ap, bi_ap, reads, writes):
        evrr[0] += 1
        if evrr[0] % 2 == 0:
            ACT(out_ap, in_ap, AF.Identity, reads, writes, scale=sc_ap, bias=bi_ap)
        else:
            TS("dve", out_ap, in_ap, sc_ap, bi_ap, ALU.mult, ALU.add, reads, writes)

    def transpose_mod(xnb, rows, dst, dst_col0, amod, shoff, v, lo=0, hi=4):
        for g in range(4):
            pb = nextps(lo, hi)
            for i in range(4):
                c = g * 4 + i
                TR(pb, pb.t[:, i * 128:i * 128 + rows], xnb.t[0:rows, c * 128:(c + 1) * 128], rows, [xnb])
            for i in range(4):
                c = g * 4 + i
                evac_mod(dst.t[:, c, dst_col0:dst_col0 + rows], pb.t[:, i * 128:i * 128 + rows],
                         amod.t[:, c, v:v + 1], modT.t[:, shoff + c, v:v + 1], [pb, amod, modT], [dst])

    TT_TILES = [(0, 256, True), (256, 512, False), (768, 512, False), (1280, 512, False), (1792, 512, False)]

    for l in range(depth):
        lam_init = 0.8 - 0.6 * float(np.exp(-0.3 * l))
        P.push_scope()
        P.dma("sp", smallp.t[:, :], smallp_in[l, :, :], writes=[smallp])
        wsl = [P.sbuf("m_wsl%d" % i, [128, 16, 512], BF16) for i in range(2)]
        bsl = [P.sbuf("m_bsl%d" % i, [1, 512], F32) for i in range(2)]
        gtmp = [P.sbuf("m_gtmp%d" % i, [128, 512], F32) for i in range(2)]
        crep = P.sbuf("m_crep", [128, 16, 3, 128], BF16)
        for j in range(16):
            for v in range(3):
                TS("dve", crep.t[:, j, v, :], onesf.t[:, :], csil.t[:, j, v:v + 1], None, ALU.mult, ALU.bypass,
                   [onesf, csil], [crep])
        gi = 0
        for k in range(24):
            w = wsl[k % 2]
            b = bsl[k % 2]
            P.dma("pool", w.t[:, :, :], w_ada[l, :, k * 512:(k + 1) * 512].rearrange("(c p) n -> p c n", p=128),
                  writes=[w])
            P.dma("sp", b.t[:, :], b_ada[l:l + 1, k * 512:(k + 1) * 512], writes=[b])
            for sub in range(4):
                pb = nextps(0, 4)
                pairs = [(w.t[:, dc, sub * 128:(sub + 1) * 128], cT3.t[:, dc, :]) for dc in range(16)]
                pairs.append((b.t[0:1, sub * 128:(sub + 1) * 128], onesf.t[0:1, 0:3]))
                MM(pb, pb.t[:, 0:3], pairs, [w, b, cT3, onesf])
                ACT(modT.t[:, k * 4 + sub, :], pb.t[:, 0:3], AF.Copy, [pb], [modT])
            if k in (8, 9, 10, 11, 20, 21, 22, 23):
                g = 0 if k < 12 else 1
                ct = k % 4
                for v in range(3):
                    pb = nextps(4, 8)
                    pairs = [(crep.t[:, dc, v, :], w.t[:, dc, :]) for dc in range(16)]
                    pairs.append((onesf.t[0:1, 0:128], b.t[0:1, :]))
                    MM(pb, pb.t[:, :], pairs, [w, b, crep, onesf])
                    gt = gtmp[gi % 2]
                    gi += 1
                    ACT(gt.t[:, :], pb.t[:, :], AF.Copy, [pb], [gt])
                    P.dma("sp", gt_d[g, v:v + 1, ct * 512:(ct + 1) * 512], gt.t[0:1, :], reads=[gt])
        for v in range(3):
            STT(a1.t[:, :, v], modT.t[:, 16:32, v], 1.0, smallp.t[:, SP_G1:SP_G1 + 16], ALU.add, ALU.mult,
                [modT, smallp], [a1])
            STT(a2.t[:, :, v], modT.t[:, 64:80, v], 1.0, smallp.t[:, SP_G2:SP_G2 + 16], ALU.add, ALU.mult,
                [modT, smallp], [a2])
        lt = P.sbuf("m_lt", [128, 2, 64], F32)
        ls = P.sbuf("m_ls", [128, 4], F32)
        dl = smallp.t[:, SP_DL:SP_DL + 256].rearrange("p (a d) -> p a d", a=4)
        TT("dve", lt.t[:, 0, :], dl[:, 0, :], dl[:, 1, :], ALU.mult, [smallp], [lt])
        TT("dve", lt.t[:, 1, :], dl[:, 2, :], dl[:, 3, :], ALU.mult, [smallp], [lt])
        P.op("dve", lambda e, ls=ls, lt=lt: e.reduce_sum(ls.t[:, 0:2], lt.t[:, :, :], AX.X), reads=[lt], writes=[ls])
        ACT(ls.t[:, 2:4], ls.t[:, 0:2], AF.Exp, [ls], [ls])
        TT("dve", neglam.t[:, :], ls.t[:, 3:4], ls.t[:, 2:3], ALU.subtract, [ls], [neglam])
        TS("dve", neglam.t[:, :], neglam.t[:, :], -lam_init, None, ALU.add, ALU.bypass, [neglam], [neglam])
        TS("dve", gsubs.t[:, :], smallp.t[:, SP_GS:SP_GS + 1], 1.0 - lam_init, None, ALU.mult, ALU.bypass,
           [smallp], [gsubs])
        P.pop_scope()
        if stop_after == ("M", l):
            break

        for s in range(NSMP):
            P.push_scope()
            hT = P.sbuf("a_hT", [128, 16, SEG], BF16)
            xt = [P.sbuf("a_xt%d" % i, [128, D], F32) for i in range(2)]
            xnb = [P.sbuf("a_xn%d" % i, [128, D], F32) for i in range(2)]
            junk = P.sbuf("a_junk", [128, D], BF16)
            ssb = P.sbuf("a_ss", [128, 192], F32)
            wsl = [P.sbuf("a_wsl%d" % i, [128, 16, 512], BF16) for i in range(2)]
            rope = P.sbuf("a_rope", [128, 2, LAT], F32)
            qf = [P.sbuf("a_qf%d" % i, [128, 512], F32) for i in range(2)]
            sgb = [P.sbuf("a_sg%d" % i, [128, 512], F32) for i in range(2)]
            ob = [P.sbuf("a_ob%d" % i, [128, 512], BF16) for i in range(3)]
            zf = [P.sbuf("a_zf%d" % i, [128, 512], F32) for i in range(2)]
            P.dma("sp", rope.t[:, :, :], rope_in[:, :, :], writes=[rope])
            for bb in range(18):
                row0 = s * SEG + bb * 128
                norm_block(xt[bb % 2], xnb[bb % 2], junk, ssb, bb, row0)
                transpose_mod(xnb[bb % 2], 128, hT, bb * 128, a1, 0, variant(s, bb < 2), 0, 4)
            slabs = [("q", 0, 0), ("q", 512, 4), ("k", 1024, 0), ("k", 1536, 4), ("v", 2048, 0), ("v", 2560, 512),
                     ("glu", 0, 0), ("glu", 256, 256), ("f", 4096, 0)]
            cnt = 0
            for si, (kind, c0, aux) in enumerate(slabs):
                w = wsl[si % 2]
                if kind == "glu":
                    P.dma("pool", w.t[:, :, 0:256],
                          w_in[l, :, 3072 + c0:3072 + c0 + 256].rearrange("(c p) n -> p c n", p=128), writes=[w])
                    P.dma("pool", w.t[:, :, 256:512],
                          w_in[l, :, 3584 + c0:3584 + c0 + 256].rearrange("(c p) n -> p c n", p=128), writes=[w])
                else:
                    P.dma("pool", w.t[:, :, :], w_in[l, :, c0:c0 + 512].rearrange("(c p) n -> p c n", p=128),
                          writes=[w])
                if kind in ("q", "k"):
                    dst = qT if kind == "q" else kT
                    for sub in range(4):
                        r0 = (aux + sub) * 128
                        for (t0, tw, isc) in TT_TILES:
                            pb = nextps(0, 5)
                            MM(pb, pb.t[:, 0:tw], [(w.t[:, dc, sub * 128:(sub + 1) * 128], hT.t[:, dc, t0:t0 + tw])
                                                   for dc in range(16)], [w, hT])
                            o = ob[cnt % 3]
                            cnt += 1
                            if isc:
                                ACT(o.t[:, 0:tw], pb.t[:, 0:tw], AF.Copy, [pb], [o])
                            else:
                                q = qf[cnt % 2]
                                sg = sgb[cnt % 2]
                                p0 = t0 - CTX
                                ACT(q.t[:, 0:tw], pb.t[:, 0:tw], AF.Copy, [pb], [q])
                                pr = nextps(5, 8)
                                MM(pr, pr.t[:, 0:tw], [(rperm.t[:, :], q.t[:, 0:tw])], [rperm, q])
                                TT("dve", sg.t[:, 0:tw], pr.t[:, 0:tw], rope.t[:, 1, p0:p0 + tw], ALU.mult,
                                   [pr, rope], [sg])
                                TT("pool", q.t[:, 0:tw], q.t[:, 0:tw], rope.t[:, 0, p0:p0 + tw], ALU.mult,
                                   [q, rope], [q])
                                TT("dve", o.t[:, 0:tw], q.t[:, 0:tw], sg.t[:, 0:tw], ALU.add, [q, sg], [o])
                            P.dma("sp", dst[r0:r0 + 128, s * SEG + t0:s * SEG + t0 + tw], o.t[:, 0:tw], reads=[o])
                elif kind == "v":
                    for tb in range(18):
                        pb = nextps(0, 5)
                        MM(pb, pb.t[:, :], [(hT.t[:, dc, tb * 128:(tb + 1) * 128], w.t[:, dc, :]) for dc in range(16)],
                           [w, hT])
                        o = ob[cnt % 3]
                        cnt += 1
                        ACT(o.t[:, :], pb.t[:, :], AF.Copy, [pb], [o])
                        P.dma("sp", vv[s * SEG + tb * 128:s * SEG + (tb + 1) * 128, aux:aux + 512], o.t[:, :],
                              reads=[o])
                elif kind == "glu":
                    for sub in range(2):
                        ch0 = aux + sub * 128
                        for (t0, tw, isc) in TT_TILES:
                            pa = nextps(0, 5)
                            MM(pa, pa.t[:, 0:tw], [(w.t[:, dc, sub * 128:(sub + 1) * 128], hT.t[:, dc, t0:t0 + tw])
                                                   for dc in range(16)], [w, hT])
                            pg = nextps(5, 8)
                            MM(pg, pg.t[:, 0:tw], [(w.t[:, dc, 256 + sub * 128:256 + (sub + 1) * 128],
                                                    hT.t[:, dc, t0:t0 + tw]) for dc in range(16)], [w, hT])
                            sg = sgb[cnt % 2]
                            z = zf[cnt % 2]
                            cnt += 1
                            ACT(sg.t[:, 0:tw], pg.t[:, 0:tw], AF.Sigmoid, [pg], [sg])
                            TT("dve", z.t[:, 0:tw], pa.t[:, 0:tw], sg.t[:, 0:tw], ALU.mult, [pa, sg], [z])
                            P.dma("sp", zT[ch0:ch0 + 128, s * SEG + t0:s * SEG + t0 + tw], z.t[:, 0:tw], reads=[z])
                else:
                    for sub in range(4):
                        for (t0, tw, isc) in TT_TILES:
                            pb = nextps(0, 5)
                            MM(pb, pb.t[:, 0:tw], [(w.t[:, dc, sub * 128:(sub + 1) * 128], hT.t[:, dc, t0:t0 + tw])
                                                   for dc in range(16)], [w, hT])
                            o = ob[cnt % 3]
                            cnt += 1
                            ACT(o.t[:, 0:tw], pb.t[:, 0:tw], AF.Copy, [pb], [o])
                            P.dma("sp", fT[sub * 128:(sub + 1) * 128, s * SEG + t0:s * SEG + t0 + tw], o.t[:, 0:tw],
                                  reads=[o])
            P.pop_scope()
        if stop_after == ("A", l):
            break

        for s in range(NSMP):
            P.push_scope()
            kh = [P.sbuf("b_kh%d" % i, [128, SEG], BF16) for i in range(2)]
            qz = [[P.sbuf("b_qz%d%d" % (m, i), [128, SEG], BF16) for i in range(2)] for m in range(2)]
            vh = [P.sbuf("b_vh%d" % i, [128, 18, 132], BF16) for i in range(2)]
            Eb = [P.sbuf("b_E%d" % i, [128, 18, 512], BF16) for i in range(2)]
            osb = [P.sbuf("b_o%d" % i, [128, 4, 128], F32) for i in range(2)]
            onb = [P.sbuf("b_on%d" % i, [128, 128], F32) for i in range(2)]
            rr = P.sbuf("b_rr", [128, 64], F32)
            rq = P.sbuf("b_rq", [128, 64], F32)
            mhalf = P.sbuf("b_mhalf", [128, 1], F32)
            junk = P.sbuf("b_junk", [128, 128], F32)
            mo = [P.sbuf("b_mo%d" % i, [128, 512], BF16) for i in range(2)]
            zp = [P.sbuf("c_zp%d" % i, [128, LAT + 30], F32) for i in range(2)]
            accL = P.sbuf("c_accL", [128, 4, LAT], F32)
            accC = P.sbuf("c_accC", [128, 4, CTX], F32)
            MEMSET("pool", mhalf.t[:, :], -0.5, [mhalf])
            for i in range(2):
                MEMSET("pool", vh[i].t[:, :, 128:132], 1.0, [vh[i]])
                MEMSET("pool", qz[0][i].t[64:128, :], 0.0, [qz[0][i]])
                MEMSET("pool", qz[1][i].t[0:64, :], 0.0, [qz[1][i]])
            rcs = [0, 0]

            def rcols(n, which=0):
                if rcs[which] + n > 64:
                    rcs[which] = 0
                c = rcs[which]
                rcs[which] += n
                return c

            cw = smallp.t[:, SP_CW:SP_CW + 124].rearrange("p (c k) -> p c k", c=4)

            def conv_gen():
                zc = 0
                for (g0, n, acc) in ((0, CTX, accC), (CTX, LAT, accL)):
                    for cc in range(4):
                        z = zp[zc % 2]
                        zc += 1
                        MEMSET("pool", z.t[:, 0:15], 0.0, [z])
                        MEMSET("pool", z.t[:, 15 + n:30 + n], 0.0, [z])
                        P.dma("sp", z.t[:, 15:15 + n], zT[cc * 128:(cc + 1) * 128, s * SEG + g0:s * SEG + g0 + n],
                              writes=[z])
                        TS("dve", acc.t[:, cc, 0:n], z.t[:, 0:n], cw[:, cc, 0:1],
                           smallp.t[:, SP_CB + cc:SP_CB + cc + 1], ALU.mult, ALU.add, [z, smallp], [acc])
                        yield
                        for k in range(1, 31):
                            STT(acc.t[:, cc, 0:n], z.t[:, k:k + n], cw[:, cc, k:k + 1], acc.t[:, cc, 0:n], ALU.mult,
                                ALU.add, [z, smallp, acc], [acc])
                            yield

            units = [(h, ti, m) for h in range(H) for ti in range(len(TT_TILES)) for m in range(2)]
            loaded = set()

            def load_head(h):
                if h in loaded:
                    return
                loaded.add(h)
                k_, v_ = kh[h % 2], vh[h % 2]
                P.dma("sp", k_.t[:, :], kT[h * 128:(h + 1) * 128, s * SEG:(s + 1) * SEG], writes=[k_])
                for m in range(2):
                    q_ = qz[m][h % 2]
                    P.dma("sp", q_.t[m * 64:(m + 1) * 64, :],
                          qT[h * 128 + m * 64:h * 128 + (m + 1) * 64, s * SEG:(s + 1) * SEG], writes=[q_])
                P.dma("sp", v_.t[:, :, 0:128],
                      vv[s * SEG:(s + 1) * SEG, h * 128:(h + 1) * 128].rearrange("(c p) e -> p c e", p=128),
                      writes=[v_])

            def post_fn(h, ti):
                q0, qw, isc = TT_TILES[ti]
                nj = qw // 128
                qtc = h * len(TT_TILES) + ti
                o_ = osb[qtc % 2]
                m_o = mo[qtc % 2]

                def post():
                    pT = nextps(6, 8)
                    for j in range(nj):
                        c0 = rcols(3, 1)
                        on = onb[j % 2]
                        P.op("dve", lambda e, j=j, c0=c0, junk=junk, rq=rq, o_=o_: e.scalar_tensor_tensor(
                            junk.t[:, :], o_.t[:, j, :], 1.0, o_.t[:, j, :], ALU.mult, ALU.mult,
                            accum_out=rq.t[:, c0:c0 + 1]), reads=[o_], writes=[junk, rq])
                        TS("dve", rq.t[:, c0 + 1:c0 + 2], rq.t[:, c0:c0 + 1], 1.0 / 128, EPS, ALU.mult, ALU.add,
                           [rq], [rq])
                        TT("pool", rq.t[:, c0 + 2:c0 + 3], rq.t[:, c0 + 1:c0 + 2], mhalf.t[:, 0:1], ALU.pow,
                           [rq, mhalf], [rq])
                        TS("dve", on.t[:, :], o_.t[:, j, :], rq.t[:, c0 + 2:c0 + 3], None, ALU.mult, ALU.bypass,
                           [o_, rq], [on])
                        TR(pT, pT.t[:, j * 128:(j + 1) * 128], on.t[:, :], 128, [on])
                    TS("dve", m_o.t[:, 0:qw], pT.t[:, 0:qw], gsubs.t[:, 0:1], None, ALU.mult, ALU.bypass,
                       [pT, gsubs], [m_o])
                    P.dma("sp", mixT[h * 128:(h + 1) * 128, s * SEG + q0:s * SEG + q0 + qw], m_o.t[:, 0:qw],
                          reads=[m_o])
                return post

            def pv_gen(u):
                h, ti, m = u
                v_ = vh[h % 2]
                q0, qw, isc = TT_TILES[ti]
                nkc = 2 if isc else 18
                nj = qw // 128
                qtc = h * len(TT_TILES) + ti
                o_ = osb[qtc % 2]
                E = Eb[m]
                for j in range(nj):
                    pO = nextps(3, 6)
                    for kc in range(nkc):
                        P.op("pe", lambda e, pO=pO, E=E, v_=v_, j=j, kc=kc, nkc=nkc: e.matmul(
                            pO.t[:, 0:129], E.t[:, kc, j * 128:(j + 1) * 128], v_.t[:, kc, 0:129],
                            start=(kc == 0), stop=(kc == nkc - 1)), reads=[E, v_], writes=[pO])
                        yield
                    c1 = rcols(2, 0)
                    r1 = rr.t[:, c1:c1 + 1]
                    RECIP(r1, pO.t[:, 128:129], [pO], [rr])
                    if m == 0:
                        TS("dve", o_.t[:, j, :], pO.t[:, 0:128], r1, None, ALU.mult, ALU.bypass, [pO, rr], [o_])
                    else:
                        r2 = rr.t[:, c1 + 1:c1 + 2]
                        TT("dve", r2, r1, neglam.t[:, 0:1], ALU.mult, [rr, neglam], [rr])
                        STT(o_.t[:, j, :], pO.t[:, 0:128], r2, o_.t[:, j, :], ALU.mult, ALU.add,
                            [pO, rr, o_], [o_])

            def block(u_next, u_cur):
                pv = pv_gen(u_cur) if u_cur is not None else None
                npv = 0
                if u_cur is not None:
                    _, qw_c, isc_c = TT_TILES[u_cur[1]]
                    npv = (qw_c // 128) * (2 if isc_c else 18)
                if u_next is not None:
                    h, ti, m = u_next
                    load_head(h)
                    k_, q_ = kh[h % 2], qz[m][h % 2]
                    q0, qw, isc = TT_TILES[ti]
                    nkc = 2 if isc else 18
                    E = Eb[m]
                    per = -(-npv // nkc)
                    for kc in range(nkc):
                        pS = nextps(0, 3)
                        MM(pS, pS.t[:, 0:qw], [(k_.t[:, kc * 128:(kc + 1) * 128], q_.t[:, q0:q0 + qw])], [k_, q_])
                        ACT(E.t[:, kc, 0:qw], pS.t[:, 0:qw], AF.Exp, [pS], [E], scale=0.125)
                        if pv is not None:
                            for _ in range(per):
                                next(pv, None)
                if pv is not None:
                    for _ in pv:
                        pass

            cg = conv_gen()
            pending = None
            block(units[0], None)
            for ui, u in enumerate(units):
                block(units[ui + 1] if ui + 1 < len(units) else None, u)
                if pending is not None:
                    pending()
                    pending = None
                if u[2] == 1:
                    pending = post_fn(u[0], u[1])
                for _ in range(4):
                    next(cg, None)
            if pending is not None:
                pending()
            for _ in cg:
                pass

            cb16 = P.sbuf("c_cb", [128, 4, 512], BF16)
            sq16 = P.sbuf("c_sq", [128, 4, 512], BF16)
            mean = P.sbuf("c_mean", [128, 512], F32)
            rstd = P.sbuf("c_rstd", [128, 512], F32)
            tmp = [P.sbuf("c_tmp%d" % i, [128, 512], F32) for i in range(2)]
            co = [P.sbuf("c_o%d" % i, [128, 512], BF16) for i in range(2)]
            for (g0, n, acc) in ((0, CTX, accC), (CTX, LAT, accL)):
                for t0 in range(0, n, 512):
                    tw = min(512, n - t0)
                    for cc in range(4):
                        ACT(cb16.t[:, cc, 0:tw], acc.t[:, cc, t0:t0 + tw], AF.Copy, [acc], [cb16])
                        ACT(sq16.t[:, cc, 0:tw], acc.t[:, cc, t0:t0 + tw], AF.Square, [acc], [sq16])
                    pM = nextps(0, 4)
                    MM(pM, pM.t[:, 0:tw], [(onesb.t[:, :], cb16.t[:, cc, 0:tw]) for cc in range(4)], [onesb, cb16])
                    pQ = nextps(4, 8)
                    MM(pQ, pQ.t[:, 0:tw], [(onesb.t[:, :], sq16.t[:, cc, 0:tw]) for cc in range(4)], [onesb, sq16])
                    TS("dve", mean.t[:, 0:tw], pM.t[:, 0:tw], 1.0 / 512, None, ALU.mult, ALU.bypass, [pM], [mean])
                    TT("dve", rstd.t[:, 0:tw], mean.t[:, 0:tw], mean.t[:, 0:tw], ALU.mult, [mean], [rstd])
                    STT(rstd.t[:, 0:tw], pQ.t[:, 0:tw], 1.0 / 512, rstd.t[:, 0:tw], ALU.mult, ALU.subtract,
                        [pQ, rstd], [rstd])
                    ACT(rstd.t[:, 0:tw], rstd.t[:, 0:tw], AF.Sqrt, [rstd, epst], [rstd], bias=epst.t[:, 0:1])
                    RECIP(rstd.t[:, 0:tw], rstd.t[:, 0:tw], [rstd], [rstd])
                    for cc in range(4):
                        t_ = tmp[cc % 2]
                        o = co[cc % 2]
                        TT("dve", t_.t[:, 0:tw], acc.t[:, cc, t0:t0 + tw], mean.t[:, 0:tw], ALU.subtract,
                           [acc, mean], [t_])
                        TT("pool", t_.t[:, 0:tw], t_.t[:, 0:tw], rstd.t[:, 0:tw], ALU.mult, [t_, rstd], [t_])
                        ACT(o.t[:, 0:tw], t_.t[:, 0:tw], AF.Silu, [t_, smallp], [o],
                            scale=smallp.t[:, SP_LG + cc:SP_LG + cc + 1], bias=smallp.t[:, SP_LB + cc:SP_LB + cc + 1])
                        P.dma("sp", mixT[1024 + cc * 128:1024 + (cc + 1) * 128,
                                         s * SEG + g0 + t0:s * SEG + g0 + t0 + tw], o.t[:, 0:tw], reads=[o])
            P.pop_scope()
        if stop_after in (("B", l), ("C", l)):
            break

        P.push_scope()
        Aall = [P.sbuf("d_A%d" % i, [128, 16, 256], BF16) for i in range(8)]
        Ac = [P.sbuf("d_Ac%d" % i, [128, 2, 256], BF16) for i in range(8)]
        uT = [P.sbuf("d_u%d" % i, [128, LAT], BF16) for i in range(2)]
        uC = [P.sbuf("d_uc%d" % i, [128, CTX], BF16) for i in range(2)]
        csc = P.sbuf("d_csc", [128, 256], BF16)
        tabs = [P.sbuf("d_tab%d" % i, [128, 16, 2, 512], BF16) for i in range(2)]
        tabc = P.sbuf("d_tabc", [128, 2, 2, 256], BF16)
        do = [P.sbuf("d_o%d" % i, [128, 512], BF16) for i in range(3)]
        P.dma("pool", csc.t[:, :], dftc_in[:, :], writes=[csc])
        for cs in range(2):
            P.dma("sp", tabc.t[:, :, cs, :], dft256_in[cs, :, :].rearrange("(c p) n -> p c n", p=128), writes=[tabc])
        dc_ = 0
        for s in range(NSMP):
            for fh in range(4):
                u = uT[(s * 4 + fh) % 2]
                uc = uC[(s * 4 + fh) % 2]
                A = Aall[s * 4 + fh]
                A2 = Ac[s * 4 + fh]
                P.dma("sp", u.t[:, :], fT[fh * 128:(fh + 1) * 128, s * SEG + CTX:(s + 1) * SEG], writes=[u])
                P.dma("sp", uc.t[:, :], fT[fh * 128:(fh + 1) * 128, s * SEG:s * SEG + CTX], writes=[uc])
                for ch in range(16):
                    pb = nextps(0, 4)
                    MM(pb, pb.t[:, 0:256], [(u.t[:, ch * 128:(ch + 1) * 128], csc.t[:, :])], [u, csc])
                    evrr[0] += 1
                    if evrr[0] % 2 == 0:
                        ACT(A.t[:, ch, :], pb.t[:, 0:256], AF.Copy, [pb], [A])
                    else:
                        P.op("dve", lambda e, A=A, pb=pb, ch=ch: e.tensor_copy(A.t[:, ch, :], pb.t[:, 0:256]),
                             reads=[pb], writes=[A])
                for ch in range(2):
                    pb = nextps(0, 4)
                    MM(pb, pb.t[:, 0:256], [(uc.t[:, ch * 128:(ch + 1) * 128], csc.t[:, :])], [uc, csc])
                    ACT(A2.t[:, ch, :], pb.t[:, 0:256], AF.Copy, [pb], [A2])
                pb = nextps(4, 8)
                pairs = []
                for ch in range(2):
                    pairs.append((A2.t[:, ch, 0:128], tabc.t[:, ch, 0, :]))
                    pairs.append((A2.t[:, ch, 128:256], tabc.t[:, ch, 1, :]))
                MM(pb, pb.t[:, 0:256], pairs, [A2, tabc])
                o = do[dc_ % 3]
                dc_ += 1
                ACT(o.t[:, 0:256], pb.t[:, 0:256], AF.Copy, [pb], [o])
                P.dma("sp", mixT[1536 + fh * 128:1536 + (fh + 1) * 128, s * SEG:s * SEG + CTX], o.t[:, 0:256],
                      reads=[o])
        for nt in range(4):
            tb = tabs[nt % 2]
            for cs in range(2):
                P.dma("sp", tb.t[:, :, cs, :],
                      dftn_in[cs, :, nt * 512:(nt + 1) * 512].rearrange("(c p) n -> p c n", p=128), writes=[tb])
            for s in range(NSMP):
                for fh in range(4):
                    A = Aall[s * 4 + fh]
                    pb = nextps(4, 8)
                    pairs = []
                    for ch in range(16):
                        pairs.append((A.t[:, ch, 0:128], tb.t[:, ch, 0, :]))
                        pairs.append((A.t[:, ch, 128:256], tb.t[:, ch, 1, :]))
                    MM(pb, pb.t[:, :], pairs, [A, tb])
                    o = do[dc_ % 3]
                    dc_ += 1
                    ACT(o.t[:, :], pb.t[:, :], AF.Copy, [pb], [o])
                    P.dma("sp", mixT[1536 + fh * 128:1536 + (fh + 1) * 128,
                                     s * SEG + CTX + nt * 512:s * SEG + CTX + (nt + 1) * 512], o.t[:, :], reads=[o])
        P.pop_scope()
        if stop_after == ("D", l):
            break

        P.push_scope()
        wout = P.sbuf("e_wout", [128, 16, D], BF16)
        gtb = [P.sbuf("e_gtb%d" % v, [128, D], F32) for v in range(3)]
        mx = [P.sbuf("e_mx%d" % i, [128, 16, 128], BF16) for i in range(2)]
        xt = [P.sbuf("e_xt%d" % i, [128, D], F32) for i in range(2)]
        tmp = [P.sbuf("e_tmp%d" % i, [128, 512], F32) for i in range(2)]
        for ct in range(4):
            P.dma("pool", wout.t[:, :, ct * 512:(ct + 1) * 512],
                  w_out[l, :, ct * 512:(ct + 1) * 512].rearrange("(c p) n -> p c n", p=128), writes=[wout])
        for v in range(3):
            P.dma("sp", gtb[v].t[:, :], gt_d[0, v, :].partition_broadcast(128), writes=[gtb[v]])
        tc_ = 0
        for b in range(NSMP * 18):
            s, bb = divmod(b, 18)
            v = variant(s, bb < 2)
            m_ = mx[b % 2]
            x_ = xt[b % 2]
            P.dma("sp", m_.t[:, :, :], mixT[:, b * 128:(b + 1) * 128].rearrange("(c p) t -> p c t", p=128),
                  writes=[m_])
            P.dma("sp", x_.t[:, :], xs[b * 128:(b + 1) * 128, :], writes=[x_])
            for dt_ in range(4):
                pb = nextps(0, 8)
                MM(pb, pb.t[:, :], [(m_.t[:, fc, :], wout.t[:, fc, dt_ * 512:(dt_ + 1) * 512]) for fc in range(16)],
                   [m_, wout])
                t_ = tmp[tc_ % 2]
                tc_ += 1
                TT("dve", t_.t[:, :], pb.t[:, :], gtb[v].t[:, dt_ * 512:(dt_ + 1) * 512], ALU.mult, [pb, gtb[v]], [t_])
                TT("pool", x_.t[:, dt_ * 512:(dt_ + 1) * 512], x_.t[:, dt_ * 512:(dt_ + 1) * 512], t_.t[:, :],
                   ALU.add, [x_, t_], [x_])
            P.dma("sp", xs[b * 128:(b + 1) * 128, :], x_.t[:, :], reads=[x_])
        P.pop_scope()
        if stop_after == ("E", l):
            break

        P.push_scope()
        wgu = [P.sbuf("f_wgu%d" % i, [128, 2, 16, 512], BF16) for i in range(2)]
        wd = P.sbuf("f_wd", [128, 8, D], BF16)
        for hf in range(2):
            P.dma("pool", wgu[hf].t[:, 0, :, :],
                  w_gate[l, 0, :, hf * 512:(hf + 1) * 512].rearrange("(c p) n -> p c n", p=128), writes=[wgu[hf]])
            P.dma("pool", wgu[hf].t[:, 1, :, :],
                  w_up[l, 0, :, hf * 512:(hf + 1) * 512].rearrange("(c p) n -> p c n", p=128), writes=[wgu[hf]])
        for hh in range(2):
            P.dma("pool", wd.t[:, hh * 4:(hh + 1) * 4, :],
                  w_down[l, 0, hh * 512:(hh + 1) * 512, :].rearrange("(c p) n -> p c n", p=128), writes=[wd])
        valsTL = P.sbuf("f_valsTL", [128, 2, 32], F32)
        valsTC = P.sbuf("f_valsTC", [32, 32], F32)
        idxTL = P.sbuf("f_idxTL", [128, 2, 32], U32)
        idxTC = P.sbuf("f_idxTC", [32, 32], U32)
        P.push_scope()
        affp = P.sbuf("f_affp", [128, 18, 32], F32)
        affL = P.sbuf("f_affL", [32, LAT], F32)
        affC = P.sbuf("f_affC", [32, CTX], F32)
        valsL = P.sbuf("f_valsL", [32, CAPL], F32)
        valsC = P.sbuf("f_valsC", [32, CAPC], F32)
        idxL = P.sbuf("f_idxL", [32, CAPL], U32)
        idxC = P.sbuf("f_idxC", [32, CAPC], U32)
        idxLf = P.sbuf("f_idxLf", [32, CAPL], F32)
        idxCf = P.sbuf("f_idxCf", [32, CAPC], F32)
        idxTLf = P.sbuf("f_idxTLf", [128, 2, 32], F32)
        idxTCf = P.sbuf("f_idxTCf", [32, 32], F32)
        xt = [P.sbuf("f_xt%d" % i, [128, D], F32) for i in range(2)]
        xnb = [P.sbuf("f_xn%d" % i, [128, D], F32) for i in range(2)]
        junk = P.sbuf("f_junk", [128, D], BF16)
        ssb = P.sbuf("f_ss", [128, 192], F32)
        h2T = [P.sbuf("f_h2T%d" % i, [128, 16, 128], BF16) for i in range(2)]
        wr = P.sbuf("f_wr", [128, 16, NE], BF16)
        sm = P.sbuf("f_sm", [128, 4 * 36], F32)
        ex = [P.sbuf("f_ex%d" % i, [128, NE], F32) for i in range(2)]
        P.dma("pool", wr.t[:, :, :], w_router[l, :, :].rearrange("(c p) e -> p c e", p=128), writes=[wr])
        for s in range(NSMP):
            for bb in range(18):
                b = s * 18 + bb
                x_ = xt[b % 2]
                xn_ = xnb[b % 2]
                h_ = h2T[b % 2]
                norm_block(x_, xn_, junk, ssb, bb, b * 128)
                P.dma("sp", xn_d[b * 128:(b + 1) * 128, :], xn_.t[:, :], reads=[xn_])
                transpose_mod(xn_, 128, h_, 0, a2, 48, variant(s, bb < 2), 0, 4)
                pb = nextps(4, 8)
                MM(pb, pb.t[:, 0:NE], [(h_.t[:, c, :], wr.t[:, c, :]) for c in range(16)], [h_, wr])
                c0 = b * 4
                P.op("dve", lambda e, pb=pb, c0=c0, sm=sm: e.reduce_max(sm.t[:, c0:c0 + 1], pb.t[:, 0:NE], AX.X),
                     reads=[pb], writes=[sm])
                TS("dve", sm.t[:, c0 + 1:c0 + 2], sm.t[:, c0:c0 + 1], -1.0, None, ALU.mult, ALU.bypass, [sm], [sm])
                e_ = ex[b % 2]
                ACT(e_.t[:, :], pb.t[:, 0:NE], AF.Exp, [pb, sm], [e_, sm], bias=sm.t[:, c0 + 1:c0 + 2],
                    accum_out=sm.t[:, c0 + 2:c0 + 3])
                RECIP(sm.t[:, c0 + 3:c0 + 4], sm.t[:, c0 + 2:c0 + 3], [sm], [sm])
                TS("dve", affp.t[:, bb, s * 16:(s + 1) * 16], e_.t[:, :], sm.t[:, c0 + 3:c0 + 4], None, ALU.mult,
                   ALU.bypass, [e_, sm], [affp])
        for bb in range(18):
            pb = nextps(0, 4)
            TR(pb, pb.t[0:32, 0:128], affp.t[:, bb, :], 128, [affp])
            if bb < 2:
                ACT(affC.t[:, bb * 128:(bb + 1) * 128], pb.t[0:32, 0:128], AF.Copy, [pb], [affC])
            else:
                ACT(affL.t[:, (bb - 2) * 128:(bb - 1) * 128], pb.t[0:32, 0:128], AF.Copy, [pb], [affL])
        for (aff, vals, idx, cap) in ((affL, valsL, idxL, CAPL), (affC, valsC, idxC, CAPC)):
            for r in range(cap // 8):
                P.op("dve", lambda e, aff=aff, vals=vals, r=r: e.max(out=vals.t[:, r * 8:(r + 1) * 8], in_=aff.t[:, :]),
                     reads=[aff], writes=[vals])
                P.op("dve", lambda e, aff=aff, vals=vals, idx=idx, r=r: e.max_index(
                    out=idx.t[:, r * 8:(r + 1) * 8], in_max=vals.t[:, r * 8:(r + 1) * 8], in_values=aff.t[:, :]),
                    reads=[aff, vals], writes=[idx])
                P.op("dve", lambda e, aff=aff, vals=vals, r=r: e.match_replace(
                    out=aff.t[:, :], in_to_replace=vals.t[:, r * 8:(r + 1) * 8], in_values=aff.t[:, :],
                    imm_value=-1.0), reads=[aff, vals], writes=[aff])
        for (idx, idxf, col) in ((idxL, idxLf, 0), (idxC, idxCf, 1)):
            P.op("dve", lambda e, idx=idx, idxf=idxf: e.tensor_copy(idxf.t[:, :], idx.t[:, :]), reads=[idx],
                 writes=[idxf])
            TS("dve", idxf.t[:, :], idxf.t[:, :], rowbase.t[:, col:col + 1], None, ALU.add, ALU.bypass,
               [idxf, rowbase], [idxf])
        for blk in range(2):
            pb = nextps(0, 4)
            TR(pb, pb.t[:, 0:32], valsL.t[:, blk * 128:(blk + 1) * 128], 32, [valsL])
            ACT(valsTL.t[:, blk, :], pb.t[:, 0:32], AF.Copy, [pb], [valsTL])
            pb = nextps(0, 4)
            TR(pb, pb.t[:, 0:32], idxLf.t[:, blk * 128:(blk + 1) * 128], 32, [idxLf])
            ACT(idxTLf.t[:, blk, :], pb.t[:, 0:32], AF.Copy, [pb], [idxTLf])
        pb = nextps(0, 4)
        TR(pb, pb.t[0:32, 0:32], valsC.t[:, :], 32, [valsC])
        ACT(valsTC.t[:, :], pb.t[0:32, 0:32], AF.Copy, [pb], [valsTC])
        pb = nextps(0, 4)
        TR(pb, pb.t[0:32, 0:32], idxCf.t[:, :], 32, [idxCf])
        ACT(idxTCf.t[:, :], pb.t[0:32, 0:32], AF.Copy, [pb], [idxTCf])
        P.op("dve", lambda e, a=idxTL, b_=idxTLf: e.tensor_copy(a.t[:, :, :], b_.t[:, :, :]), reads=[idxTLf],
             writes=[idxTL])
        P.op("dve", lambda e, a=idxTC, b_=idxTCf: e.tensor_copy(a.t[:, :], b_.t[:, :]), reads=[idxTCf],
             writes=[idxTC])
        P.pop_scope()

        xg = [P.sbuf("f_xg%d" % i, [128, D], F32) for i in range(4)]
        xsT = P.sbuf("f_xsT", [128, 16, 576], BF16)
        hidT = P.sbuf("f_hidT", [128, 8, 576], BF16)
        sgt = [P.sbuf("f_sgt%d" % i, [128, 288], F32) for i in range(2)]
        yo = [P.sbuf("f_yo%d" % i, [128, D], F32) for i in range(2)]
        gt2b = [P.sbuf("f_gt2b%d" % v, [128, D], F32) for v in range(3)]
        for v in range(3):
            P.dma("sp", gt2b[v].t[:, :], gt_d[1, v, :].partition_broadcast(128), writes=[gt2b[v]])
        xacc = [[Buf("xacc%d%d" % (s, g)) for g in range(2)] for s in range(NSMP)]
        blocks = []
        for s in range(NSMP):
            blocks.append((s, 0, 0, 128, s * 288))
            blocks.append((s, 0, 1, 128, s * 288 + 128))
            blocks.append((s, 1, 0, 32, s * 288 + 256))
        gcs = [0, 0, 0]

        def load_wgu(e_, hf):
            P.dma("pool", wgu[hf].t[:, 0, :, :],
                  w_gate[l, e_, :, hf * 512:(hf + 1) * 512].rearrange("(c p) n -> p c n", p=128), writes=[wgu[hf]])
            P.dma("pool", wgu[hf].t[:, 1, :, :],
                  w_up[l, e_, :, hf * 512:(hf + 1) * 512].rearrange("(c p) n -> p c n", p=128), writes=[wgu[hf]])

        def load_wd(e_):
            for hh in range(2):
                P.dma("pool", wd.t[:, hh * 4:(hh + 1) * 4, :],
                      w_down[l, e_, hh * 512:(hh + 1) * 512, :].rearrange("(c p) n -> p c n", p=128), writes=[wd])

        def gather(e_, bi):
            (s, isc, blk, rows, cb) = blocks[bi]
            g_ = xg[gcs[0] % 4]
            gcs[0] += 1
            col = s * 16 + e_
            iap = idxTC.t[0:32, col:col + 1] if isc else idxTL.t[:, blk, col:col + 1]
            ibuf = idxTC if isc else idxTL
            P.dma_fn("pool", lambda e, g_=g_, rows=rows, iap=iap: e.indirect_dma_start(
                out=g_.t[0:rows, :], out_offset=None, in_=xn_d[:, :],
                in_offset=bass.IndirectOffsetOnAxis(ap=iap, axis=0)), reads=[ibuf], writes=[g_])
            return g_

        def gate_up(hf):
            for fc in range(4):
                for sg_ in range(2):
                    cs_ = slice(sg_ * 288, (sg_ + 1) * 288)
                    pg = nextps(3, 6)
                    MM(pg, pg.t[:, 0:288], [(wgu[hf].t[:, 0, dc, fc * 128:(fc + 1) * 128], xsT.t[:, dc, cs_])
                                            for dc in range(16)], [wgu[hf], xsT])
                    pu = nextps(3, 6)
                    MM(pu, pu.t[:, 0:288], [(wgu[hf].t[:, 1, dc, fc * 128:(fc + 1) * 128], xsT.t[:, dc, cs_])
                                            for dc in range(16)], [wgu[hf], xsT])
                    sg = sgt[gcs[1] % 2]
                    gcs[1] += 1
                    ACT(sg.t[:, :], pg.t[:, 0:288], AF.Silu, [pg], [sg])
                    TT("dve", hidT.t[:, hf * 4 + fc, cs_], sg.t[:, :], pu.t[:, 0:288], ALU.mult, [sg, pu], [hidT])

        pre = {bi: gather(0, bi) for bi in range(4)}
        for ex_ in range(NE):
            for bi, (s, isc, blk, rows, cb) in enumerate(blocks):
                g_ = pre[bi] if bi in pre else gather(ex_, bi)
                transpose_mod(g_, rows, xsT, cb, a2, 48, variant(s, isc), 0, 3)
            pre = {}
            gate_up(0)
            if ex_ + 1 < NE:
                load_wgu(ex_ + 1, 0)
            gate_up(1)
            if ex_ + 1 < NE:
                load_wgu(ex_ + 1, 1)
                pre = {bi: gather(ex_ + 1, bi) for bi in range(4)}
            for (s, isc, blk, rows, cb) in blocks:
                y_ = yo[gcs[2] % 2]
                gcs[2] += 1
                col = s * 16 + ex_
                v = variant(s, isc)
                gap = valsTC.t[0:32, col:col + 1] if isc else valsTL.t[:, blk, col:col + 1]
                gbuf = valsTC if isc else valsTL
                iap = idxTC.t[0:32, col:col + 1] if isc else idxTL.t[:, blk, col:col + 1]
                ibuf = idxTC if isc else idxTL
                for dt_ in range(4):
                    pb = nextps(6, 8)
                    MM(pb, pb.t[0:rows, :], [(hidT.t[:, fc, cb:cb + rows], wd.t[:, fc, dt_ * 512:(dt_ + 1) * 512])
                                             for fc in range(8)], [hidT, wd])
                    STT(y_.t[0:rows, dt_ * 512:(dt_ + 1) * 512], pb.t[0:rows, :], gap,
                        gt2b[v].t[0:rows, dt_ * 512:(dt_ + 1) * 512], ALU.mult, ALU.mult, [pb, gbuf, gt2b[v]], [y_])
                xa = xacc[s][isc]
                P.dma_fn("pool", lambda e, y_=y_, rows=rows, iap=iap: e.indirect_dma_start(
                    out=xs[:, :], out_offset=bass.IndirectOffsetOnAxis(ap=iap, axis=0), in_=y_.t[0:rows, :],
                    in_offset=None, compute_op=ALU.add), reads=[y_, ibuf, xa], writes=[xa])
            if ex_ + 1 < NE:
                load_wd(ex_ + 1)
        P.pop_scope()
        if stop_after == ("F", l):
            break

    if stop_after is None:
        P.push_scope()
        gfin = P.sbuf("z_gfin", [128, D], F32)
        xt = [P.sbuf("z_xt%d" % i, [128, D], F32) for i in range(2)]
        xo = [P.sbuf("z_xo%d" % i, [128, D], F32) for i in range(2)]
        junk = P.sbuf("z_junk", [128, D], BF16)
        ssb = P.sbuf("z_ss", [128, 192], F32)
        P.dma("sp", gfin.t[:, :], gfin_in[:, :], writes=[gfin])
        for s in range(NSMP):
            for bb in range(16):
                i = s * 16 + bb
                x_ = xt[i % 2]
                o_ = xo[i % 2]
                row0 = s * SEG + CTX + bb * 128
                P.dma("sp", x_.t[:, :], xs[row0:row0 + 128, :], writes=[x_])
                col = i % 64
                ACT(junk.t[:, :], x_.t[:, :], AF.Square, [x_], [junk, ssb], accum_out=ssb.t[:, col:col + 1])
                TS("dve", ssb.t[:, col + 64:col + 65], ssb.t[:, col:col + 1], 1.0 / D, EPS, ALU.mult, ALU.add,
                   [ssb], [ssb])
                TT("pool", ssb.t[:, col + 128:col + 129], ssb.t[:, col + 64:col + 65], mhalf_g.t[:, 0:1], ALU.pow,
                   [ssb, mhalf_g], [ssb])
                STT(o_.t[:, :], x_.t[:, :], ssb.t[:, col + 128:col + 129], gfin.t[:, :], ALU.mult, ALU.mult,
                    [x_, ssb, gfin], [o_])
                P.dma("sp", out_d[s, bb * 128:(bb + 1) * 128, :], o_.t[:, :], reads=[o_], final=True)
        P.pop_scope()
    else:
        P.barrier()

    for name in dump:
        ap_, shp, dt_ = scratch[name]
        o = nc.dram_tensor("dump_" + name, list(shp), dt_, kind="ExternalOutput").ap()
        P.dma("sp", o, ap_, final=True)
    P.emit()
    return nc


def _const_tables():
    import ml_dtypes
    bf = ml_dtypes.bfloat16
    ident = np.eye(128, dtype=np.float32)
    rperm = np.zeros((128, 128), np.float32)
    for dest in range(128):
        if dest % 32 < 16:
            rperm[dest + 16, dest] = -1.0
        else:
            rperm[dest - 16, dest] = 1.0
    t = np.arange(LAT)
    pos = np.stack([t // 64, t % 64], axis=-1).astype(np.float32)
    inv_freq = (10000.0 ** (-np.arange(16, dtype=np.float32) / 16)).astype(np.float32)
    dd = np.arange(64)
    ang = (pos[:, dd // 32] * inv_freq[dd % 16][None, :]).astype(np.float32)
    ropeT = np.zeros((128, 2, LAT), np.float32)
    ropeT[:, 0, :] = np.cos(ang).astype(np.float32).T[np.arange(128) % 64]
    ropeT[:, 1, :] = np.sin(ang).astype(np.float32).T[np.arange(128) % 64]

    def dft(nn):
        j = np.arange(nn, dtype=np.int64)
        m = (j[:, None] * j[None, :]) % nn
        a = 2.0 * np.pi * m.astype(np.float64) / nn
        return np.cos(a) / np.sqrt(nn), np.sin(a) / np.sqrt(nn)

    c128, s128 = dft(128)
    dftc = np.concatenate([c128, s128], axis=1).astype(np.float32)
    cn, sn = dft(LAT)
    dftn = np.stack([cn, -sn]).astype(np.float32).astype(bf)
    cc, sc = dft(CTX)
    dft256 = np.stack([cc, -sc]).astype(np.float32).astype(bf)
    rowbase = np.zeros((32, 2), np.float32)
    for s in range(NSMP):
        rowbase[s * 16:(s + 1) * 16, 0] = s * SEG + CTX
        rowbase[s * 16:(s + 1) * 16, 1] = s * SEG
    return dict(ident=ident, rperm=rperm, ropeT=ropeT, dftc=dftc, dftn=dftn, dft256=dft256, rowbase=rowbase)


def _pack_small(g_norm1, g_norm2, conv_w, conv_b, conv_ln_g, conv_ln_b, g_sub, diff_lambda):
    depth = g_norm1.shape[0]
    sp = np.zeros((depth, 128, NSP), np.float32)
    for l in range(depth):
        sp[l, :, SP_G1:SP_G1 + 16] = g_norm1[l].reshape(16, 128).T
        sp[l, :, SP_G2:SP_G2 + 16] = g_norm2[l].reshape(16, 128).T
        cw = conv_w[l].reshape(31, 4, 128)
        sp[l, :, SP_CW:SP_CW + 124] = np.transpose(cw, (2, 1, 0)).reshape(128, 124)
        sp[l, :, SP_CB:SP_CB + 4] = conv_b[l].reshape(4, 128).T
        sp[l, :, SP_LG:SP_LG + 4] = conv_ln_g[l].reshape(4, 128).T
        sp[l, :, SP_LB:SP_LB + 4] = conv_ln_b[l].reshape(4, 128).T
        sp[l, :, SP_GS] = g_sub[l]
        sp[l, :, SP_DL:SP_DL + 256] = diff_lambda[l].reshape(1, 256)
    return sp


def make_in_maps(inputs, n_cores, depth):
    f32 = lambda a: np.ascontiguousarray(np.asarray(a, dtype=np.float32))
    consts = _const_tables()
    sp = _pack_small(*(f32(inputs[k])[:depth] for k in ("g_norm1", "g_norm2", "conv_w", "conv_b", "conv_ln_g",
                                                         "conv_ln_b", "g_sub", "diff_lambda")))
    gfin = np.ascontiguousarray(np.broadcast_to(f32(inputs["g_final"])[None, :], (128, D)))
    shared = dict(consts)
    shared["smallp"] = sp
    shared["gfin"] = gfin
    for k in ("w_ada", "b_ada", "w_in", "w_out", "w_router", "w_gate", "w_up", "w_down"):
        a = inputs[k]
        shared[k] = np.asarray(a)[:depth] if depth != np.asarray(a).shape[0] else np.asarray(a)
    x = np.asarray(inputs["x"])
    ctx = np.asarray(inputs["ctx"])
    c = f32(inputs["c"])
    c_ctx = f32(inputs["c_ctx"])
    maps = []
    for i in range(n_cores):
        m = dict(shared)
        m["x"] = x[NSMP * i:NSMP * (i + 1)]
        m["ctx"] = ctx[NSMP * i:NSMP * (i + 1)]
        cv = np.stack([c[NSMP * i], c[NSMP * i + 1], c_ctx], axis=-1)
        m["cvec"] = np.ascontiguousarray(cv.reshape(16, 128, 3).transpose(1, 0, 2))
        maps.append(m)
    return maps


def kernel(**inputs):
    n_cores = 8
    depth = 4
    nc = build_program(depth=depth)
    maps = make_in_maps(inputs, n_cores, depth)
    res = run_bass_kernel_spmd(nc, maps, core_ids=list(range(n_cores)))
    out = np.concatenate([np.asarray(r["out"]) for r in res.results], axis=0)
    return out.astype(np.float32, copy=False)
```

```python
import numpy as np
import concourse.bass as bass
import concourse.mybir as mybir
from concourse.bass_utils import run_bass_kernel_spmd

F32 = mybir.dt.float32
BF16 = mybir.dt.bfloat16
I32 = mybir.dt.int32
U32 = mybir.dt.uint32
AF = mybir.ActivationFunctionType
ALU = mybir.AluOpType
AX = mybir.AxisListType

SEM_CAP = 30000
N_DMA_SEMS = 24


class Buf:
    __slots__ = ("name", "t", "last_write", "readers")

    def __init__(self, name, t=None):
        self.name = name
        self.t = t
        self.last_write = None
        self.readers = []


class _Op:
    __slots__ = ("waits", "fn", "marked", "dma_sem", "dma_val", "final")

    def __init__(self, waits, fn):
        self.waits = waits
        self.fn = fn
        self.marked = False
        self.dma_sem = None
        self.dma_val = 0
        self.final = False


class Prog:
    ENGS = ("pe", "act", "dve", "pool", "sp")

    def __init__(self, nc):
        self.nc = nc
        self.ops = {e: [] for e in self.ENGS}
        self.known = {e: {} for e in self.ENGS}
        self.ctx = []
        self.dma_sems = []
        self.dma_cnt = [0] * N_DMA_SEMS
        self.dma_last_issuer = [None] * N_DMA_SEMS
        self.dma_rr = 0
        self.finals = []
        self._n = 0
        self.n_psum = 0
        self.scopes = []

    def _enter(self, cm):
        self.ctx.append(cm)
        return cm.__enter__()

    def sbuf(self, name, shape, dtype):
        self._n += 1
        name = "%s_%d" % (name, self._n)
        t = self._enter(self.nc.sbuf_tensor(name, list(shape), dtype))
        return Buf(name, t)

    def psum(self, name, shape=(128, 512), dtype=F32):
        t = self._enter(self.nc.psum_tensor(name, list(shape), dtype))
        return Buf(name, t)

    def dram(self, name, shape, dtype, addr_space="Local"):
        t = self.nc.dram_tensor(name, list(shape), dtype, kind="Internal", addr_space=addr_space).ap()
        return Buf(name, t)

    def _deps(self, eng, reads, writes):
        deps = []
        for b in reads:
            if b.last_write is not None:
                deps.append(b.last_write)
        for b in writes:
            if b.last_write is not None:
                deps.append(b.last_write)
            deps.extend(b.readers)
        waits = []
        kn = self.known[eng]
        for tok in deps:
            kind, key, val = tok
            if kind == "c":
                if key == "pe" and eng == "pe":
                    continue
                if kn.get(key, 0) >= val:
                    continue
                kn[key] = val
                self.ops[key][val - 1].marked = True
                waits.append(tok)
            else:
                k2 = ("d", key)
                if kn.get(k2, 0) >= val:
                    continue
                kn[k2] = val
                waits.append(tok)
        return waits

    def _commit(self, tok, reads, writes):
        for b in writes:
            b.last_write = tok
            b.readers = []
        for b in reads:
            if b in writes:
                continue
            if tok[0] == "c":
                b.readers = [r for r in b.readers if not (r[0] == "c" and r[1] == tok[1])]
            b.readers.append(tok)

    def op(self, eng, fn, reads=(), writes=()):
        waits = self._deps(eng, reads, writes)
        o = _Op(waits, fn)
        self.ops[eng].append(o)
        tok = ("c", eng, len(self.ops[eng]))
        self._commit(tok, reads, writes)
        return tok

    def dma(self, eng, out, in_, reads=(), writes=(), final=False, **kw):
        def fn(e, out=out, in_=in_, kw=kw):
            return e.dma_start(out=out, in_=in_, **kw)
        return self.dma_fn(eng, fn, reads, writes, final)

    def dma_fn(self, eng, fn, reads=(), writes=(), final=False):
        waits = self._deps(eng, reads, writes)
        j = self.dma_rr
        self.dma_rr = (self.dma_rr + 1) % N_DMA_SEMS
        prev = self.dma_cnt[j]
        kn = self.known[eng]
        if prev > 0 and kn.get(("d", j), 0) < prev:
            kn[("d", j)] = prev
            waits.append(("d", j, prev))
        o = _Op(waits, fn)
        o.dma_sem = j
        self.dma_cnt[j] = prev + 16
        o.dma_val = prev + 16
        self.ops[eng].append(o)
        tok = ("d", j, prev + 16)
        self._commit(tok, reads, writes)
        if final:
            self.finals.append(tok)
        return tok

    def barrier(self):
        toks = []
        for e in self.ENGS:
            n = len(self.ops[e])
            while n > 0 and (self.ops[e][n - 1].dma_sem is not None or self.ops[e][n - 1].fn is None):
                n -= 1
            if n > 0:
                toks.append(("c", e, n))
        for j in range(N_DMA_SEMS):
            if self.dma_cnt[j] > 0:
                toks.append(("d", j, self.dma_cnt[j]))
        for e in self.ENGS:
            kn = self.known[e]
            waits = []
            for tok in toks:
                kind, key, val = tok
                if kind == "c":
                    if key == e:
                        continue
                    if kn.get(key, 0) >= val:
                        continue
                    kn[key] = val
                    self.ops[key][val - 1].marked = True
                    waits.append(tok)
                else:
                    k2 = ("d", key)
                    if kn.get(k2, 0) >= val:
                        continue
                    kn[k2] = val
                    waits.append(tok)
            if waits:
                self.ops[e].append(_Op(waits, None))

    def push_scope(self):
        self.scopes.append(len(self.ctx))

    def pop_scope(self):
        self.barrier()
        n = self.scopes.pop()
        while len(self.ctx) > n:
            self.ctx.pop().__exit__(None, None, None)

    def barrier_tokens(self):
        toks = []
        for e in self.ENGS:
            if self.ops[e]:
                toks.append(("c", e, len(self.ops[e])))
        return toks

    def emit(self):
        nc = self.nc
        fw = []
        for tok in self.finals:
            fw.append(tok)
        if fw:
            self.ops["sp"].append(_Op(fw, None))
        rank = {}
        nsem = {}
        for e in self.ENGS:
            r = 0
            for i, o in enumerate(self.ops[e]):
                if o.dma_sem is None and o.marked:
                    r += 1
                    rank[(e, i + 1)] = r
            nsem[e] = (r + SEM_CAP - 1) // SEM_CAP
        csems = {e: [self._enter(nc.semaphore("s_%s_%d" % (e, k))) for k in range(max(1, nsem[e]))]
                 for e in self.ENGS}
        dsems = [self._enter(nc.semaphore("s_dma_%d" % k)) for k in range(N_DMA_SEMS)]
        block = self._enter(nc.Block())

        def run(ename, eng):
            for i, o in enumerate(self.ops[ename]):
                for tok in o.waits:
                    if tok[0] == "c":
                        r = rank[(tok[1], tok[2])] - 1
                        eng.wait_ge(csems[tok[1]][r // SEM_CAP], (r % SEM_CAP) + 1)
                    else:
                        eng.wait_ge(dsems[tok[1]], tok[2])
                if o.fn is None:
                    continue
                ins = o.fn(eng)
                if o.dma_sem is not None:
                    ins.then_inc(dsems[o.dma_sem], 16)
                elif o.marked:
                    r = rank[(ename, i + 1)] - 1
                    ins.then_inc(csems[ename][r // SEM_CAP], 1)

        @block.tensor
        def _(e):
            run("pe", e)

        @block.scalar
        def _(e):
            run("act", e)

        @block.vector
        def _(e):
            run("dve", e)

        @block.gpsimd
        def _(e):
            run("pool", e)

        @block.sync
        def _(e):
            run("sp", e)

        while self.ctx:
            self.ctx.pop().__exit__(None, None, None)


D = 2048
LAT = 2048
CTX = 256
SEG = LAT + CTX
NSMP = 2
NT = NSMP * SEG
H = 8
INW = 4608
NE = 16
FF = 1024
CAPL = 256
CAPC = 32
EPS = 1e-6
NSP = 425
SP_G1, SP_G2, SP_CW, SP_CB, SP_LG, SP_LB, SP_GS, SP_DL = 0, 16, 32, 156, 160, 164, 168, 169


def build_program(depth=4, stop_after=None, dump=()):
    nc = bass.Bass("TRN2", target_bir_lowering=False)
    P = Prog(nc)

    def ein(name, shape, dt=F32):
        return nc.dram_tensor(name, list(shape), dt, kind="ExternalInput").ap()

    x_in = ein("x", [NSMP, LAT, D])
    ctx_in = ein("ctx", [NSMP, CTX, D])
    cvec_in = ein("cvec", [128, 16, 3])
    w_ada = ein("w_ada", [depth, D, 6 * D])
    b_ada = ein("b_ada", [depth, 6 * D])
    w_in = ein("w_in", [depth, D, INW])
    w_out = ein("w_out", [depth, D, D])
    w_router = ein("w_router", [depth, D, NE])
    w_gate = ein("w_gate", [depth, NE, D, FF])
    w_up = ein("w_up", [depth, NE, D, FF])
    w_down = ein("w_down", [depth, NE, FF, D])
    smallp_in = ein("smallp", [depth, 128, NSP])
    gfin_in = ein("gfin", [128, D])
    ident_in = ein("ident", [128, 128])
    rperm_in = ein("rperm", [128, 128])
    rope_in = ein("ropeT", [128, 2, LAT])
    dftc_in = ein("dftc", [128, 256])
    dftn_in = ein("dftn", [2, LAT, LAT], BF16)
    dft256_in = ein("dft256", [2, CTX, CTX], BF16)
    rowbase_in = ein("rowbase", [32, 2])
    out_d = nc.dram_tensor("out", [NSMP, LAT, D], F32, kind="ExternalOutput").ap()

    xs = P.dram("xs", [NT, D], F32).t
    qT = P.dram("qT", [1024, NT], BF16).t
    kT = P.dram("kT", [1024, NT], BF16).t
    vv = P.dram("vv", [NT, 1024], BF16).t
    zT = P.dram("zT", [512, NT], BF16).t
    fT = P.dram("fT", [512, NT], BF16).t
    mixT = P.dram("mixT", [D, NT], BF16).t
    xn_d = P.dram("xn_d", [NT, D], F32).t
    gt_d = P.dram("gt_d", [2, 3, D], F32).t
    scratch = {"xs": (xs, [NT, D], F32), "qT": (qT, [1024, NT], BF16), "kT": (kT, [1024, NT], BF16),
               "vv": (vv, [NT, 1024], BF16), "zT": (zT, [512, NT], BF16), "fT": (fT, [512, NT], BF16),
               "mixT": (mixT, [D, NT], BF16), "xn_d": (xn_d, [NT, D], F32), "gt_d": (gt_d, [2, 3, D], F32)}

    ps = [P.psum("ps%d" % i) for i in range(8)]

    ident = P.sbuf("ident", [128, 128], F32)
    rperm = P.sbuf("rperm", [128, 128], F32)
    onesb = P.sbuf("onesb", [128, 128], BF16)
    onesf = P.sbuf("onesf", [128, 128], F32)
    epst = P.sbuf("epst", [128, 1], F32)
    csil = P.sbuf("csil", [128, 16, 3], F32)
    cT3 = P.sbuf("cT3", [128, 16, 3], BF16)
    modT = P.sbuf("modT", [128, 96, 3], F32)
    smallp = P.sbuf("smallp", [128, NSP], F32)
    a1 = P.sbuf("a1", [128, 16, 3], F32)
    a2 = P.sbuf("a2", [128, 16, 3], F32)
    neglam = P.sbuf("neglam", [128, 1], F32)
    gsubs = P.sbuf("gsubs", [128, 1], F32)
    rowbase = P.sbuf("rowbase", [32, 2], F32)
    mhalf_g = P.sbuf("mhalf_g", [128, 1], F32)

    def MM(psb, out_ap, pairs, reads):
        n = len(pairs)
        for i, (l, r) in enumerate(pairs):
            P.op("pe", lambda e, l=l, r=r, i=i: e.matmul(out_ap, l, r, start=(i == 0), stop=(i == n - 1)),
                 reads=reads, writes=[psb])

    def TR(psb, out_ap, in_ap, rows, reads):
        P.op("pe", lambda e: e.transpose(out_ap, in_ap, ident.t[0:rows, 0:rows]), reads=list(reads) + [ident],
             writes=[psb])

    def ACT(out, in_, func, reads, writes, **kw):
        P.op("act", lambda e: e.activation(out, in_, func, **kw), reads=reads, writes=writes)

    def TS(eng, out, in0, s1, s2, op0, op1, reads, writes):
        P.op(eng, lambda e: e.tensor_scalar(out, in0, s1, s2, op0, op1), reads=reads, writes=writes)

    def TT(eng, out, in0, in1, op, reads, writes):
        P.op(eng, lambda e: e.tensor_tensor(out, in0, in1, op), reads=reads, writes=writes)

    def STT(out, in0, scalar, in1, op0, op1, reads, writes):
        P.op("dve", lambda e: e.scalar_tensor_tensor(out, in0, scalar, in1, op0, op1), reads=reads, writes=writes)

    def RECIP(out, in_, reads, writes):
        P.op("dve", lambda e: e.reciprocal(out, in_), reads=reads, writes=writes)

    def MEMSET(eng, ap, val, writes):
        P.op(eng, lambda e: e.memset(ap, val), reads=[], writes=writes)

    def variant(s, is_ctx):
        return 2 if is_ctx else s

    P.dma("sp", ident.t[:, :], ident_in[:, :], writes=[ident])
    P.dma("sp", rperm.t[:, :], rperm_in[:, :], writes=[rperm])
    P.dma("sp", csil.t[:, :, :], cvec_in[:, :, :], writes=[csil])
    P.dma("sp", rowbase.t[:, :], rowbase_in[:, :], writes=[rowbase])
    for s in range(NSMP):
        P.dma("sp", xs[s * SEG:s * SEG + CTX, :], ctx_in[s, :, :])
        P.dma("sp", xs[s * SEG + CTX:(s + 1) * SEG, :], x_in[s, :, :])
    MEMSET("dve", onesb.t[:, :], 1.0, [onesb])
    MEMSET("dve", onesf.t[:, :], 1.0, [onesf])
    MEMSET("dve", epst.t[:, :], EPS, [epst])
    MEMSET("dve", mhalf_g.t[:, :], -0.5, [mhalf_g])
    ACT(csil.t[:, :, :], csil.t[:, :, :], AF.Silu, [csil], [csil])
    P.op("dve", lambda e: e.tensor_copy(cT3.t[:, :, :], csil.t[:, :, :]), reads=[csil], writes=[cT3])
    P.barrier()

    def norm_block(xt, xnb, junk, ssb, col, row0, dst_store=None):
        P.dma("sp", xt.t[:, :], xs[row0:row0 + 128, :], writes=[xt])
        ACT(junk.t[:, :], xt.t[:, :], AF.Square, [xt], [junk, ssb], accum_out=ssb.t[:, col:col + 1])
        TS("dve", ssb.t[:, col + 64:col + 65], ssb.t[:, col:col + 1], 1.0 / D, EPS, ALU.mult, ALU.add, [ssb], [ssb])
        TT("pool", ssb.t[:, col + 128:col + 129], ssb.t[:, col + 64:col + 65], mhalf_g.t[:, 0:1], ALU.pow,
           [ssb, mhalf_g], [ssb])
        TS("pool", xnb.t[:, :], xt.t[:, :], ssb.t[:, col + 128:col + 129], 1.0, ALU.mult, ALU.mult,
           [xt, ssb], [xnb])

    psrr = [0]

    def nextps(lo=0, hi=8):
        i = lo + (psrr[0] % (hi - lo))
        psrr[0] += 1
        return ps[i]

    evrr = [0]

    def evac_mod(out_ap, in_ap, sc_ap, bi_ap, reads, writes):
        evrr[0] += 1
        if evrr[0] % 2 == 0:
            ACT(out_ap, in_ap, AF.Identity, reads, writes, scale=sc_ap, bias=bi_ap)
        else:
            TS("dve", out_ap, in_ap, sc_ap, bi_ap, ALU.mult, ALU.add, reads, writes)

    def transpose_mod(xnb, rows, dst, dst_col0, amod, shoff, v, lo=0, hi=4):
        for g in range(4):
            pb = nextps(lo, hi)
            for i in range(4):
                c = g * 4 + i
                TR(pb, pb.t[:, i * 128:i * 128 + rows], xnb.t[0:rows, c * 128:(c + 1) * 128], rows, [xnb])
            for i in range(4):
                c = g * 4 + i
                evac_mod(dst.t[:, c, dst_col0:dst_col0 + rows], pb.t[:, i * 128:i * 128 + rows],
                         amod.t[:, c, v:v + 1], modT.t[:, shoff + c, v:v + 1], [pb, amod, modT], [dst])

    TT_TILES = [(0, 256, True), (256, 512, False), (768, 512, False), (1280, 512, False), (1792, 512, False)]

    for l in range(depth):
        lam_init = 0.8 - 0.6 * float(np.exp(-0.3 * l))
        P.push_scope()
        P.dma("sp", smallp.t[:, :], smallp_in[l, :, :], writes=[smallp])
        wsl = [P.sbuf("m_wsl%d" % i, [128, 16, 512], BF16) for i in range(2)]
        bsl = [P.sbuf("m_bsl%d" % i, [1, 512], F32) for i in range(2)]
        gtmp = [P.sbuf("m_gtmp%d" % i, [128, 512], F32) for i in range(2)]
        crep = P.sbuf("m_crep", [128, 16, 3, 128], BF16)
        for j in range(16):
            for v in range(3):
                TS("dve", crep.t[:, j, v, :], onesf.t[:, :], csil.t[:, j, v:v + 1], None, ALU.mult, ALU.bypass,
                   [onesf, csil], [crep])
        gi = 0
        for k in range(24):
            w = wsl[k % 2]
            b = bsl[k % 2]
            P.dma("pool", w.t[:, :, :], w_ada[l, :, k * 512:(k + 1) * 512].rearrange("(c p) n -> p c n", p=128),
                  writes=[w])
            P.dma("sp", b.t[:, :], b_ada[l:l + 1, k * 512:(k + 1) * 512], writes=[b])
            for sub in range(4):
                pb = nextps(0, 4)
                pairs = [(w.t[:, dc, sub * 128:(sub + 1) * 128], cT3.t[:, dc, :]) for dc in range(16)]
                pairs.append((b.t[0:1, sub * 128:(sub + 1) * 128], onesf.t[0:1, 0:3]))
                MM(pb, pb.t[:, 0:3], pairs, [w, b, cT3, onesf])
                ACT(modT.t[:, k * 4 + sub, :], pb.t[:, 0:3], AF.Copy, [pb], [modT])
            if k in (8, 9, 10, 11, 20, 21, 22, 23):
                g = 0 if k < 12 else 1
                ct = k % 4
                for v in range(3):
                    pb = nextps(4, 8)
                    pairs = [(crep.t[:, dc, v, :], w.t[:, dc, :]) for dc in range(16)]
                    pairs.append((onesf.t[0:1, 0:128], b.t[0:1, :]))
                    MM(pb, pb.t[:, :], pairs, [w, b, crep, onesf])
                    gt = gtmp[gi % 2]
                    gi += 1
                    ACT(gt.t[:, :], pb.t[:, :], AF.Copy, [pb], [gt])
                    P.dma("sp", gt_d[g, v:v + 1, ct * 512:(ct + 1) * 512], gt.t[0:1, :], reads=[gt])
        for v in range(3):
            STT(a1.t[:, :, v], modT.t[:, 16:32, v], 1.0, smallp.t[:, SP_G1:SP_G1 + 16], ALU.add, ALU.mult,
                [modT, smallp], [a1])
            STT(a2.t[:, :, v], modT.t[:, 64:80, v], 1.0, smallp.t[:, SP_G2:SP_G2 + 16], ALU.add, ALU.mult,
                [modT, smallp], [a2])
        lt = P.sbuf("m_lt", [128, 2, 64], F32)
        ls = P.sbuf("m_ls", [128, 4], F32)
        dl = smallp.t[:, SP_DL:SP_DL + 256].rearrange("p (a d) -> p a d", a=4)
        TT("dve", lt.t[:, 0, :], dl[:, 0, :], dl[:, 1, :], ALU.mult, [smallp], [lt])
        TT("dve", lt.t[:, 1, :], dl[:, 2, :], dl[:, 3, :], ALU.mult, [smallp], [lt])
        P.op("dve", lambda e, ls=ls, lt=lt: e.reduce_sum(ls.t[:, 0:2], lt.t[:, :, :], AX.X), reads=[lt], writes=[ls])
        ACT(ls.t[:, 2:4], ls.t[:, 0:2], AF.Exp, [ls], [ls])
        TT("dve", neglam.t[:, :], ls.t[:, 3:4], ls.t[:, 2:3], ALU.subtract, [ls], [neglam])
        TS("dve", neglam.t[:, :], neglam.t[:, :], -lam_init, None, ALU.add, ALU.bypass, [neglam], [neglam])
        TS("dve", gsubs.t[:, :], smallp.t[:, SP_GS:SP_GS + 1], 1.0 - lam_init, None, ALU.mult, ALU.bypass,
           [smallp], [gsubs])
        P.pop_scope()
        if stop_after == ("M", l):
            break

        for s in range(NSMP):
            P.push_scope()
            hT = P.sbuf("a_hT", [128, 16, SEG], BF16)
            xt = [P.sbuf("a_xt%d" % i, [128, D], F32) for i in range(2)]
            xnb = [P.sbuf("a_xn%d" % i, [128, D], F32) for i in range(2)]
            junk = P.sbuf("a_junk", [128, D], BF16)
            ssb = P.sbuf("a_ss", [128, 192], F32)
            wsl = [P.sbuf("a_wsl%d" % i, [128, 16, 512], BF16) for i in range(2)]
            rope = P.sbuf("a_rope", [128, 2, LAT], F32)
            qf = [P.sbuf("a_qf%d" % i, [128, 512], F32) for i in range(2)]
            sgb = [P.sbuf("a_sg%d" % i, [128, 512], F32) for i in range(2)]
            ob = [P.sbuf("a_ob%d" % i, [128, 512], BF16) for i in range(3)]
            zf = [P.sbuf("a_zf%d" % i, [128, 512], BF16) for i in range(2)]
            P.dma("sp", rope.t[:, :, :], rope_in[:, :, :], writes=[rope])
            for bb in range(18):
                row0 = s * SEG + bb * 128
                norm_block(xt[bb % 2], xnb[bb % 2], junk, ssb, bb, row0)
                transpose_mod(xnb[bb % 2], 128, hT, bb * 128, a1, 0, variant(s, bb < 2), 0, 4)
            slabs = [("q", 0, 0), ("q", 512, 4), ("k", 1024, 0), ("k", 1536, 4), ("v", 2048, 0), ("v", 2560, 512),
                     ("glu", 0, 0), ("glu", 256, 256), ("f", 4096, 0)]
            cnt = 0
            for si, (kind, c0, aux) in enumerate(slabs):
                w = wsl[si % 2]
                if kind == "glu":
                    P.dma("pool", w.t[:, :, 0:256],
                          w_in[l, :, 3072 + c0:3072 + c0 + 256].rearrange("(c p) n -> p c n", p=128), writes=[w])
                    P.dma("pool", w.t[:, :, 256:512],
                          w_in[l, :, 3584 + c0:3584 + c0 + 256].rearrange("(c p) n -> p c n", p=128), writes=[w])
                else:
                    P.dma("pool", w.t[:, :, :], w_in[l, :, c0:c0 + 512].rearrange("(c p) n -> p c n", p=128),
                          writes=[w])
                if kind in ("q", "k"):
                    dst = qT if kind == "q" else kT
                    for sub in range(4):
                        r0 = (aux + sub) * 128
                        for (t0, tw, isc) in TT_TILES:
                            pb = nextps(0, 5)
                            MM(pb, pb.t[:, 0:tw], [(w.t[:, dc, sub * 128:(sub + 1) * 128], hT.t[:, dc, t0:t0 + tw])
                                                   for dc in range(16)], [w, hT])
                            o = ob[cnt % 3]
                            cnt += 1
                            if isc:
                                ACT(o.t[:, 0:tw], pb.t[:, 0:tw], AF.Copy, [pb], [o])
                            else:
                                q = qf[cnt % 2]
                                sg = sgb[cnt % 2]
                                p0 = t0 - CTX
                                ACT(q.t[:, 0:tw], pb.t[:, 0:tw], AF.Copy, [pb], [q])
                                pr = nextps(5, 8)
                                MM(pr, pr.t[:, 0:tw], [(rperm.t[:, :], q.t[:, 0:tw])], [rperm, q])
                                TT("dve", sg.t[:, 0:tw], pr.t[:, 0:tw], rope.t[:, 1, p0:p0 + tw], ALU.mult,
                                   [pr, rope], [sg])
                                TT("pool", q.t[:, 0:tw], q.t[:, 0:tw], rope.t[:, 0, p0:p0 + tw], ALU.mult,
                                   [q, rope], [q])
                                TT("dve", o.t[:, 0:tw], q.t[:, 0:tw], sg.t[:, 0:tw], ALU.add, [q, sg], [o])
                            P.dma("sp", dst[r0:r0 + 128, s * SEG + t0:s * SEG + t0 + tw], o.t[:, 0:tw], reads=[o])
                elif kind == "v":
                    for tb in range(18):
                        pb = nextps(0, 5)
                        MM(pb, pb.t[:, :], [(hT.t[:, dc, tb * 128:(tb + 1) * 128], w.t[:, dc, :]) for dc in range(16)],
                           [w, hT])
                        o = ob[cnt % 3]
                        cnt += 1
                        ACT(o.t[:, :], pb.t[:, :], AF.Copy, [pb], [o])
                        P.dma("sp", vv[s * SEG + tb * 128:s * SEG + (tb + 1) * 128, aux:aux + 512], o.t[:, :],
                              reads=[o])
                elif kind == "glu":
                    for sub in range(2):
                        ch0 = aux + sub * 128
                        for (t0, tw, isc) in TT_TILES:
                            pa = nextps(0, 5)
                            MM(pa, pa.t[:, 0:tw], [(w.t[:, dc, sub * 128:(sub + 1) * 128], hT.t[:, dc, t0:t0 + tw])
                                                   for dc in range(16)], [w, hT])
                            pg = nextps(5, 8)
                            MM(pg, pg.t[:, 0:tw], [(w.t[:, dc, 256 + sub * 128:256 + (sub + 1) * 128],
                                                    hT.t[:, dc, t0:t0 + tw]) for dc in range(16)], [w, hT])
                            sg = sgb[cnt % 2]
                            z = zf[cnt % 2]
                            cnt += 1
                            ACT(sg.t[:, 0:tw], pg.t[:, 0:tw], AF.Sigmoid, [pg], [sg])
                            TT("dve", z.t[:, 0:tw], pa.t[:, 0:tw], sg.t[:, 0:tw], ALU.mult, [pa, sg], [z])
                            P.dma("sp", zT[ch0:ch0 + 128, s * SEG + t0:s * SEG + t0 + tw], z.t[:, 0:tw], reads=[z])
                else:
                    for sub in range(4):
                        for (t0, tw, isc) in TT_TILES:
                            pb = nextps(0, 5)
                            MM(pb, pb.t[:, 0:tw], [(w.t[:, dc, sub * 128:(sub + 1) * 128], hT.t[:, dc, t0:t0 + tw])
                                                   for dc in range(16)], [w, hT])
                            o = ob[cnt % 3]
                            cnt += 1
                            ACT(o.t[:, 0:tw], pb.t[:, 0:tw], AF.Copy, [pb], [o])
                            P.dma("sp", fT[sub * 128:(sub + 1) * 128, s * SEG + t0:s * SEG + t0 + tw], o.t[:, 0:tw],
                                  reads=[o])
            P.pop_scope()
        if stop_after == ("A", l):
            break

        for s in range(NSMP):
            P.push_scope()
            kh = [P.sbuf("b_kh%d" % i, [128, SEG], BF16) for i in range(2)]
            qz = [[P.sbuf("b_qz%d%d" % (m, i), [128, SEG], BF16) for i in range(2)] for m in range(2)]
            vh = [P.sbuf("b_vh%d" % i, [128, 18, 132], BF16) for i in range(2)]
            Eb = [P.sbuf("b_E%d" % i, [128, 18, 512], BF16) for i in range(2)]
            osb = [P.sbuf("b_o%d" % i, [128, 4, 128], F32) for i in range(2)]
            onb = [P.sbuf("b_on%d" % i, [128, 128], F32) for i in range(4)]
            rr = P.sbuf("b_rr", [128, 64], F32)
            rq = P.sbuf("b_rq", [128, 64], F32)
            mhalf = P.sbuf("b_mhalf", [128, 1], F32)
            junk = P.sbuf("b_junk", [128, 128], F32)
            mo = [P.sbuf("b_mo%d" % i, [128, 512], BF16) for i in range(2)]
            zp = [P.sbuf("c_zp%d" % i, [128, LAT + 30], BF16) for i in range(2)]
            dg = P.sbuf("c_dg", [128, 124, 128], BF16)
            accL = P.sbuf("c_accL", [128, 4, LAT], F32)
            accC = P.sbuf("c_accC", [128, 4, CTX], F32)
            MEMSET("pool", mhalf.t[:, :], -0.5, [mhalf])
            for i in range(2):
                MEMSET("pool", vh[i].t[:, :, 128:132], 1.0, [vh[i]])
                MEMSET("pool", qz[0][i].t[64:128, :], 0.0, [qz[0][i]])
                MEMSET("pool", qz[1][i].t[0:64, :], 0.0, [qz[1][i]])
            rcs = [0, 0]

            def rcols(n, which=0):
                if rcs[which] + n > 64:
                    rcs[which] = 0
                c = rcs[which]
                rcs[which] += n
                return c

            cw = smallp.t[:, SP_CW:SP_CW + 124].rearrange("p (c k) -> p c k", c=4)

            for cc in range(4):
                for k in range(31):
                    TS("dve", dg.t[:, cc * 31 + k, :], ident.t[:, :], cw[:, cc, k:k + 1], None, ALU.mult, ALU.bypass,
                       [ident, smallp], [dg])

            def conv_gen():
                zc = 0
                for (g0, n, acc) in ((0, CTX, accC), (CTX, LAT, accL)):
                    for cc in range(4):
                        z = zp[zc % 2]
                        zc += 1
                        MEMSET("pool", z.t[:, 0:15], 0.0, [z])
                        MEMSET("pool", z.t[:, 15 + n:30 + n], 0.0, [z])
                        P.dma("sp", z.t[:, 15:15 + n], zT[cc * 128:(cc + 1) * 128, s * SEG + g0:s * SEG + g0 + n],
                              writes=[z])
                        for t0 in range(0, n, 512):
                            tw = min(512, n - t0)
                            pb = nextps(6, 8)
                            for k in range(31):
                                P.op("pe", lambda e, pb=pb, z=z, cc=cc, k=k, t0=t0, tw=tw, dg=dg: e.matmul(
                                    pb.t[:, 0:tw], dg.t[:, cc * 31 + k, :], z.t[:, t0 + k:t0 + k + tw],
                                    start=(k == 0), stop=(k == 30)), reads=[dg, z], writes=[pb])
                                yield
                            TS("dve", acc.t[:, cc, t0:t0 + tw], pb.t[:, 0:tw], smallp.t[:, SP_CB + cc:SP_CB + cc + 1],
                               None, ALU.add, ALU.bypass, [pb, smallp], [acc])

            units = [(h, ti, m) for h in range(H) for ti in range(len(TT_TILES)) for m in range(2)]
            loaded = set()

            def load_head(h):
                if h in loaded:
                    return
                loaded.add(h)
                k_, v_ = kh[h % 2], vh[h % 2]
                P.dma("sp", k_.t[:, :], kT[h * 128:(h + 1) * 128, s * SEG:(s + 1) * SEG], writes=[k_])
                for m in range(2):
                    q_ = qz[m][h % 2]
                    P.dma("sp", q_.t[m * 64:(m + 1) * 64, :],
                          qT[h * 128 + m * 64:h * 128 + (m + 1) * 64, s * SEG:(s + 1) * SEG], writes=[q_])
                P.dma("sp", v_.t[:, :, 0:128],
                      vv[s * SEG:(s + 1) * SEG, h * 128:(h + 1) * 128].rearrange("(c p) e -> p c e", p=128),
                      writes=[v_])

            def post_fn(h, ti):
                q0, qw, isc = TT_TILES[ti]
                nj = qw // 128
                qtc = h * len(TT_TILES) + ti
                o_ = osb[qtc % 2]
                m_o = mo[qtc % 2]

                def post_a():
                    for j in range(nj):
                        c0 = rcols(3, 1)
                        on = onb[j]
                        P.op("dve", lambda e, j=j, c0=c0, junk=junk, rq=rq, o_=o_: e.scalar_tensor_tensor(
                            junk.t[:, :], o_.t[:, j, :], 1.0, o_.t[:, j, :], ALU.mult, ALU.mult,
                            accum_out=rq.t[:, c0:c0 + 1]), reads=[o_], writes=[junk, rq])
                        TS("dve", rq.t[:, c0 + 1:c0 + 2], rq.t[:, c0:c0 + 1], 1.0 / 128, EPS, ALU.mult, ALU.add,
                           [rq], [rq])
                        TT("pool", rq.t[:, c0 + 2:c0 + 3], rq.t[:, c0 + 1:c0 + 2], mhalf.t[:, 0:1], ALU.pow,
                           [rq, mhalf], [rq])
                        TS("dve", on.t[:, :], o_.t[:, j, :], rq.t[:, c0 + 2:c0 + 3], None, ALU.mult, ALU.bypass,
                           [o_, rq], [on])

                def post_b():
                    pT = ps[5]
                    for j in range(nj):
                        TR(pT, pT.t[:, j * 128:(j + 1) * 128], onb[j].t[:, :], 128, [onb[j]])
                    TS("dve", m_o.t[:, 0:qw], pT.t[:, 0:qw], gsubs.t[:, 0:1], None, ALU.mult, ALU.bypass,
                       [pT, gsubs], [m_o])
                    P.dma("sp", mixT[h * 128:(h + 1) * 128, s * SEG + q0:s * SEG + q0 + qw], m_o.t[:, 0:qw],
                          reads=[m_o])
                return post_a, post_b

            def pv_gen(u):
                h, ti, m = u
                v_ = vh[h % 2]
                q0, qw, isc = TT_TILES[ti]
                nkc = 2 if isc else 18
                nj = qw // 128
                qtc = h * len(TT_TILES) + ti
                o_ = osb[qtc % 2]
                E = Eb[m]
                for j in range(nj):
                    pO = nextps(3, 5)
                    for kc in range(nkc):
                        P.op("pe", lambda e, pO=pO, E=E, v_=v_, j=j, kc=kc, nkc=nkc: e.matmul(
                            pO.t[:, 0:129], E.t[:, kc, j * 128:(j + 1) * 128], v_.t[:, kc, 0:129],
                            start=(kc == 0), stop=(kc == nkc - 1)), reads=[E, v_], writes=[pO])
                        yield
                    c1 = rcols(2, 0)
                    r1 = rr.t[:, c1:c1 + 1]
                    RECIP(r1, pO.t[:, 128:129], [pO], [rr])
                    if m == 0:
                        TS("dve", o_.t[:, j, :], pO.t[:, 0:128], r1, None, ALU.mult, ALU.bypass, [pO, rr], [o_])
                    else:
                        r2 = rr.t[:, c1 + 1:c1 + 2]
                        TT("dve", r2, r1, neglam.t[:, 0:1], ALU.mult, [rr, neglam], [rr])
                        STT(o_.t[:, j, :], pO.t[:, 0:128], r2, o_.t[:, j, :], ALU.mult, ALU.add,
                            [pO, rr, o_], [o_])

            def block(u_next, u_cur):
                pv = pv_gen(u_cur) if u_cur is not None else None
                npv = 0
                if u_cur is not None:
                    _, qw_c, isc_c = TT_TILES[u_cur[1]]
                    npv = (qw_c // 128) * (2 if isc_c else 18)
                if u_next is not None:
                    h, ti, m = u_next
                    load_head(h)
                    k_, q_ = kh[h % 2], qz[m][h % 2]
                    q0, qw, isc = TT_TILES[ti]
                    nkc = 2 if isc else 18
                    E = Eb[m]
                    per = -(-npv // nkc)
                    for kc in range(nkc):
                        pS = nextps(0, 3)
                        MM(pS, pS.t[:, 0:qw], [(k_.t[:, kc * 128:(kc + 1) * 128], q_.t[:, q0:q0 + qw])], [k_, q_])
                        ACT(E.t[:, kc, 0:qw], pS.t[:, 0:qw], AF.Exp, [pS], [E], scale=0.125)
                        if pv is not None:
                            for _ in range(per):
                                next(pv, None)
                if pv is not None:
                    for _ in pv:
                        pass

            cg = conv_gen()
            pending = None
            block(units[0], None)
            for ui, u in enumerate(units):
                block(units[ui + 1] if ui + 1 < len(units) else None, u)
                if pending is not None:
                    pending()
                    pending = None
                if u[2] == 1:
                    pa_, pending = post_fn(u[0], u[1])
                    pa_()
                for _ in range(8):
                    next(cg, None)
            if pending is not None:
                pending()
            for _ in cg:
                pass

            cb16 = P.sbuf("c_cb", [128, 4, 512], BF16)
            sq16 = P.sbuf("c_sq", [128, 4, 512], BF16)
            mean = P.sbuf("c_mean", [128, 512], F32)
            rstd = P.sbuf("c_rstd", [128, 512], F32)
            tmp = [P.sbuf("c_tmp%d" % i, [128, 512], F32) for i in range(2)]
            co = [P.sbuf("c_o%d" % i, [128, 512], BF16) for i in range(2)]
            for (g0, n, acc) in ((0, CTX, accC), (CTX, LAT, accL)):
                for t0 in range(0, n, 512):
                    tw = min(512, n - t0)
                    for cc in range(4):
                        ACT(cb16.t[:, cc, 0:tw], acc.t[:, cc, t0:t0 + tw], AF.Copy, [acc], [cb16])
                        ACT(sq16.t[:, cc, 0:tw], acc.t[:, cc, t0:t0 + tw], AF.Square, [acc], [sq16])
                    pM = nextps(0, 4)
                    MM(pM, pM.t[:, 0:tw], [(onesb.t[:, :], cb16.t[:, cc, 0:tw]) for cc in range(4)], [onesb, cb16])
                    pQ = nextps(4, 8)
                    MM(pQ, pQ.t[:, 0:tw], [(onesb.t[:, :], sq16.t[:, cc, 0:tw]) for cc in range(4)], [onesb, sq16])
                    TS("dve", mean.t[:, 0:tw], pM.t[:, 0:tw], 1.0 / 512, None, ALU.mult, ALU.bypass, [pM], [mean])
                    TT("dve", rstd.t[:, 0:tw], mean.t[:, 0:tw], mean.t[:, 0:tw], ALU.mult, [mean], [rstd])
                    STT(rstd.t[:, 0:tw], pQ.t[:, 0:tw], 1.0 / 512, rstd.t[:, 0:tw], ALU.mult, ALU.subtract,
                        [pQ, rstd], [rstd])
                    ACT(rstd.t[:, 0:tw], rstd.t[:, 0:tw], AF.Sqrt, [rstd, epst], [rstd], bias=epst.t[:, 0:1])
                    RECIP(rstd.t[:, 0:tw], rstd.t[:, 0:tw], [rstd], [rstd])
                    for cc in range(4):
                        t_ = tmp[cc % 2]
                        o = co[cc % 2]
                        TT("dve", t_.t[:, 0:tw], acc.t[:, cc, t0:t0 + tw], mean.t[:, 0:tw], ALU.subtract,
                           [acc, mean], [t_])
                        TT("pool", t_.t[:, 0:tw], t_.t[:, 0:tw], rstd.t[:, 0:tw], ALU.mult, [t_, rstd], [t_])
                        ACT(o.t[:, 0:tw], t_.t[:, 0:tw], AF.Silu, [t_, smallp], [o],
                            scale=smallp.t[:, SP_LG + cc:SP_LG + cc + 1], bias=smallp.t[:, SP_LB + cc:SP_LB + cc + 1])
                        P.dma("sp", mixT[1024 + cc * 128:1024 + (cc + 1) * 128,
                                         s * SEG + g0 + t0:s * SEG + g0 + t0 + tw], o.t[:, 0:tw], reads=[o])
            P.pop_scope()
        if stop_after in (("B", l), ("C", l)):
            break

        P.push_scope()
        Aall = [P.sbuf("d_A%d" % i, [128, 16, 256], BF16) for i in range(8)]
        Ac = [P.sbuf("d_Ac%d" % i, [128, 2, 256], BF16) for i in range(8)]
        uT = [P.sbuf("d_u%d" % i, [128, LAT], BF16) for i in range(2)]
        uC = [P.sbuf("d_uc%d" % i, [128, CTX], BF16) for i in range(2)]
        csc = P.sbuf("d_csc", [128, 256], BF16)
        tabs = [P.sbuf("d_tab%d" % i, [128, 16, 2, 512], BF16) for i in range(2)]
        tabc = P.sbuf("d_tabc", [128, 2, 2, 256], BF16)
        do = [P.sbuf("d_o%d" % i, [128, 512], BF16) for i in range(3)]
        P.dma("pool", csc.t[:, :], dftc_in[:, :], writes=[csc])
        for cs in range(2):
            P.dma("sp", tabc.t[:, :, cs, :], dft256_in[cs, :, :].rearrange("(c p) n -> p c n", p=128), writes=[tabc])
        dc_ = 0
        for s in range(NSMP):
            for fh in range(4):
                u = uT[(s * 4 + fh) % 2]
                uc = uC[(s * 4 + fh) % 2]
                A = Aall[s * 4 + fh]
                A2 = Ac[s * 4 + fh]
                P.dma("sp", u.t[:, :], fT[fh * 128:(fh + 1) * 128, s * SEG + CTX:(s + 1) * SEG], writes=[u])
                P.dma("sp", uc.t[:, :], fT[fh * 128:(fh + 1) * 128, s * SEG:s * SEG + CTX], writes=[uc])
                for ch in range(16):
                    pb = nextps(0, 4)
                    MM(pb, pb.t[:, 0:256], [(u.t[:, ch * 128:(ch + 1) * 128], csc.t[:, :])], [u, csc])
                    evrr[0] += 1
                    if evrr[0] % 2 == 0:
                        ACT(A.t[:, ch, :], pb.t[:, 0:256], AF.Copy, [pb], [A])
                    else:
                        P.op("dve", lambda e, A=A, pb=pb, ch=ch: e.tensor_copy(A.t[:, ch, :], pb.t[:, 0:256]),
                             reads=[pb], writes=[A])
                for ch in range(2):
                    pb = nextps(0, 4)
                    MM(pb, pb.t[:, 0:256], [(uc.t[:, ch * 128:(ch + 1) * 128], csc.t[:, :])], [uc, csc])
                    ACT(A2.t[:, ch, :], pb.t[:, 0:256], AF.Copy, [pb], [A2])
                pb = nextps(4, 8)
                pairs = []
                for ch in range(2):
                    pairs.append((A2.t[:, ch, 0:128], tabc.t[:, ch, 0, :]))
                    pairs.append((A2.t[:, ch, 128:256], tabc.t[:, ch, 1, :]))
                MM(pb, pb.t[:, 0:256], pairs, [A2, tabc])
                o = do[dc_ % 3]
                dc_ += 1
                ACT(o.t[:, 0:256], pb.t[:, 0:256], AF.Copy, [pb], [o])
                P.dma("sp", mixT[1536 + fh * 128:1536 + (fh + 1) * 128, s * SEG:s * SEG + CTX], o.t[:, 0:256],
                      reads=[o])
        for nt in range(4):
            tb = tabs[nt % 2]
            for cs in range(2):
                P.dma("sp", tb.t[:, :, cs, :],
                      dftn_in[cs, :, nt * 512:(nt + 1) * 512].rearrange("(c p) n -> p c n", p=128), writes=[tb])
            for s in range(NSMP):
                for fh in range(4):
                    A = Aall[s * 4 + fh]
                    pb = nextps(4, 8)
                    pairs = []
                    for ch in range(16):
                        pairs.append((A.t[:, ch, 0:128], tb.t[:, ch, 0, :]))
                        pairs.append((A.t[:, ch, 128:256], tb.t[:, ch, 1, :]))
                    MM(pb, pb.t[:, :], pairs, [A, tb])
                    o = do[dc_ % 3]
                    dc_ += 1
                    ACT(o.t[:, :], pb.t[:, :], AF.Copy, [pb], [o])
                    P.dma("sp", mixT[1536 + fh * 128:1536 + (fh + 1) * 128,
                                     s * SEG + CTX + nt * 512:s * SEG + CTX + (nt + 1) * 512], o.t[:, :], reads=[o])
        P.pop_scope()
        if stop_after == ("D", l):
            break

        P.push_scope()
        wout = P.sbuf("e_wout", [128, 16, D], BF16)
        gtb = [P.sbuf("e_gtb%d" % v, [128, D], F32) for v in range(3)]
        mx = [P.sbuf("e_mx%d" % i, [128, 16, 128], BF16) for i in range(2)]
        xt = [P.sbuf("e_xt%d" % i, [128, D], F32) for i in range(2)]
        tmp = [P.sbuf("e_tmp%d" % i, [128, 512], F32) for i in range(2)]
        for ct in range(4):
            P.dma("pool", wout.t[:, :, ct * 512:(ct + 1) * 512],
                  w_out[l, :, ct * 512:(ct + 1) * 512].rearrange("(c p) n -> p c n", p=128), writes=[wout])
        for v in range(3):
            P.dma("sp", gtb[v].t[:, :], gt_d[0, v, :].partition_broadcast(128), writes=[gtb[v]])
        tc_ = 0
        for b in range(NSMP * 18):
            s, bb = divmod(b, 18)
            v = variant(s, bb < 2)
            m_ = mx[b % 2]
            x_ = xt[b % 2]
            P.dma("sp", m_.t[:, :, :], mixT[:, b * 128:(b + 1) * 128].rearrange("(c p) t -> p c t", p=128),
                  writes=[m_])
            P.dma("sp", x_.t[:, :], xs[b * 128:(b + 1) * 128, :], writes=[x_])
            for dt_ in range(4):
                pb = nextps(0, 8)
                MM(pb, pb.t[:, :], [(m_.t[:, fc, :], wout.t[:, fc, dt_ * 512:(dt_ + 1) * 512]) for fc in range(16)],
                   [m_, wout])
                t_ = tmp[tc_ % 2]
                tc_ += 1
                TT("dve", t_.t[:, :], pb.t[:, :], gtb[v].t[:, dt_ * 512:(dt_ + 1) * 512], ALU.mult, [pb, gtb[v]], [t_])
                TT("pool", x_.t[:, dt_ * 512:(dt_ + 1) * 512], x_.t[:, dt_ * 512:(dt_ + 1) * 512], t_.t[:, :],
                   ALU.add, [x_, t_], [x_])
            P.dma("sp", xs[b * 128:(b + 1) * 128, :], x_.t[:, :], reads=[x_])
        P.pop_scope()
        if stop_after == ("E", l):
            break

        P.push_scope()
        wgu = [P.sbuf("f_wgu%d" % i, [128, 2, 16, 512], BF16) for i in range(2)]
        wd = P.sbuf("f_wd", [128, 8, D], BF16)
        for hf in range(2):
            P.dma("pool", wgu[hf].t[:, 0, :, :],
                  w_gate[l, 0, :, hf * 512:(hf + 1) * 512].rearrange("(c p) n -> p c n", p=128), writes=[wgu[hf]])
            P.dma("pool", wgu[hf].t[:, 1, :, :],
                  w_up[l, 0, :, hf * 512:(hf + 1) * 512].rearrange("(c p) n -> p c n", p=128), writes=[wgu[hf]])
        for hh in range(2):
            P.dma("pool", wd.t[:, hh * 4:(hh + 1) * 4, :],
                  w_down[l, 0, hh * 512:(hh + 1) * 512, :].rearrange("(c p) n -> p c n", p=128), writes=[wd])
        valsTL = P.sbuf("f_valsTL", [128, 2, 32], F32)
        valsTC = P.sbuf("f_valsTC", [32, 32], F32)
        idxTL = P.sbuf("f_idxTL", [128, 2, 32], U32)
        idxTC = P.sbuf("f_idxTC", [32, 32], U32)
        P.push_scope()
        affp = P.sbuf("f_affp", [128, 18, 32], F32)
        affL = P.sbuf("f_affL", [32, LAT], F32)
        affC = P.sbuf("f_affC", [32, CTX], F32)
        valsL = P.sbuf("f_valsL", [32, CAPL], F32)
        valsC = P.sbuf("f_valsC", [32, CAPC], F32)
        idxL = P.sbuf("f_idxL", [32, CAPL], U32)
        idxC = P.sbuf("f_idxC", [32, CAPC], U32)
        idxLf = P.sbuf("f_idxLf", [32, CAPL], F32)
        idxCf = P.sbuf("f_idxCf", [32, CAPC], F32)
        idxTLf = P.sbuf("f_idxTLf", [128, 2, 32], F32)
        idxTCf = P.sbuf("f_idxTCf", [32, 32], F32)
        xt = [P.sbuf("f_xt%d" % i, [128, D], F32) for i in range(2)]
        xnb = [P.sbuf("f_xn%d" % i, [128, D], F32) for i in range(2)]
        junk = P.sbuf("f_junk", [128, D], BF16)
        ssb = P.sbuf("f_ss", [128, 192], F32)
        h2T = [P.sbuf("f_h2T%d" % i, [128, 16, 128], BF16) for i in range(2)]
        wr = P.sbuf("f_wr", [128, 16, NE], BF16)
        sm = P.sbuf("f_sm", [128, 4 * 36], F32)
        ex = [P.sbuf("f_ex%d" % i, [128, NE], F32) for i in range(2)]
        P.dma("pool", wr.t[:, :, :], w_router[l, :, :].rearrange("(c p) e -> p c e", p=128), writes=[wr])
        for s in range(NSMP):
            for bb in range(18):
                b = s * 18 + bb
                x_ = xt[b % 2]
                xn_ = xnb[b % 2]
                h_ = h2T[b % 2]
                norm_block(x_, xn_, junk, ssb, bb, b * 128)
                P.dma("sp", xn_d[b * 128:(b + 1) * 128, :], xn_.t[:, :], reads=[xn_])
                transpose_mod(xn_, 128, h_, 0, a2, 48, variant(s, bb < 2), 0, 4)
                pb = nextps(4, 8)
                MM(pb, pb.t[:, 0:NE], [(h_.t[:, c, :], wr.t[:, c, :]) for c in range(16)], [h_, wr])
                c0 = b * 4
                P.op("dve", lambda e, pb=pb, c0=c0, sm=sm: e.reduce_max(sm.t[:, c0:c0 + 1], pb.t[:, 0:NE], AX.X),
                     reads=[pb], writes=[sm])
                TS("dve", sm.t[:, c0 + 1:c0 + 2], sm.t[:, c0:c0 + 1], -1.0, None, ALU.mult, ALU.bypass, [sm], [sm])
                e_ = ex[b % 2]
                ACT(e_.t[:, :], pb.t[:, 0:NE], AF.Exp, [pb, sm], [e_, sm], bias=sm.t[:, c0 + 1:c0 + 2],
                    accum_out=sm.t[:, c0 + 2:c0 + 3])
                RECIP(sm.t[:, c0 + 3:c0 + 4], sm.t[:, c0 + 2:c0 + 3], [sm], [sm])
                TS("dve", affp.t[:, bb, s * 16:(s + 1) * 16], e_.t[:, :], sm.t[:, c0 + 3:c0 + 4], None, ALU.mult,
                   ALU.bypass, [e_, sm], [affp])
        for bb in range(18):
            pb = nextps(0, 4)
            TR(pb, pb.t[0:32, 0:128], affp.t[:, bb, :], 128, [affp])
            if bb < 2:
                ACT(affC.t[:, bb * 128:(bb + 1) * 128], pb.t[0:32, 0:128], AF.Copy, [pb], [affC])
            else:
                ACT(affL.t[:, (bb - 2) * 128:(bb - 1) * 128], pb.t[0:32, 0:128], AF.Copy, [pb], [affL])
        for (aff, vals, idx, cap) in ((affL, valsL, idxL, CAPL), (affC, valsC, idxC, CAPC)):
            for r in range(cap // 8):
                P.op("dve", lambda e, aff=aff, vals=vals, r=r: e.max(out=vals.t[:, r * 8:(r + 1) * 8], in_=aff.t[:, :]),
                     reads=[aff], writes=[vals])
                P.op("dve", lambda e, aff=aff, vals=vals, idx=idx, r=r: e.max_index(
                    out=idx.t[:, r * 8:(r + 1) * 8], in_max=vals.t[:, r * 8:(r + 1) * 8], in_values=aff.t[:, :]),
                    reads=[aff, vals], writes=[idx])
                P.op("dve", lambda e, aff=aff, vals=vals, r=r: e.match_replace(
                    out=aff.t[:, :], in_to_replace=vals.t[:, r * 8:(r + 1) * 8], in_values=aff.t[:, :],
                    imm_value=-1.0), reads=[aff, vals], writes=[aff])
        for (idx, idxf, col) in ((idxL, idxLf, 0), (idxC, idxCf, 1)):
            P.op("dve", lambda e, idx=idx, idxf=idxf: e.tensor_copy(idxf.t[:, :], idx.t[:, :]), reads=[idx],
                 writes=[idxf])
            TS("dve", idxf.t[:, :], idxf.t[:, :], rowbase.t[:, col:col + 1], None, ALU.add, ALU.bypass,
               [idxf, rowbase], [idxf])
        for blk in range(2):
            pb = nextps(0, 4)
            TR(pb, pb.t[:, 0:32], valsL.t[:, blk * 128:(blk + 1) * 128], 32, [valsL])
            ACT(valsTL.t[:, blk, :], pb.t[:, 0:32], AF.Copy, [pb], [valsTL])
            pb = nextps(0, 4)
            TR(pb, pb.t[:, 0:32], idxLf.t[:, blk * 128:(blk + 1) * 128], 32, [idxLf])
            ACT(idxTLf.t[:, blk, :], pb.t[:, 0:32], AF.Copy, [pb], [idxTLf])
        pb = nextps(0, 4)
        TR(pb, pb.t[0:32, 0:32], valsC.t[:, :], 32, [valsC])
        ACT(valsTC.t[:, :], pb.t[0:32, 0:32], AF.Copy, [pb], [valsTC])
        pb = nextps(0, 4)
        TR(pb, pb.t[0:32, 0:32], idxCf.t[:, :], 32, [idxCf])
        ACT(idxTCf.t[:, :], pb.t[0:32, 0:32], AF.Copy, [pb], [idxTCf])
        P.op("dve", lambda e, a=idxTL, b_=idxTLf: e.tensor_copy(a.t[:, :, :], b_.t[:, :, :]), reads=[idxTLf],
             writes=[idxTL])
        P.op("dve", lambda e, a=idxTC, b_=idxTCf: e.tensor_copy(a.t[:, :], b_.t[:, :]), reads=[idxTCf],
             writes=[idxTC])
        P.pop_scope()

        xg = [P.sbuf("f_xg%d" % i, [128, D], F32) for i in range(4)]
        xsT = P.sbuf("f_xsT", [128, 16, 576], BF16)
        hidT = P.sbuf("f_hidT", [128, 8, 576], BF16)
        sgt = [P.sbuf("f_sgt%d" % i, [128, 288], F32) for i in range(2)]
        yo = [P.sbuf("f_yo%d" % i, [128, D], F32) for i in range(2)]
        gt2b = [P.sbuf("f_gt2b%d" % v, [128, D], F32) for v in range(3)]
        for v in range(3):
            P.dma("sp", gt2b[v].t[:, :], gt_d[1, v, :].partition_broadcast(128), writes=[gt2b[v]])
        xacc = [[Buf("xacc%d%d" % (s, g)) for g in range(2)] for s in range(NSMP)]
        blocks = []
        for s in range(NSMP):
            blocks.append((s, 0, 0, 128, s * 288))
            blocks.append((s, 0, 1, 128, s * 288 + 128))
            blocks.append((s, 1, 0, 32, s * 288 + 256))
        gcs = [0, 0, 0]

        def load_wgu(e_, hf):
            P.dma("pool", wgu[hf].t[:, 0, :, :],
                  w_gate[l, e_, :, hf * 512:(hf + 1) * 512].rearrange("(c p) n -> p c n", p=128), writes=[wgu[hf]])
            P.dma("pool", wgu[hf].t[:, 1, :, :],
                  w_up[l, e_, :, hf * 512:(hf + 1) * 512].rearrange("(c p) n -> p c n", p=128), writes=[wgu[hf]])

        def load_wd(e_):
            for hh in range(2):
                P.dma("pool", wd.t[:, hh * 4:(hh + 1) * 4, :],
                      w_down[l, e_, hh * 512:(hh + 1) * 512, :].rearrange("(c p) n -> p c n", p=128), writes=[wd])

        def gather(e_, bi):
            (s, isc, blk, rows, cb) = blocks[bi]
            g_ = xg[gcs[0] % 4]
            gcs[0] += 1
            col = s * 16 + e_
            iap = idxTC.t[0:32, col:col + 1] if isc else idxTL.t[:, blk, col:col + 1]
            ibuf = idxTC if isc else idxTL
            P.dma_fn("pool", lambda e, g_=g_, rows=rows, iap=iap: e.indirect_dma_start(
                out=g_.t[0:rows, :], out_offset=None, in_=xn_d[:, :],
                in_offset=bass.IndirectOffsetOnAxis(ap=iap, axis=0)), reads=[ibuf], writes=[g_])
            return g_

        def gate_up(hf):
            for fc in range(4):
                for sg_ in range(2):
                    cs_ = slice(sg_ * 288, (sg_ + 1) * 288)
                    pg = nextps(3, 6)
                    MM(pg, pg.t[:, 0:288], [(wgu[hf].t[:, 0, dc, fc * 128:(fc + 1) * 128], xsT.t[:, dc, cs_])
                                            for dc in range(16)], [wgu[hf], xsT])
                    pu = nextps(3, 6)
                    MM(pu, pu.t[:, 0:288], [(wgu[hf].t[:, 1, dc, fc * 128:(fc + 1) * 128], xsT.t[:, dc, cs_])
                                            for dc in range(16)], [wgu[hf], xsT])
                    sg = sgt[gcs[1] % 2]
                    gcs[1] += 1
                    ACT(sg.t[:, :], pg.t[:, 0:288], AF.Silu, [pg], [sg])
                    TT("dve", hidT.t[:, hf * 4 + fc, cs_], sg.t[:, :], pu.t[:, 0:288], ALU.mult, [sg, pu], [hidT])

        pre = {bi: gather(0, bi) for bi in range(4)}
        for ex_ in range(NE):
            for bi, (s, isc, blk, rows, cb) in enumerate(blocks):
                g_ = pre[bi] if bi in pre else gather(ex_, bi)
                transpose_mod(g_, rows, xsT, cb, a2, 48, variant(s, isc), 0, 3)
            pre = {}
            gate_up(0)
            if ex_ + 1 < NE:
                load_wgu(ex_ + 1, 0)
            gate_up(1)
            if ex_ + 1 < NE:
                load_wgu(ex_ + 1, 1)
                pre = {bi: gather(ex_ + 1, bi) for bi in range(4)}
            for (s, isc, blk, rows, cb) in blocks:
                y_ = yo[gcs[2] % 2]
                gcs[2] += 1
                col = s * 16 + ex_
                v = variant(s, isc)
                gap = valsTC.t[0:32, col:col + 1] if isc else valsTL.t[:, blk, col:col + 1]
                gbuf = valsTC if isc else valsTL
                iap = idxTC.t[0:32, col:col + 1] if isc else idxTL.t[:, blk, col:col + 1]
                ibuf = idxTC if isc else idxTL
                for dt_ in range(4):
                    pb = nextps(6, 8)
                    MM(pb, pb.t[0:rows, :], [(hidT.t[:, fc, cb:cb + rows], wd.t[:, fc, dt_ * 512:(dt_ + 1) * 512])
                                             for fc in range(8)], [hidT, wd])
                    STT(y_.t[0:rows, dt_ * 512:(dt_ + 1) * 512], pb.t[0:rows, :], gap,
                        gt2b[v].t[0:rows, dt_ * 512:(dt_ + 1) * 512], ALU.mult, ALU.mult, [pb, gbuf, gt2b[v]], [y_])
                xa = xacc[s][isc]
                P.dma_fn("pool", lambda e, y_=y_, rows=rows, iap=iap: e.indirect_dma_start(
                    out=xs[:, :], out_offset=bass.IndirectOffsetOnAxis(ap=iap, axis=0), in_=y_.t[0:rows, :],
                    in_offset=None, compute_op=ALU.add), reads=[y_, ibuf, xa], writes=[xa])
            if ex_ + 1 < NE:
                load_wd(ex_ + 1)
        P.pop_scope()
        if stop_after == ("F", l):
            break

    if stop_after is None:
        P.push_scope()
        gfin = P.sbuf("z_gfin", [128, D], F32)
        xt = [P.sbuf("z_xt%d" % i, [128, D], F32) for i in range(2)]
        xo = [P.sbuf("z_xo%d" % i, [128, D], F32) for i in range(2)]
        junk = P.sbuf("z_junk", [128, D], BF16)
        ssb = P.sbuf("z_ss", [128, 192], F32)
        P.dma("sp", gfin.t[:, :], gfin_in[:, :], writes=[gfin])
        for s in range(NSMP):
            for bb in range(16):
                i = s * 16 + bb
                x_ = xt[i % 2]
                o_ = xo[i % 2]
                row0 = s * SEG + CTX + bb * 128
                P.dma("sp", x_.t[:, :], xs[row0:row0 + 128, :], writes=[x_])
                col = i % 64
                ACT(junk.t[:, :], x_.t[:, :], AF.Square, [x_], [junk, ssb], accum_out=ssb.t[:, col:col + 1])
                TS("dve", ssb.t[:, col + 64:col + 65], ssb.t[:, col:col + 1], 1.0 / D, EPS, ALU.mult, ALU.add,
                   [ssb], [ssb])
                TT("pool", ssb.t[:, col + 128:col + 129], ssb.t[:, col + 64:col + 65], mhalf_g.t[:, 0:1], ALU.pow,
                   [ssb, mhalf_g], [ssb])
                STT(o_.t[:, :], x_.t[:, :], ssb.t[:, col + 128:col + 129], gfin.t[:, :], ALU.mult, ALU.mult,
                    [x_, ssb, gfin], [o_])
                P.dma("sp", out_d[s, bb * 128:(bb + 1) * 128, :], o_.t[:, :], reads=[o_], final=True)
        P.pop_scope()
    else:
        P.barrier()

    for name in dump:
        ap_, shp, dt_ = scratch[name]
        o = nc.dram_tensor("dump_" + name, list(shp), dt_, kind="ExternalOutput").ap()
        P.dma("sp", o, ap_, final=True)
    P.emit()
    return nc


def _const_tables():
    import ml_dtypes
    bf = ml_dtypes.bfloat16
    ident = np.eye(128, dtype=np.float32)
    rperm = np.zeros((128, 128), np.float32)
    for dest in range(128):
        if dest % 32 < 16:
            rperm[dest + 16, dest] = -1.0
        else:
            rperm[dest - 16, dest] = 1.0
    t = np.arange(LAT)
    pos = np.stack([t // 64, t % 64], axis=-1).astype(np.float32)
    inv_freq = (10000.0 ** (-np.arange(16, dtype=np.float32) / 16)).astype(np.float32)
    dd = np.arange(64)
    ang = (pos[:, dd // 32] * inv_freq[dd % 16][None, :]).astype(np.float32)
    ropeT = np.zeros((128, 2, LAT), np.float32)
    ropeT[:, 0, :] = np.cos(ang).astype(np.float32).T[np.arange(128) % 64]
    ropeT[:, 1, :] = np.sin(ang).astype(np.float32).T[np.arange(128) % 64]

    def dft(nn):
        j = np.arange(nn, dtype=np.int64)
        m = (j[:, None] * j[None, :]) % nn
        a = 2.0 * np.pi * m.astype(np.float64) / nn
        return np.cos(a) / np.sqrt(nn), np.sin(a) / np.sqrt(nn)

    c128, s128 = dft(128)
    dftc = np.concatenate([c128, s128], axis=1).astype(np.float32)
    cn, sn = dft(LAT)
    dftn = np.stack([cn, -sn]).astype(np.float32).astype(bf)
    cc, sc = dft(CTX)
    dft256 = np.stack([cc, -sc]).astype(np.float32).astype(bf)
    rowbase = np.zeros((32, 2), np.float32)
    for s in range(NSMP):
        rowbase[s * 16:(s + 1) * 16, 0] = s * SEG + CTX
        rowbase[s * 16:(s + 1) * 16, 1] = s * SEG
    return dict(ident=ident, rperm=rperm, ropeT=ropeT, dftc=dftc, dftn=dftn, dft256=dft256, rowbase=rowbase)


def _pack_small(g_norm1, g_norm2, conv_w, conv_b, conv_ln_g, conv_ln_b, g_sub, diff_lambda):
    depth = g_norm1.shape[0]
    sp = np.zeros((depth, 128, NSP), np.float32)
    for l in range(depth):
        sp[l, :, SP_G1:SP_G1 + 16] = g_norm1[l].reshape(16, 128).T
        sp[l, :, SP_G2:SP_G2 + 16] = g_norm2[l].reshape(16, 128).T
        cw = conv_w[l].reshape(31, 4, 128)
        sp[l, :, SP_CW:SP_CW + 124] = np.transpose(cw, (2, 1, 0)).reshape(128, 124)
        sp[l, :, SP_CB:SP_CB + 4] = conv_b[l].reshape(4, 128).T
        sp[l, :, SP_LG:SP_LG + 4] = conv_ln_g[l].reshape(4, 128).T
        sp[l, :, SP_LB:SP_LB + 4] = conv_ln_b[l].reshape(4, 128).T
        sp[l, :, SP_GS] = g_sub[l]
        sp[l, :, SP_DL:SP_DL + 256] = diff_lambda[l].reshape(1, 256)
    return sp


def make_in_maps(inputs, n_cores, depth):
    f32 = lambda a: np.ascontiguousarray(np.asarray(a, dtype=np.float32))
    consts = _const_tables()
    sp = _pack_small(*(f32(inputs[k])[:depth] for k in ("g_norm1", "g_norm2", "conv_w", "conv_b", "conv_ln_g",
                                                         "conv_ln_b", "g_sub", "diff_lambda")))
    gfin = np.ascontiguousarray(np.broadcast_to(f32(inputs["g_final"])[None, :], (128, D)))
    shared = dict(consts)
    shared["smallp"] = sp
    shared["gfin"] = gfin
    for k in ("w_ada", "b_ada", "w_in", "w_out", "w_router", "w_gate", "w_up", "w_down"):
        a = inputs[k]
        shared[k] = np.asarray(a)[:depth] if depth != np.asarray(a).shape[0] else np.asarray(a)
    x = np.asarray(inputs["x"])
    ctx = np.asarray(inputs["ctx"])
    c = f32(inputs["c"])
    c_ctx = f32(inputs["c_ctx"])
    maps = []
    for i in range(n_cores):
        m = dict(shared)
        m["x"] = x[NSMP * i:NSMP * (i + 1)]
        m["ctx"] = ctx[NSMP * i:NSMP * (i + 1)]
        cv = np.stack([c[NSMP * i], c[NSMP * i + 1], c_ctx], axis=-1)
        m["cvec"] = np.ascontiguousarray(cv.reshape(16, 128, 3).transpose(1, 0, 2))
        maps.append(m)
    return maps


def kernel(**inputs):
    n_cores = 8
    depth = 4
    nc = build_program(depth=depth)
    maps = make_in_maps(inputs, n_cores, depth)
    res = run_bass_kernel_spmd(nc, maps, core_ids=list(range(n_cores)))
    out = np.concatenate([np.asarray(r["out"]) for r in res.results], axis=0)
    return out.astype(np.float32, copy=False)
```

```python
import numpy as np
import concourse.bass as bass
import concourse.mybir as mybir
from concourse.bass_utils import run_bass_kernel_spmd

F32 = mybir.dt.float32
BF16 = mybir.dt.bfloat16
I32 = mybir.dt.int32
U32 = mybir.dt.uint32
AF = mybir.ActivationFunctionType
ALU = mybir.AluOpType
AX = mybir.AxisListType

SEM_CAP = 30000
N_DMA_SEMS = 24


class Buf:
    __slots__ = ("name", "t", "last_write", "readers", "nowaw", "writers", "war")

    def __init__(self, name, t=None):
        self.name = name
        self.t = t
        self.last_write = None
        self.readers = []
        self.nowaw = False
        self.writers = {}
        self.war = []


class _Op:
    __slots__ = ("waits", "fn", "marked", "dma_sem", "dma_val", "final")

    def __init__(self, waits, fn):
        self.waits = waits
        self.fn = fn
        self.marked = False
        self.dma_sem = None
        self.dma_val = 0
        self.final = False


class Prog:
    ENGS = ("pe", "act", "dve", "pool", "sp")

    def __init__(self, nc):
        self.nc = nc
        self.ops = {e: [] for e in self.ENGS}
        self.known = {e: {} for e in self.ENGS}
        self.ctx = []
        self.dma_sems = []
        self.dma_cnt = [0] * N_DMA_SEMS
        self.dma_last_issuer = [None] * N_DMA_SEMS
        self.dma_rr = 0
        self.finals = []
        self._n = 0
        self.n_psum = 0
        self.scopes = []

    def _enter(self, cm):
        self.ctx.append(cm)
        return cm.__enter__()

    def sbuf(self, name, shape, dtype, nowaw=False):
        self._n += 1
        name = "%s_%d" % (name, self._n)
        t = self._enter(self.nc.sbuf_tensor(name, list(shape), dtype))
        b = Buf(name, t)
        b.nowaw = nowaw
        return b

    def psum(self, name, shape=(128, 512), dtype=F32):
        t = self._enter(self.nc.psum_tensor(name, list(shape), dtype))
        return Buf(name, t)

    def dram(self, name, shape, dtype, addr_space="Local"):
        t = self.nc.dram_tensor(name, list(shape), dtype, kind="Internal", addr_space=addr_space).ap()
        return Buf(name, t)

    def _deps(self, eng, reads, writes):
        deps = []
        for b in reads:
            if b.nowaw:
                deps.extend(b.writers.values())
            elif b.last_write is not None:
                deps.append(b.last_write)
        for b in writes:
            if b.nowaw:
                if b.readers:
                    b.war = list(b.readers)
                    b.readers = []
                deps.extend(b.war)
                if b in reads:
                    deps.extend(b.writers.values())
                continue
            if b.last_write is not None:
                deps.append(b.last_write)
            deps.extend(b.readers)
        waits = []
        kn = self.known[eng]
        for tok in deps:
            kind, key, val = tok
            if kind == "c":
                if key == "pe" and eng == "pe":
                    continue
                if kn.get(key, 0) >= val:
                    continue
                kn[key] = val
                self.ops[key][val - 1].marked = True
                waits.append(tok)
            else:
                k2 = ("d", key)
                if kn.get(k2, 0) >= val:
                    continue
                kn[k2] = val
                waits.append(tok)
        return waits

    def _commit(self, tok, reads, writes):
        for b in writes:
            b.last_write = tok
            if b.nowaw:
                b.writers[tok[1] if tok[0] == "c" else ("d", tok[1])] = tok
            else:
                b.readers = []
        for b in reads:
            if b in writes:
                continue
            if tok[0] == "c":
                b.readers = [r for r in b.readers if not (r[0] == "c" and r[1] == tok[1])]
            b.readers.append(tok)

    def op(self, eng, fn, reads=(), writes=()):
        waits = self._deps(eng, reads, writes)
        o = _Op(waits, fn)
        self.ops[eng].append(o)
        tok = ("c", eng, len(self.ops[eng]))
        self._commit(tok, reads, writes)
        return tok

    def dma(self, eng, out, in_, reads=(), writes=(), final=False, **kw):
        def fn(e, out=out, in_=in_, kw=kw):
            return e.dma_start(out=out, in_=in_, **kw)
        return self.dma_fn(eng, fn, reads, writes, final)

    def dma_fn(self, eng, fn, reads=(), writes=(), final=False):
        waits = self._deps(eng, reads, writes)
        j = self.dma_rr
        self.dma_rr = (self.dma_rr + 1) % N_DMA_SEMS
        prev = self.dma_cnt[j]
        kn = self.known[eng]
        if prev > 0 and kn.get(("d", j), 0) < prev:
            kn[("d", j)] = prev
            waits.append(("d", j, prev))
        o = _Op(waits, fn)
        o.dma_sem = j
        self.dma_cnt[j] = prev + 16
        o.dma_val = prev + 16
        self.ops[eng].append(o)
        tok = ("d", j, prev + 16)
        self._commit(tok, reads, writes)
        if final:
            self.finals.append(tok)
        return tok

    def barrier(self):
        toks = []
        for e in self.ENGS:
            n = len(self.ops[e])
            while n > 0 and (self.ops[e][n - 1].dma_sem is not None or self.ops[e][n - 1].fn is None):
                n -= 1
            if n > 0:
                toks.append(("c", e, n))
        for j in range(N_DMA_SEMS):
            if self.dma_cnt[j] > 0:
                toks.append(("d", j, self.dma_cnt[j]))
        for e in self.ENGS:
            kn = self.known[e]
            waits = []
            for tok in toks:
                kind, key, val = tok
                if kind == "c":
                    if key == e:
                        continue
                    if kn.get(key, 0) >= val:
                        continue
                    kn[key] = val
                    self.ops[key][val - 1].marked = True
                    waits.append(tok)
                else:
                    k2 = ("d", key)
                    if kn.get(k2, 0) >= val:
                        continue
                    kn[k2] = val
                    waits.append(tok)
            if waits:
                self.ops[e].append(_Op(waits, None))

    def push_scope(self):
        self.scopes.append(len(self.ctx))

    def pop_scope(self):
        self.barrier()
        n = self.scopes.pop()
        while len(self.ctx) > n:
            self.ctx.pop().__exit__(None, None, None)

    def barrier_tokens(self):
        toks = []
        for e in self.ENGS:
            if self.ops[e]:
                toks.append(("c", e, len(self.ops[e])))
        return toks

    def emit(self):
        nc = self.nc
        fw = []
        for tok in self.finals:
            fw.append(tok)
        if fw:
            self.ops["sp"].append(_Op(fw, None))
        rank = {}
        nsem = {}
        for e in self.ENGS:
            r = 0
            for i, o in enumerate(self.ops[e]):
                if o.dma_sem is None and o.marked:
                    r += 1
                    rank[(e, i + 1)] = r
            nsem[e] = (r + SEM_CAP - 1) // SEM_CAP
        csems = {e: [self._enter(nc.semaphore("s_%s_%d" % (e, k))) for k in range(max(1, nsem[e]))]
                 for e in self.ENGS}
        dsems = [self._enter(nc.semaphore("s_dma_%d" % k)) for k in range(N_DMA_SEMS)]
        block = self._enter(nc.Block())

        def run(ename, eng):
            for i, o in enumerate(self.ops[ename]):
                for tok in o.waits:
                    if tok[0] == "c":
                        r = rank[(tok[1], tok[2])] - 1
                        eng.wait_ge(csems[tok[1]][r // SEM_CAP], (r % SEM_CAP) + 1)
                    else:
                        eng.wait_ge(dsems[tok[1]], tok[2])
                if o.fn is None:
                    continue
                ins = o.fn(eng)
                if o.dma_sem is not None:
                    ins.then_inc(dsems[o.dma_sem], 16)
                elif o.marked:
                    r = rank[(ename, i + 1)] - 1
                    ins.then_inc(csems[ename][r // SEM_CAP], 1)

        @block.tensor
        def _(e):
            run("pe", e)

        @block.scalar
        def _(e):
            run("act", e)

        @block.vector
        def _(e):
            run("dve", e)

        @block.gpsimd
        def _(e):
            run("pool", e)

        @block.sync
        def _(e):
            run("sp", e)

        while self.ctx:
            self.ctx.pop().__exit__(None, None, None)


D = 2048
LAT = 2048
CTX = 256
SEG = LAT + CTX
NSMP = 2
NT = NSMP * SEG
H = 8
INW = 4608
NE = 16
FF = 1024
CAPL = 256
CAPC = 32
EPS = 1e-6
NSP = 425
SP_G1, SP_G2, SP_CW, SP_CB, SP_LG, SP_LB, SP_GS, SP_DL = 0, 16, 32, 156, 160, 164, 168, 169


def build_program(depth=4, stop_after=None, dump=()):
    nc = bass.Bass("TRN2", target_bir_lowering=False)
    P = Prog(nc)

    def ein(name, shape, dt=F32):
        return nc.dram_tensor(name, list(shape), dt, kind="ExternalInput").ap()

    x_in = ein("x", [NSMP, LAT, D])
    ctx_in = ein("ctx", [NSMP, CTX, D])
    cvec_in = ein("cvec", [128, 16, 3])
    w_ada = ein("w_ada", [depth, D, 6 * D])
    b_ada = ein("b_ada", [depth, 6 * D])
    w_in = ein("w_in", [depth, D, INW])
    w_out = ein("w_out", [depth, D, D])
    w_router = ein("w_router", [depth, D, NE])
    w_gate = ein("w_gate", [depth, NE, D, FF])
    w_up = ein("w_up", [depth, NE, D, FF])
    w_down = ein("w_down", [depth, NE, FF, D])
    smallp_in = ein("smallp", [depth, 128, NSP])
    gfin_in = ein("gfin", [128, D])
    ident_in = ein("ident", [128, 128])
    rperm_in = ein("rperm", [128, 128])
    rope_in = ein("ropeT", [128, 2, LAT])
    dftc_in = ein("dftc", [128, 256])
    dftn_in = ein("dftn", [2, LAT, LAT], BF16)
    dft256_in = ein("dft256", [2, CTX, CTX], BF16)
    rowbase_in = ein("rowbase", [32, 2])
    out_d = nc.dram_tensor("out", [NSMP, LAT, D], F32, kind="ExternalOutput").ap()

    xs = P.dram("xs", [NT, D], F32).t
    qT = P.dram("qT", [1024, NT], BF16).t
    kT = P.dram("kT", [1024, NT], BF16).t
    vv = P.dram("vv", [NT, 1024], BF16).t
    zT = P.dram("zT", [512, NT], BF16).t
    fT = P.dram("fT", [512, NT], BF16).t
    mixT = P.dram("mixT", [D, NT], BF16).t
    xn_d = P.dram("xn_d", [NT, D], F32).t
    gt_d = P.dram("gt_d", [2, 3, D], F32).t
    scratch = {"xs": (xs, [NT, D], F32), "qT": (qT, [1024, NT], BF16), "kT": (kT, [1024, NT], BF16),
               "vv": (vv, [NT, 1024], BF16), "zT": (zT, [512, NT], BF16), "fT": (fT, [512, NT], BF16),
               "mixT": (mixT, [D, NT], BF16), "xn_d": (xn_d, [NT, D], F32), "gt_d": (gt_d, [2, 3, D], F32)}

    ps = [P.psum("ps%d" % i) for i in range(8)]

    ident = P.sbuf("ident", [128, 128], F32)
    rperm = P.sbuf("rperm", [128, 128], F32)
    onesb = P.sbuf("onesb", [128, 128], BF16)
    onesf = P.sbuf("onesf", [128, 128], F32)
    epst = P.sbuf("epst", [128, 1], F32)
    csil = P.sbuf("csil", [128, 16, 3], F32)
    cT3 = P.sbuf("cT3", [128, 16, 3], BF16)
    modT = P.sbuf("modT", [128, 96, 3], F32)
    smallp = P.sbuf("smallp", [128, NSP], F32)
    a1 = P.sbuf("a1", [128, 16, 3], F32)
    a2 = P.sbuf("a2", [128, 16, 3], F32)
    neglam = P.sbuf("neglam", [128, 1], F32)
    gsubs = P.sbuf("gsubs", [128, 1], F32)
    rowbase = P.sbuf("rowbase", [32, 2], F32)
    mhalf_g = P.sbuf("mhalf_g", [128, 1], F32)

    def MM(psb, out_ap, pairs, reads):
        n = len(pairs)
        for i, (l, r) in enumerate(pairs):
            P.op("pe", lambda e, l=l, r=r, i=i: e.matmul(out_ap, l, r, start=(i == 0), stop=(i == n - 1)),
                 reads=reads, writes=[psb])

    def TR(psb, out_ap, in_ap, rows, reads):
        P.op("pe", lambda e: e.transpose(out_ap, in_ap, ident.t[0:rows, 0:rows]), reads=list(reads) + [ident],
             writes=[psb])

    def ACT(out, in_, func, reads, writes, **kw):
        P.op("act", lambda e: e.activation(out, in_, func, **kw), reads=reads, writes=writes)

    def TS(eng, out, in0, s1, s2, op0, op1, reads, writes):
        P.op(eng, lambda e: e.tensor_scalar(out, in0, s1, s2, op0, op1), reads=reads, writes=writes)

    def TT(eng, out, in0, in1, op, reads, writes):
        P.op(eng, lambda e: e.tensor_tensor(out, in0, in1, op), reads=reads, writes=writes)

    def STT(out, in0, scalar, in1, op0, op1, reads, writes):
        P.op("dve", lambda e: e.scalar_tensor_tensor(out, in0, scalar, in1, op0, op1), reads=reads, writes=writes)

    def RECIP(out, in_, reads, writes):
        P.op("dve", lambda e: e.reciprocal(out, in_), reads=reads, writes=writes)

    def MEMSET(eng, ap, val, writes):
        P.op(eng, lambda e: e.memset(ap, val), reads=[], writes=writes)

    def variant(s, is_ctx):
        return 2 if is_ctx else s

    P.dma("sp", ident.t[:, :], ident_in[:, :], writes=[ident])
    P.dma("sp", rperm.t[:, :], rperm_in[:, :], writes=[rperm])
    P.dma("sp", csil.t[:, :, :], cvec_in[:, :, :], writes=[csil])
    P.dma("sp", rowbase.t[:, :], rowbase_in[:, :], writes=[rowbase])
    for s in range(NSMP):
        P.dma("sp", xs[s * SEG:s * SEG + CTX, :], ctx_in[s, :, :])
        P.dma("sp", xs[s * SEG + CTX:(s + 1) * SEG, :], x_in[s, :, :])
    MEMSET("dve", onesb.t[:, :], 1.0, [onesb])
    MEMSET("dve", onesf.t[:, :], 1.0, [onesf])
    MEMSET("dve", epst.t[:, :], EPS, [epst])
    MEMSET("dve", mhalf_g.t[:, :], -0.5, [mhalf_g])
    ACT(csil.t[:, :, :], csil.t[:, :, :], AF.Silu, [csil], [csil])
    P.op("dve", lambda e: e.tensor_copy(cT3.t[:, :, :], csil.t[:, :, :]), reads=[csil], writes=[cT3])
    P.barrier()

    def norm_block(xt, xnb, junk, ssb, col, row0, dst_store=None):
        P.dma("sp", xt.t[:, :], xs[row0:row0 + 128, :], writes=[xt])
        ACT(junk.t[:, :], xt.t[:, :], AF.Square, [xt], [junk, ssb], accum_out=ssb.t[:, col:col + 1])
        TS("dve", ssb.t[:, col + 64:col + 65], ssb.t[:, col:col + 1], 1.0 / D, EPS, ALU.mult, ALU.add, [ssb], [ssb])
        TT("pool", ssb.t[:, col + 128:col + 129], ssb.t[:, col + 64:col + 65], mhalf_g.t[:, 0:1], ALU.pow,
           [ssb, mhalf_g], [ssb])
        TS("pool", xnb.t[:, :], xt.t[:, :], ssb.t[:, col + 128:col + 129], 1.0, ALU.mult, ALU.mult,
           [xt, ssb], [xnb])

    psrr = [0]

    def nextps(lo=0, hi=8):
        i = lo + (psrr[0] % (hi - lo))
        psrr[0] += 1
        return ps[i]

    evrr = [0]

    def evac_mod(out_ap, in_ap, sc_ap, bi_ap, reads, writes, use_act=None):
        evrr[0] += 1
        if use_act is None:
            use_act = (evrr[0] % 2 == 0)
        if use_act:
            ACT(out_ap, in_ap, AF.Identity, reads, writes, scale=sc_ap, bias=bi_ap)
        else:
            TS("dve", out_ap, in_ap, sc_ap, bi_ap, ALU.mult, ALU.add, reads, writes)

    def transpose_mod(xnb, rows, dst, dst_col0, amod, shoff, v, lo=0, hi=4):
        for g in range(4):
            pb = nextps(lo, hi)
            for i in range(4):
                c = g * 4 + i
                TR(pb, pb.t[:, i * 128:i * 128 + rows], xnb.t[0:rows, c * 128:(c + 1) * 128], rows, [xnb])
            for i in range(4):
                c = g * 4 + i
                evac_mod(dst.t[:, c, dst_col0:dst_col0 + rows], pb.t[:, i * 128:i * 128 + rows],
                         amod.t[:, c, v:v + 1], modT.t[:, shoff + c, v:v + 1], [pb, amod, modT], [dst],
                         use_act=(g % 2 == 0))

    TT_TILES = [(0, 256, True), (256, 512, False), (768, 512, False), (1280, 512, False), (1792, 512, False)]

    for l in range(depth):
        lam_init = 0.8 - 0.6 * float(np.exp(-0.3 * l))
        P.push_scope()
        P.dma("sp", smallp.t[:, :], smallp_in[l, :, :], writes=[smallp])
        wsl = [P.sbuf("m_wsl%d" % i, [128, 16, 512], BF16) for i in range(2)]
        bsl = [P.sbuf("m_bsl%d" % i, [1, 512], F32) for i in range(2)]
        gtmp = [P.sbuf("m_gtmp%d" % i, [128, 512], F32) for i in range(2)]
        crep = P.sbuf("m_crep", [128, 16, 3, 128], BF16)
        for j in range(16):
            for v in range(3):
                TS("dve", crep.t[:, j, v, :], onesf.t[:, :], csil.t[:, j, v:v + 1], None, ALU.mult, ALU.bypass,
                   [onesf, csil], [crep])
        gi = 0
        for k in range(24):
            w = wsl[k % 2]
            b = bsl[k % 2]
            P.dma("pool", w.t[:, :, :], w_ada[l, :, k * 512:(k + 1) * 512].rearrange("(c p) n -> p c n", p=128),
                  writes=[w])
            P.dma("sp", b.t[:, :], b_ada[l:l + 1, k * 512:(k + 1) * 512], writes=[b])
            for sub in range(4):
                pb = nextps(0, 4)
                pairs = [(w.t[:, dc, sub * 128:(sub + 1) * 128], cT3.t[:, dc, :]) for dc in range(16)]
                pairs.append((b.t[0:1, sub * 128:(sub + 1) * 128], onesf.t[0:1, 0:3]))
                MM(pb, pb.t[:, 0:3], pairs, [w, b, cT3, onesf])
                ACT(modT.t[:, k * 4 + sub, :], pb.t[:, 0:3], AF.Copy, [pb], [modT])
            if k in (8, 9, 10, 11, 20, 21, 22, 23):
                g = 0 if k < 12 else 1
                ct = k % 4
                for v in range(3):
                    pb = nextps(4, 8)
                    pairs = [(crep.t[:, dc, v, :], w.t[:, dc, :]) for dc in range(16)]
                    pairs.append((onesf.t[0:1, 0:128], b.t[0:1, :]))
                    MM(pb, pb.t[:, :], pairs, [w, b, crep, onesf])
                    gt = gtmp[gi % 2]
                    gi += 1
                    ACT(gt.t[:, :], pb.t[:, :], AF.Copy, [pb], [gt])
                    P.dma("sp", gt_d[g, v:v + 1, ct * 512:(ct + 1) * 512], gt.t[0:1, :], reads=[gt])
        for v in range(3):
            STT(a1.t[:, :, v], modT.t[:, 16:32, v], 1.0, smallp.t[:, SP_G1:SP_G1 + 16], ALU.add, ALU.mult,
                [modT, smallp], [a1])
            STT(a2.t[:, :, v], modT.t[:, 64:80, v], 1.0, smallp.t[:, SP_G2:SP_G2 + 16], ALU.add, ALU.mult,
                [modT, smallp], [a2])
        lt = P.sbuf("m_lt", [128, 2, 64], F32)
        ls = P.sbuf("m_ls", [128, 4], F32)
        dl = smallp.t[:, SP_DL:SP_DL + 256].rearrange("p (a d) -> p a d", a=4)
        TT("dve", lt.t[:, 0, :], dl[:, 0, :], dl[:, 1, :], ALU.mult, [smallp], [lt])
        TT("dve", lt.t[:, 1, :], dl[:, 2, :], dl[:, 3, :], ALU.mult, [smallp], [lt])
        P.op("dve", lambda e, ls=ls, lt=lt: e.reduce_sum(ls.t[:, 0:2], lt.t[:, :, :], AX.X), reads=[lt], writes=[ls])
        ACT(ls.t[:, 2:4], ls.t[:, 0:2], AF.Exp, [ls], [ls])
        TT("dve", neglam.t[:, :], ls.t[:, 3:4], ls.t[:, 2:3], ALU.subtract, [ls], [neglam])
        TS("dve", neglam.t[:, :], neglam.t[:, :], -lam_init, None, ALU.add, ALU.bypass, [neglam], [neglam])
        TS("dve", gsubs.t[:, :], smallp.t[:, SP_GS:SP_GS + 1], 1.0 - lam_init, None, ALU.mult, ALU.bypass,
           [smallp], [gsubs])
        P.pop_scope()
        if stop_after == ("M", l):
            break

        for s in range(NSMP):
            P.push_scope()
            hT = P.sbuf("a_hT", [128, 16, SEG], BF16, nowaw=True)
            xt = [P.sbuf("a_xt%d" % i, [128, D], F32) for i in range(2)]
            xnb = [P.sbuf("a_xn%d" % i, [128, D], F32) for i in range(2)]
            junk = P.sbuf("a_junk", [128, D], BF16)
            ssb = P.sbuf("a_ss", [128, 192], F32)
            wsl = [P.sbuf("a_wsl%d" % i, [128, 16, 512], BF16) for i in range(2)]
            rope = P.sbuf("a_rope", [128, 2, LAT], F32)
            qf = [P.sbuf("a_qf%d" % i, [128, 512], F32) for i in range(2)]
            sgb = [P.sbuf("a_sg%d" % i, [128, 512], F32) for i in range(2)]
            ob = [P.sbuf("a_ob%d" % i, [128, 512], BF16) for i in range(3)]
            zf = [P.sbuf("a_zf%d" % i, [128, 512], BF16) for i in range(2)]
            P.dma("sp", rope.t[:, :, :], rope_in[:, :, :], writes=[rope])
            for bb in range(18):
                row0 = s * SEG + bb * 128
                norm_block(xt[bb % 2], xnb[bb % 2], junk, ssb, bb, row0)
                transpose_mod(xnb[bb % 2], 128, hT, bb * 128, a1, 0, variant(s, bb < 2), 0, 4)
            slabs = [("q", 0, 0), ("q", 512, 4), ("k", 1024, 0), ("k", 1536, 4), ("v", 2048, 0), ("v", 2560, 512),
                     ("glu", 0, 0), ("glu", 256, 256), ("f", 4096, 0)]
            cnt = 0
            for si, (kind, c0, aux) in enumerate(slabs):
                w = wsl[si % 2]
                if kind == "glu":
                    P.dma("pool", w.t[:, :, 0:256],
                          w_in[l, :, 3072 + c0:3072 + c0 + 256].rearrange("(c p) n -> p c n", p=128), writes=[w])
                    P.dma("pool", w.t[:, :, 256:512],
                          w_in[l, :, 3584 + c0:3584 + c0 + 256].rearrange("(c p) n -> p c n", p=128), writes=[w])
                else:
                    P.dma("pool", w.t[:, :, :], w_in[l, :, c0:c0 + 512].rearrange("(c p) n -> p c n", p=128),
                          writes=[w])
                if kind in ("q", "k"):
                    dst = qT if kind == "q" else kT
                    for sub in range(4):
                        r0 = (aux + sub) * 128
                        for (t0, tw, isc) in TT_TILES:
                            pb = nextps(0, 5)
                            MM(pb, pb.t[:, 0:tw], [(w.t[:, dc, sub * 128:(sub + 1) * 128], hT.t[:, dc, t0:t0 + tw])
                                                   for dc in range(16)], [w, hT])
                            o = ob[cnt % 3]
                            cnt += 1
                            if isc:
                                ACT(o.t[:, 0:tw], pb.t[:, 0:tw], AF.Copy, [pb], [o])
                            else:
                                q = qf[cnt % 2]
                                sg = sgb[cnt % 2]
                                p0 = t0 - CTX
                                ACT(q.t[:, 0:tw], pb.t[:, 0:tw], AF.Copy, [pb], [q])
                                pr = nextps(5, 8)
                                MM(pr, pr.t[:, 0:tw], [(rperm.t[:, :], q.t[:, 0:tw])], [rperm, q])
                                TT("dve", sg.t[:, 0:tw], pr.t[:, 0:tw], rope.t[:, 1, p0:p0 + tw], ALU.mult,
                                   [pr, rope], [sg])
                                TT("pool", q.t[:, 0:tw], q.t[:, 0:tw], rope.t[:, 0, p0:p0 + tw], ALU.mult,
                                   [q, rope], [q])
                                TT("dve", o.t[:, 0:tw], q.t[:, 0:tw], sg.t[:, 0:tw], ALU.add, [q, sg], [o])
                            P.dma("sp", dst[r0:r0 + 128, s * SEG + t0:s * SEG + t0 + tw], o.t[:, 0:tw], reads=[o])
                elif kind == "v":
                    for tb in range(18):
                        pb = nextps(0, 5)
                        MM(pb, pb.t[:, :], [(hT.t[:, dc, tb * 128:(tb + 1) * 128], w.t[:, dc, :]) for dc in range(16)],
                           [w, hT])
                        o = ob[cnt % 3]
                        cnt += 1
                        ACT(o.t[:, :], pb.t[:, :], AF.Copy, [pb], [o])
                        P.dma("sp", vv[s * SEG + tb * 128:s * SEG + (tb + 1) * 128, aux:aux + 512], o.t[:, :],
                              reads=[o])
                elif kind == "glu":
                    for sub in range(2):
                        ch0 = aux + sub * 128
                        for (t0, tw, isc) in TT_TILES:
                            pa = nextps(0, 5)
                            MM(pa, pa.t[:, 0:tw], [(w.t[:, dc, sub * 128:(sub + 1) * 128], hT.t[:, dc, t0:t0 + tw])
                                                   for dc in range(16)], [w, hT])
                            pg = nextps(5, 8)
                            MM(pg, pg.t[:, 0:tw], [(w.t[:, dc, 256 + sub * 128:256 + (sub + 1) * 128],
                                                    hT.t[:, dc, t0:t0 + tw]) for dc in range(16)], [w, hT])
                            sg = sgb[cnt % 2]
                            z = zf[cnt % 2]
                            cnt += 1
                            ACT(sg.t[:, 0:tw], pg.t[:, 0:tw], AF.Sigmoid, [pg], [sg])
                            TT("dve", z.t[:, 0:tw], pa.t[:, 0:tw], sg.t[:, 0:tw], ALU.mult, [pa, sg], [z])
                            P.dma("sp", zT[ch0:ch0 + 128, s * SEG + t0:s * SEG + t0 + tw], z.t[:, 0:tw], reads=[z])
                else:
                    for sub in range(4):
                        for (t0, tw, isc) in TT_TILES:
                            pb = nextps(0, 5)
                            MM(pb, pb.t[:, 0:tw], [(w.t[:, dc, sub * 128:(sub + 1) * 128], hT.t[:, dc, t0:t0 + tw])
                                                   for dc in range(16)], [w, hT])
                            o = ob[cnt % 3]
                            cnt += 1
                            ACT(o.t[:, 0:tw], pb.t[:, 0:tw], AF.Copy, [pb], [o])
                            P.dma("sp", fT[sub * 128:(sub + 1) * 128, s * SEG + t0:s * SEG + t0 + tw], o.t[:, 0:tw],
                                  reads=[o])
            P.pop_scope()
        if stop_after == ("A", l):
            break

        for s in range(NSMP):
            P.push_scope()
            kh = [P.sbuf("b_kh%d" % i, [128, SEG], BF16) for i in range(2)]
            qz = [[P.sbuf("b_qz%d%d" % (m, i), [128, SEG], BF16) for i in range(2)] for m in range(2)]
            vh = [P.sbuf("b_vh%d" % i, [128, 18, 132], BF16) for i in range(2)]
            Eb = [P.sbuf("b_E%d" % i, [128, 18, 512], BF16, nowaw=True) for i in range(2)]
            osb = [P.sbuf("b_o%d" % i, [128, 4, 128], F32) for i in range(2)]
            onb = [P.sbuf("b_on%d" % i, [128, 128], F32) for i in range(4)]
            rr = P.sbuf("b_rr", [128, 64], F32)
            rq = P.sbuf("b_rq", [128, 64], F32)
            mhalf = P.sbuf("b_mhalf", [128, 1], F32)
            junk = P.sbuf("b_junk", [128, 128], F32)
            mo = [P.sbuf("b_mo%d" % i, [128, 512], BF16) for i in range(2)]
            zp = [P.sbuf("c_zp%d" % i, [128, LAT + 30], BF16) for i in range(2)]
            dg = P.sbuf("c_dg", [128, 124, 128], BF16)
            accL = P.sbuf("c_accL", [128, 4, LAT], F32)
            accC = P.sbuf("c_accC", [128, 4, CTX], F32)
            MEMSET("pool", mhalf.t[:, :], -0.5, [mhalf])
            for i in range(2):
                MEMSET("pool", vh[i].t[:, :, 128:132], 1.0, [vh[i]])
                MEMSET("pool", qz[0][i].t[64:128, :], 0.0, [qz[0][i]])
                MEMSET("pool", qz[1][i].t[0:64, :], 0.0, [qz[1][i]])
            rcs = [0, 0]

            def rcols(n, which=0):
                if rcs[which] + n > 64:
                    rcs[which] = 0
                c = rcs[which]
                rcs[which] += n
                return c

            cw = smallp.t[:, SP_CW:SP_CW + 124].rearrange("p (c k) -> p c k", c=4)

            for cc in range(4):
                for k in range(31):
                    TS("dve", dg.t[:, cc * 31 + k, :], ident.t[:, :], cw[:, cc, k:k + 1], None, ALU.mult, ALU.bypass,
                       [ident, smallp], [dg])

            def conv_gen():
                zc = 0
                for (g0, n, acc) in ((0, CTX, accC), (CTX, LAT, accL)):
                    for cc in range(4):
                        z = zp[zc % 2]
                        zc += 1
                        MEMSET("pool", z.t[:, 0:15], 0.0, [z])
                        MEMSET("pool", z.t[:, 15 + n:30 + n], 0.0, [z])
                        P.dma("sp", z.t[:, 15:15 + n], zT[cc * 128:(cc + 1) * 128, s * SEG + g0:s * SEG + g0 + n],
                              writes=[z])
                        for t0 in range(0, n, 512):
                            tw = min(512, n - t0)
                            pb = nextps(6, 8)
                            for k in range(31):
                                P.op("pe", lambda e, pb=pb, z=z, cc=cc, k=k, t0=t0, tw=tw, dg=dg: e.matmul(
                                    pb.t[:, 0:tw], dg.t[:, cc * 31 + k, :], z.t[:, t0 + k:t0 + k + tw],
                                    start=(k == 0), stop=(k == 30)), reads=[dg, z], writes=[pb])
                                yield
                            TS("dve", acc.t[:, cc, t0:t0 + tw], pb.t[:, 0:tw], smallp.t[:, SP_CB + cc:SP_CB + cc + 1],
                               None, ALU.add, ALU.bypass, [pb, smallp], [acc])

            units = [(h, ti, m) for h in range(H) for ti in range(len(TT_TILES)) for m in range(2)]
            loaded = set()

            def load_head(h):
                if h in loaded:
                    return
                loaded.add(h)
                k_, v_ = kh[h % 2], vh[h % 2]
                P.dma("sp", k_.t[:, :], kT[h * 128:(h + 1) * 128, s * SEG:(s + 1) * SEG], writes=[k_])
                for m in range(2):
                    q_ = qz[m][h % 2]
                    P.dma("sp", q_.t[m * 64:(m + 1) * 64, :],
                          qT[h * 128 + m * 64:h * 128 + (m + 1) * 64, s * SEG:(s + 1) * SEG], writes=[q_])
                P.dma("sp", v_.t[:, :, 0:128],
                      vv[s * SEG:(s + 1) * SEG, h * 128:(h + 1) * 128].rearrange("(c p) e -> p c e", p=128),
                      writes=[v_])

            def post_fn(h, ti):
                q0, qw, isc = TT_TILES[ti]
                nj = qw // 128
                qtc = h * len(TT_TILES) + ti
                o_ = osb[qtc % 2]
                m_o = mo[qtc % 2]

                def post_a():
                    for j in range(nj):
                        c0 = rcols(3, 1)
                        on = onb[j]
                        P.op("dve", lambda e, j=j, c0=c0, junk=junk, rq=rq, o_=o_: e.scalar_tensor_tensor(
                            junk.t[:, :], o_.t[:, j, :], 1.0, o_.t[:, j, :], ALU.mult, ALU.mult,
                            accum_out=rq.t[:, c0:c0 + 1]), reads=[o_], writes=[junk, rq])
                        TS("dve", rq.t[:, c0 + 1:c0 + 2], rq.t[:, c0:c0 + 1], 1.0 / 128, EPS, ALU.mult, ALU.add,
                           [rq], [rq])
                        TT("pool", rq.t[:, c0 + 2:c0 + 3], rq.t[:, c0 + 1:c0 + 2], mhalf.t[:, 0:1], ALU.pow,
                           [rq, mhalf], [rq])
                        TS("dve", on.t[:, :], o_.t[:, j, :], rq.t[:, c0 + 2:c0 + 3], None, ALU.mult, ALU.bypass,
                           [o_, rq], [on])

                def post_b():
                    pT = ps[5]
                    for j in range(nj):
                        TR(pT, pT.t[:, j * 128:(j + 1) * 128], onb[j].t[:, :], 128, [onb[j]])
                    TS("dve", m_o.t[:, 0:qw], pT.t[:, 0:qw], gsubs.t[:, 0:1], None, ALU.mult, ALU.bypass,
                       [pT, gsubs], [m_o])
                    P.dma("sp", mixT[h * 128:(h + 1) * 128, s * SEG + q0:s * SEG + q0 + qw], m_o.t[:, 0:qw],
                          reads=[m_o])
                return post_a, post_b

            def pv_gen(u):
                h, ti, m = u
                v_ = vh[h % 2]
                q0, qw, isc = TT_TILES[ti]
                nkc = 2 if isc else 18
                nj = qw // 128
                qtc = h * len(TT_TILES) + ti
                o_ = osb[qtc % 2]
                E = Eb[m]
                for j in range(nj):
                    pO = nextps(3, 5)
                    for kc in range(nkc):
                        P.op("pe", lambda e, pO=pO, E=E, v_=v_, j=j, kc=kc, nkc=nkc: e.matmul(
                            pO.t[:, 0:129], E.t[:, kc, j * 128:(j + 1) * 128], v_.t[:, kc, 0:129],
                            start=(kc == 0), stop=(kc == nkc - 1)), reads=[E, v_], writes=[pO])
                        yield
                    c1 = rcols(2, 0)
                    r1 = rr.t[:, c1:c1 + 1]
                    RECIP(r1, pO.t[:, 128:129], [pO], [rr])
                    if m == 0:
                        TS("dve", o_.t[:, j, :], pO.t[:, 0:128], r1, None, ALU.mult, ALU.bypass, [pO, rr], [o_])
                    else:
                        r2 = rr.t[:, c1 + 1:c1 + 2]
                        TT("dve", r2, r1, neglam.t[:, 0:1], ALU.mult, [rr, neglam], [rr])
                        STT(o_.t[:, j, :], pO.t[:, 0:128], r2, o_.t[:, j, :], ALU.mult, ALU.add,
                            [pO, rr, o_], [o_])

            def block(u_next, u_cur):
                pv = pv_gen(u_cur) if u_cur is not None else None
                npv = 0
                if u_cur is not None:
                    _, qw_c, isc_c = TT_TILES[u_cur[1]]
                    npv = (qw_c // 128) * (2 if isc_c else 18)
                if u_next is not None:
                    h, ti, m = u_next
                    load_head(h)
                    k_, q_ = kh[h % 2], qz[m][h % 2]
                    q0, qw, isc = TT_TILES[ti]
                    nkc = 2 if isc else 18
                    E = Eb[m]
                    per = -(-npv // nkc)
                    for kc in range(nkc):
                        pS = nextps(0, 3)
                        MM(pS, pS.t[:, 0:qw], [(k_.t[:, kc * 128:(kc + 1) * 128], q_.t[:, q0:q0 + qw])], [k_, q_])
                        ACT(E.t[:, kc, 0:qw], pS.t[:, 0:qw], AF.Exp, [pS], [E], scale=0.125)
                        if pv is not None:
                            for _ in range(per):
                                next(pv, None)
                if pv is not None:
                    for _ in pv:
                        pass

            cg = conv_gen()
            pending = None
            block(units[0], None)
            for ui, u in enumerate(units):
                block(units[ui + 1] if ui + 1 < len(units) else None, u)
                if pending is not None:
                    pending()
                    pending = None
                if u[2] == 1:
                    pa_, pending = post_fn(u[0], u[1])
                    pa_()
                for _ in range(8):
                    next(cg, None)
            if pending is not None:
                pending()
            for _ in cg:
                pass

            cb16 = P.sbuf("c_cb", [128, 4, 512], BF16)
            sq16 = P.sbuf("c_sq", [128, 4, 512], BF16)
            mean = P.sbuf("c_mean", [128, 512], F32)
            rstd = P.sbuf("c_rstd", [128, 512], F32)
            tmp = [P.sbuf("c_tmp%d" % i, [128, 512], F32) for i in range(2)]
            co = [P.sbuf("c_o%d" % i, [128, 512], BF16) for i in range(2)]
            for (g0, n, acc) in ((0, CTX, accC), (CTX, LAT, accL)):
                for t0 in range(0, n, 512):
                    tw = min(512, n - t0)
                    for cc in range(4):
                        ACT(cb16.t[:, cc, 0:tw], acc.t[:, cc, t0:t0 + tw], AF.Copy, [acc], [cb16])
                        ACT(sq16.t[:, cc, 0:tw], acc.t[:, cc, t0:t0 + tw], AF.Square, [acc], [sq16])
                    pM = nextps(0, 4)
                    MM(pM, pM.t[:, 0:tw], [(onesb.t[:, :], cb16.t[:, cc, 0:tw]) for cc in range(4)], [onesb, cb16])
                    pQ = nextps(4, 8)
                    MM(pQ, pQ.t[:, 0:tw], [(onesb.t[:, :], sq16.t[:, cc, 0:tw]) for cc in range(4)], [onesb, sq16])
                    TS("dve", mean.t[:, 0:tw], pM.t[:, 0:tw], 1.0 / 512, None, ALU.mult, ALU.bypass, [pM], [mean])
                    TT("dve", rstd.t[:, 0:tw], mean.t[:, 0:tw], mean.t[:, 0:tw], ALU.mult, [mean], [rstd])
                    STT(rstd.t[:, 0:tw], pQ.t[:, 0:tw], 1.0 / 512, rstd.t[:, 0:tw], ALU.mult, ALU.subtract,
                        [pQ, rstd], [rstd])
                    ACT(rstd.t[:, 0:tw], rstd.t[:, 0:tw], AF.Sqrt, [rstd, epst], [rstd], bias=epst.t[:, 0:1])
                    RECIP(rstd.t[:, 0:tw], rstd.t[:, 0:tw], [rstd], [rstd])
                    for cc in range(4):
                        t_ = tmp[cc % 2]
                        o = co[cc % 2]
                        TT("dve", t_.t[:, 0:tw], acc.t[:, cc, t0:t0 + tw], mean.t[:, 0:tw], ALU.subtract,
                           [acc, mean], [t_])
                        TT("pool", t_.t[:, 0:tw], t_.t[:, 0:tw], rstd.t[:, 0:tw], ALU.mult, [t_, rstd], [t_])
                        ACT(o.t[:, 0:tw], t_.t[:, 0:tw], AF.Silu, [t_, smallp], [o],
                            scale=smallp.t[:, SP_LG + cc:SP_LG + cc + 1], bias=smallp.t[:, SP_LB + cc:SP_LB + cc + 1])
                        P.dma("sp", mixT[1024 + cc * 128:1024 + (cc + 1) * 128,
                                         s * SEG + g0 + t0:s * SEG + g0 + t0 + tw], o.t[:, 0:tw], reads=[o])
            P.pop_scope()
        if stop_after in (("B", l), ("C", l)):
            break

        P.push_scope()
        Aall = [P.sbuf("d_A%d" % i, [128, 16, 256], BF16, nowaw=True) for i in range(8)]
        Ac = [P.sbuf("d_Ac%d" % i, [128, 2, 256], BF16) for i in range(8)]
        uT = [P.sbuf("d_u%d" % i, [128, LAT], BF16) for i in range(2)]
        uC = [P.sbuf("d_uc%d" % i, [128, CTX], BF16) for i in range(2)]
        csc = P.sbuf("d_csc", [128, 256], BF16)
        tabs = [P.sbuf("d_tab%d" % i, [128, 16, 2, 512], BF16) for i in range(2)]
        tabc = P.sbuf("d_tabc", [128, 2, 2, 256], BF16)
        do = [P.sbuf("d_o%d" % i, [128, 512], BF16) for i in range(3)]
        P.dma("pool", csc.t[:, :], dftc_in[:, :], writes=[csc])
        for cs in range(2):
            P.dma("sp", tabc.t[:, :, cs, :], dft256_in[cs, :, :].rearrange("(c p) n -> p c n", p=128), writes=[tabc])
        dc_ = 0
        for s in range(NSMP):
            for fh in range(4):
                u = uT[(s * 4 + fh) % 2]
                uc = uC[(s * 4 + fh) % 2]
                A = Aall[s * 4 + fh]
                A2 = Ac[s * 4 + fh]
                P.dma("sp", u.t[:, :], fT[fh * 128:(fh + 1) * 128, s * SEG + CTX:(s + 1) * SEG], writes=[u])
                P.dma("sp", uc.t[:, :], fT[fh * 128:(fh + 1) * 128, s * SEG:s * SEG + CTX], writes=[uc])
                for ch in range(16):
                    pb = nextps(0, 4)
                    MM(pb, pb.t[:, 0:256], [(u.t[:, ch * 128:(ch + 1) * 128], csc.t[:, :])], [u, csc])
                    evrr[0] += 1
                    if evrr[0] % 2 == 0:
                        ACT(A.t[:, ch, :], pb.t[:, 0:256], AF.Copy, [pb], [A])
                    else:
                        P.op("dve", lambda e, A=A, pb=pb, ch=ch: e.tensor_copy(A.t[:, ch, :], pb.t[:, 0:256]),
                             reads=[pb], writes=[A])
                for ch in range(2):
                    pb = nextps(0, 4)
                    MM(pb, pb.t[:, 0:256], [(uc.t[:, ch * 128:(ch + 1) * 128], csc.t[:, :])], [uc, csc])
                    ACT(A2.t[:, ch, :], pb.t[:, 0:256], AF.Copy, [pb], [A2])
                pb = nextps(4, 8)
                pairs = []
                for ch in range(2):
                    pairs.append((A2.t[:, ch, 0:128], tabc.t[:, ch, 0, :]))
                    pairs.append((A2.t[:, ch, 128:256], tabc.t[:, ch, 1, :]))
                MM(pb, pb.t[:, 0:256], pairs, [A2, tabc])
                o = do[dc_ % 3]
                dc_ += 1
                ACT(o.t[:, 0:256], pb.t[:, 0:256], AF.Copy, [pb], [o])
                P.dma("sp", mixT[1536 + fh * 128:1536 + (fh + 1) * 128, s * SEG:s * SEG + CTX], o.t[:, 0:256],
                      reads=[o])
        for nt in range(4):
            tb = tabs[nt % 2]
            for cs in range(2):
                P.dma("sp", tb.t[:, :, cs, :],
                      dftn_in[cs, :, nt * 512:(nt + 1) * 512].rearrange("(c p) n -> p c n", p=128), writes=[tb])
            for s in range(NSMP):
                for fh in range(4):
                    A = Aall[s * 4 + fh]
                    pb = nextps(4, 8)
                    pairs = []
                    for ch in range(16):
                        pairs.append((A.t[:, ch, 0:128], tb.t[:, ch, 0, :]))
                        pairs.append((A.t[:, ch, 128:256], tb.t[:, ch, 1, :]))
                    MM(pb, pb.t[:, :], pairs, [A, tb])
                    o = do[dc_ % 3]
                    dc_ += 1
                    ACT(o.t[:, :], pb.t[:, :], AF.Copy, [pb], [o])
                    P.dma("sp", mixT[1536 + fh * 128:1536 + (fh + 1) * 128,
                                     s * SEG + CTX + nt * 512:s * SEG + CTX + (nt + 1) * 512], o.t[:, :], reads=[o])
        P.pop_scope()
        if stop_after == ("D", l):
            break

        P.push_scope()
        wout = P.sbuf("e_wout", [128, 16, D], BF16)
        gtb = [P.sbuf("e_gtb%d" % v, [128, D], F32) for v in range(3)]
        mx = [P.sbuf("e_mx%d" % i, [128, 16, 128], BF16) for i in range(2)]
        xt = [P.sbuf("e_xt%d" % i, [128, D], F32) for i in range(2)]
        tmp = [P.sbuf("e_tmp%d" % i, [128, 512], F32) for i in range(2)]
        for ct in range(4):
            P.dma("pool", wout.t[:, :, ct * 512:(ct + 1) * 512],
                  w_out[l, :, ct * 512:(ct + 1) * 512].rearrange("(c p) n -> p c n", p=128), writes=[wout])
        for v in range(3):
            P.dma("sp", gtb[v].t[:, :], gt_d[0, v, :].partition_broadcast(128), writes=[gtb[v]])
        tc_ = 0
        for b in range(NSMP * 18):
            s, bb = divmod(b, 18)
            v = variant(s, bb < 2)
            m_ = mx[b % 2]
            x_ = xt[b % 2]
            P.dma("sp", m_.t[:, :, :], mixT[:, b * 128:(b + 1) * 128].rearrange("(c p) t -> p c t", p=128),
                  writes=[m_])
            P.dma("sp", x_.t[:, :], xs[b * 128:(b + 1) * 128, :], writes=[x_])
            for dt_ in range(4):
                pb = nextps(0, 8)
                MM(pb, pb.t[:, :], [(m_.t[:, fc, :], wout.t[:, fc, dt_ * 512:(dt_ + 1) * 512]) for fc in range(16)],
                   [m_, wout])
                t_ = tmp[tc_ % 2]
                tc_ += 1
                TT("dve", t_.t[:, :], pb.t[:, :], gtb[v].t[:, dt_ * 512:(dt_ + 1) * 512], ALU.mult, [pb, gtb[v]], [t_])
                TT("pool", x_.t[:, dt_ * 512:(dt_ + 1) * 512], x_.t[:, dt_ * 512:(dt_ + 1) * 512], t_.t[:, :],
                   ALU.add, [x_, t_], [x_])
            P.dma("sp", xs[b * 128:(b + 1) * 128, :], x_.t[:, :], reads=[x_])
        P.pop_scope()
        if stop_after == ("E", l):
            break

        P.push_scope()
        wgu = [P.sbuf("f_wgu%d" % i, [128, 2, 16, 512], BF16) for i in range(2)]
        wd = P.sbuf("f_wd", [128, 8, D], BF16)
        for hf in range(2):
            P.dma("pool", wgu[hf].t[:, 0, :, :],
                  w_gate[l, 0, :, hf * 512:(hf + 1) * 512].rearrange("(c p) n -> p c n", p=128), writes=[wgu[hf]])
            P.dma("pool", wgu[hf].t[:, 1, :, :],
                  w_up[l, 0, :, hf * 512:(hf + 1) * 512].rearrange("(c p) n -> p c n", p=128), writes=[wgu[hf]])
        for hh in range(2):
            P.dma("pool", wd.t[:, hh * 4:(hh + 1) * 4, :],
                  w_down[l, 0, hh * 512:(hh + 1) * 512, :].rearrange("(c p) n -> p c n", p=128), writes=[wd])
        valsTL = P.sbuf("f_valsTL", [128, 2, 32], F32)
        valsTC = P.sbuf("f_valsTC", [32, 32], F32)
        idxTL = P.sbuf("f_idxTL", [128, 2, 32], U32)
        idxTC = P.sbuf("f_idxTC", [32, 32], U32)
        P.push_scope()
        affp = P.sbuf("f_affp", [128, 18, 32], F32)
        affL = P.sbuf("f_affL", [32, LAT], F32)
        affC = P.sbuf("f_affC", [32, CTX], F32)
        valsL = P.sbuf("f_valsL", [32, CAPL], F32)
        valsC = P.sbuf("f_valsC", [32, CAPC], F32)
        idxL = P.sbuf("f_idxL", [32, CAPL], U32)
        idxC = P.sbuf("f_idxC", [32, CAPC], U32)
        idxLf = P.sbuf("f_idxLf", [32, CAPL], F32)
        idxCf = P.sbuf("f_idxCf", [32, CAPC], F32)
        idxTLf = P.sbuf("f_idxTLf", [128, 2, 32], F32)
        idxTCf = P.sbuf("f_idxTCf", [32, 32], F32)
        xt = [P.sbuf("f_xt%d" % i, [128, D], F32) for i in range(2)]
        xnb = [P.sbuf("f_xn%d" % i, [128, D], F32) for i in range(2)]
        junk = P.sbuf("f_junk", [128, D], BF16)
        ssb = P.sbuf("f_ss", [128, 192], F32)
        h2T = [P.sbuf("f_h2T%d" % i, [128, 16, 128], BF16, nowaw=True) for i in range(2)]
        wr = P.sbuf("f_wr", [128, 16, NE], BF16)
        sm = P.sbuf("f_sm", [128, 4 * 36], F32)
        ex = [P.sbuf("f_ex%d" % i, [128, NE], F32) for i in range(2)]
        P.dma("pool", wr.t[:, :, :], w_router[l, :, :].rearrange("(c p) e -> p c e", p=128), writes=[wr])
        for s in range(NSMP):
            for bb in range(18):
                b = s * 18 + bb
                x_ = xt[b % 2]
                xn_ = xnb[b % 2]
                h_ = h2T[b % 2]
                norm_block(x_, xn_, junk, ssb, bb, b * 128)
                P.dma("sp", xn_d[b * 128:(b + 1) * 128, :], xn_.t[:, :], reads=[xn_])
                transpose_mod(xn_, 128, h_, 0, a2, 48, variant(s, bb < 2), 0, 4)
                pb = nextps(4, 8)
                MM(pb, pb.t[:, 0:NE], [(h_.t[:, c, :], wr.t[:, c, :]) for c in range(16)], [h_, wr])
                c0 = b * 4
                P.op("dve", lambda e, pb=pb, c0=c0, sm=sm: e.reduce_max(sm.t[:, c0:c0 + 1], pb.t[:, 0:NE], AX.X),
                     reads=[pb], writes=[sm])
                TS("dve", sm.t[:, c0 + 1:c0 + 2], sm.t[:, c0:c0 + 1], -1.0, None, ALU.mult, ALU.bypass, [sm], [sm])
                e_ = ex[b % 2]
                ACT(e_.t[:, :], pb.t[:, 0:NE], AF.Exp, [pb, sm], [e_, sm], bias=sm.t[:, c0 + 1:c0 + 2],
                    accum_out=sm.t[:, c0 + 2:c0 + 3])
                RECIP(sm.t[:, c0 + 3:c0 + 4], sm.t[:, c0 + 2:c0 + 3], [sm], [sm])
                TS("dve", affp.t[:, bb, s * 16:(s + 1) * 16], e_.t[:, :], sm.t[:, c0 + 3:c0 + 4], None, ALU.mult,
                   ALU.bypass, [e_, sm], [affp])
        for bb in range(18):
            pb = nextps(0, 4)
            TR(pb, pb.t[0:32, 0:128], affp.t[:, bb, :], 128, [affp])
            if bb < 2:
                ACT(affC.t[:, bb * 128:(bb + 1) * 128], pb.t[0:32, 0:128], AF.Copy, [pb], [affC])
            else:
                ACT(affL.t[:, (bb - 2) * 128:(bb - 1) * 128], pb.t[0:32, 0:128], AF.Copy, [pb], [affL])
        for (aff, vals, idx, cap) in ((affL, valsL, idxL, CAPL), (affC, valsC, idxC, CAPC)):
            for r in range(cap // 8):
                P.op("dve", lambda e, aff=aff, vals=vals, r=r: e.max(out=vals.t[:, r * 8:(r + 1) * 8], in_=aff.t[:, :]),
                     reads=[aff], writes=[vals])
                P.op("dve", lambda e, aff=aff, vals=vals, idx=idx, r=r: e.max_index(
                    out=idx.t[:, r * 8:(r + 1) * 8], in_max=vals.t[:, r * 8:(r + 1) * 8], in_values=aff.t[:, :]),
                    reads=[aff, vals], writes=[idx])
                P.op("dve", lambda e, aff=aff, vals=vals, r=r: e.match_replace(
                    out=aff.t[:, :], in_to_replace=vals.t[:, r * 8:(r + 1) * 8], in_values=aff.t[:, :],
                    imm_value=-1.0), reads=[aff, vals], writes=[aff])
        for (idx, idxf, col) in ((idxL, idxLf, 0), (idxC, idxCf, 1)):
            P.op("dve", lambda e, idx=idx, idxf=idxf: e.tensor_copy(idxf.t[:, :], idx.t[:, :]), reads=[idx],
                 writes=[idxf])
            TS("dve", idxf.t[:, :], idxf.t[:, :], rowbase.t[:, col:col + 1], None, ALU.add, ALU.bypass,
               [idxf, rowbase], [idxf])
        for blk in range(2):
            pb = nextps(0, 4)
            TR(pb, pb.t[:, 0:32], valsL.t[:, blk * 128:(blk + 1) * 128], 32, [valsL])
            ACT(valsTL.t[:, blk, :], pb.t[:, 0:32], AF.Copy, [pb], [valsTL])
            pb = nextps(0, 4)
            TR(pb, pb.t[:, 0:32], idxLf.t[:, blk * 128:(blk + 1) * 128], 32, [idxLf])
            ACT(idxTLf.t[:, blk, :], pb.t[:, 0:32], AF.Copy, [pb], [idxTLf])
        pb = nextps(0, 4)
        TR(pb, pb.t[0:32, 0:32], valsC.t[:, :], 32, [valsC])
        ACT(valsTC.t[:, :], pb.t[0:32, 0:32], AF.Copy, [pb], [valsTC])
        pb = nextps(0, 4)
        TR(pb, pb.t[0:32, 0:32], idxCf.t[:, :], 32, [idxCf])
        ACT(idxTCf.t[:, :], pb.t[0:32, 0:32], AF.Copy, [pb], [idxTCf])
        P.op("dve", lambda e, a=idxTL, b_=idxTLf: e.tensor_copy(a.t[:, :, :], b_.t[:, :, :]), reads=[idxTLf],
             writes=[idxTL])
        P.op("dve", lambda e, a=idxTC, b_=idxTCf: e.tensor_copy(a.t[:, :], b_.t[:, :]), reads=[idxTCf],
             writes=[idxTC])
        P.pop_scope()

        xg = [P.sbuf("f_xg%d" % i, [128, D], F32) for i in range(4)]
        xsT = P.sbuf("f_xsT", [128, 16, 576], BF16, nowaw=True)
        hidT = P.sbuf("f_hidT", [128, 8, 576], BF16, nowaw=True)
        sgt = [P.sbuf("f_sgt%d" % i, [128, 288], F32) for i in range(2)]
        yo = [P.sbuf("f_yo%d" % i, [128, D], F32, nowaw=True) for i in range(2)]
        gt2b = [P.sbuf("f_gt2b%d" % v, [128, D], F32) for v in range(3)]
        for v in range(3):
            P.dma("sp", gt2b[v].t[:, :], gt_d[1, v, :].partition_broadcast(128), writes=[gt2b[v]])
        xacc = [[Buf("xacc%d%d" % (s, g)) for g in range(2)] for s in range(NSMP)]
        blocks = []
        for s in range(NSMP):
            blocks.append((s, 0, 0, 128, s * 288))
            blocks.append((s, 0, 1, 128, s * 288 + 128))
            blocks.append((s, 1, 0, 32, s * 288 + 256))
        gcs = [0, 0, 0]

        def load_wgu(e_, hf):
            P.dma("pool", wgu[hf].t[:, 0, :, :],
                  w_gate[l, e_, :, hf * 512:(hf + 1) * 512].rearrange("(c p) n -> p c n", p=128), writes=[wgu[hf]])
            P.dma("pool", wgu[hf].t[:, 1, :, :],
                  w_up[l, e_, :, hf * 512:(hf + 1) * 512].rearrange("(c p) n -> p c n", p=128), writes=[wgu[hf]])

        def load_wd(e_):
            for hh in range(2):
                P.dma("pool", wd.t[:, hh * 4:(hh + 1) * 4, :],
                      w_down[l, e_, hh * 512:(hh + 1) * 512, :].rearrange("(c p) n -> p c n", p=128), writes=[wd])

        def gather(e_, bi):
            (s, isc, blk, rows, cb) = blocks[bi]
            g_ = xg[gcs[0] % 4]
            gcs[0] += 1
            col = s * 16 + e_
            iap = idxTC.t[0:32, col:col + 1] if isc else idxTL.t[:, blk, col:col + 1]
            ibuf = idxTC if isc else idxTL
            P.dma_fn("pool", lambda e, g_=g_, rows=rows, iap=iap: e.indirect_dma_start(
                out=g_.t[0:rows, :], out_offset=None, in_=xn_d[:, :],
                in_offset=bass.IndirectOffsetOnAxis(ap=iap, axis=0)), reads=[ibuf], writes=[g_])
            return g_

        def gate_up(hf):
            for fc in range(4):
                for sg_ in range(2):
                    cs_ = slice(sg_ * 288, (sg_ + 1) * 288)
                    pg = nextps(3, 6)
                    MM(pg, pg.t[:, 0:288], [(wgu[hf].t[:, 0, dc, fc * 128:(fc + 1) * 128], xsT.t[:, dc, cs_])
                                            for dc in range(16)], [wgu[hf], xsT])
                    pu = nextps(3, 6)
                    MM(pu, pu.t[:, 0:288], [(wgu[hf].t[:, 1, dc, fc * 128:(fc + 1) * 128], xsT.t[:, dc, cs_])
                                            for dc in range(16)], [wgu[hf], xsT])
                    sg = sgt[gcs[1] % 2]
                    gcs[1] += 1
                    ACT(sg.t[:, :], pg.t[:, 0:288], AF.Silu, [pg], [sg])
                    TT("dve", hidT.t[:, hf * 4 + fc, cs_], sg.t[:, :], pu.t[:, 0:288], ALU.mult, [sg, pu], [hidT])

        pre = {bi: gather(0, bi) for bi in range(4)}
        for ex_ in range(NE):
            for bi, (s, isc, blk, rows, cb) in enumerate(blocks):
                g_ = pre[bi] if bi in pre else gather(ex_, bi)
                transpose_mod(g_, rows, xsT, cb, a2, 48, variant(s, isc), 0, 3)
            pre = {}
            gate_up(0)
            if ex_ + 1 < NE:
                load_wgu(ex_ + 1, 0)
            gate_up(1)
            if ex_ + 1 < NE:
                load_wgu(ex_ + 1, 1)
                pre = {bi: gather(ex_ + 1, bi) for bi in range(4)}
            for (s, isc, blk, rows, cb) in blocks:
                y_ = yo[gcs[2] % 2]
                gcs[2] += 1
                col = s * 16 + ex_
                v = variant(s, isc)
                gap = valsTC.t[0:32, col:col + 1] if isc else valsTL.t[:, blk, col:col + 1]
                gbuf = valsTC if isc else valsTL
                iap = idxTC.t[0:32, col:col + 1] if isc else idxTL.t[:, blk, col:col + 1]
                ibuf = idxTC if isc else idxTL
                for dt_ in range(4):
                    pb = nextps(6, 8)
                    MM(pb, pb.t[0:rows, :], [(hidT.t[:, fc, cb:cb + rows], wd.t[:, fc, dt_ * 512:(dt_ + 1) * 512])
                                             for fc in range(8)], [hidT, wd])
                    STT(y_.t[0:rows, dt_ * 512:(dt_ + 1) * 512], pb.t[0:rows, :], gap,
                        gt2b[v].t[0:rows, dt_ * 512:(dt_ + 1) * 512], ALU.mult, ALU.mult, [pb, gbuf, gt2b[v]], [y_])
                xa = xacc[s][isc]
                P.dma_fn("pool", lambda e, y_=y_, rows=rows, iap=iap: e.indirect_dma_start(
                    out=xs[:, :], out_offset=bass.IndirectOffsetOnAxis(ap=iap, axis=0), in_=y_.t[0:rows, :],
                    in_offset=None, compute_op=ALU.add), reads=[y_, ibuf, xa], writes=[xa])
            if ex_ + 1 < NE:
                load_wd(ex_ + 1)
        P.pop_scope()
        if stop_after == ("F", l):
            break

    if stop_after is None:
        P.push_scope()
        gfin = P.sbuf("z_gfin", [128, D], F32)
        xt = [P.sbuf("z_xt%d" % i, [128, D], F32) for i in range(2)]
        xo = [P.sbuf("z_xo%d" % i, [128, D], F32) for i in range(2)]
        junk = P.sbuf("z_junk", [128, D], BF16)
        ssb = P.sbuf("z_ss", [128, 192], F32)
        P.dma("sp", gfin.t[:, :], gfin_in[:, :], writes=[gfin])
        for s in range(NSMP):
            for bb in range(16):
                i = s * 16 + bb
                x_ = xt[i % 2]
                o_ = xo[i % 2]
                row0 = s * SEG + CTX + bb * 128
                P.dma("sp", x_.t[:, :], xs[row0:row0 + 128, :], writes=[x_])
                col = i % 64
                ACT(junk.t[:, :], x_.t[:, :], AF.Square, [x_], [junk, ssb], accum_out=ssb.t[:, col:col + 1])
                TS("dve", ssb.t[:, col + 64:col + 65], ssb.t[:, col:col + 1], 1.0 / D, EPS, ALU.mult, ALU.add,
                   [ssb], [ssb])
                TT("pool", ssb.t[:, col + 128:col + 129], ssb.t[:, col + 64:col + 65], mhalf_g.t[:, 0:1], ALU.pow,
                   [ssb, mhalf_g], [ssb])
                STT(o_.t[:, :], x_.t[:, :], ssb.t[:, col + 128:col + 129], gfin.t[:, :], ALU.mult, ALU.mult,
                    [x_, ssb, gfin], [o_])
                P.dma("sp", out_d[s, bb * 128:(bb + 1) * 128, :], o_.t[:, :], reads=[o_], final=True)
        P.pop_scope()
    else:
        P.barrier()

    for name in dump:
        ap_, shp, dt_ = scratch[name]
        o = nc.dram_tensor("dump_" + name, list(shp), dt_, kind="ExternalOutput").ap()
        P.dma("sp", o, ap_, final=True)
    P.emit()
    return nc


def _const_tables():
    import ml_dtypes
    bf = ml_dtypes.bfloat16
    ident = np.eye(128, dtype=np.float32)
    rperm = np.zeros((128, 128), np.float32)
    for dest in range(128):
        if dest % 32 < 16:
            rperm[dest + 16, dest] = -1.0
        else:
            rperm[dest - 16, dest] = 1.0
    t = np.arange(LAT)
    pos = np.stack([t // 64, t % 64], axis=-1).astype(np.float32)
    inv_freq = (10000.0 ** (-np.arange(16, dtype=np.float32) / 16)).astype(np.float32)
    dd = np.arange(64)
    ang = (pos[:, dd // 32] * inv_freq[dd % 16][None, :]).astype(np.float32)
    ropeT = np.zeros((128, 2, LAT), np.float32)
    ropeT[:, 0, :] = np.cos(ang).astype(np.float32).T[np.arange(128) % 64]
    ropeT[:, 1, :] = np.sin(ang).astype(np.float32).T[np.arange(128) % 64]

    def dft(nn):
        j = np.arange(nn, dtype=np.int64)
        m = (j[:, None] * j[None, :]) % nn
        a = 2.0 * np.pi * m.astype(np.float64) / nn
        return np.cos(a) / np.sqrt(nn), np.sin(a) / np.sqrt(nn)

    c128, s128 = dft(128)
    dftc = np.concatenate([c128, s128], axis=1).astype(np.float32)
    cn, sn = dft(LAT)
    dftn = np.stack([cn, -sn]).astype(np.float32).astype(bf)
    cc, sc = dft(CTX)
    dft256 = np.stack([cc, -sc]).astype(np.float32).astype(bf)
    rowbase = np.zeros((32, 2), np.float32)
    for s in range(NSMP):
        rowbase[s * 16:(s + 1) * 16, 0] = s * SEG + CTX
        rowbase[s * 16:(s + 1) * 16, 1] = s * SEG
    return dict(ident=ident, rperm=rperm, ropeT=ropeT, dftc=dftc, dftn=dftn, dft256=dft256, rowbase=rowbase)


def _pack_small(g_norm1, g_norm2, conv_w, conv_b, conv_ln_g, conv_ln_b, g_sub, diff_lambda):
    depth = g_norm1.shape[0]
    sp = np.zeros((depth, 128, NSP), np.float32)
    for l in range(depth):
        sp[l, :, SP_G1:SP_G1 + 16] = g_norm1[l].reshape(16, 128).T
        sp[l, :, SP_G2:SP_G2 + 16] = g_norm2[l].reshape(16, 128).T
        cw = conv_w[l].reshape(31, 4, 128)
        sp[l, :, SP_CW:SP_CW + 124] = np.transpose(cw, (2, 1, 0)).reshape(128, 124)
        sp[l, :, SP_CB:SP_CB + 4] = conv_b[l].reshape(4, 128).T
        sp[l, :, SP_LG:SP_LG + 4] = conv_ln_g[l].reshape(4, 128).T
        sp[l, :, SP_LB:SP_LB + 4] = conv_ln_b[l].reshape(4, 128).T
        sp[l, :, SP_GS] = g_sub[l]
        sp[l, :, SP_DL:SP_DL + 256] = diff_lambda[l].reshape(1, 256)
    return sp


def make_in_maps(inputs, n_cores, depth):
    f32 = lambda a: np.ascontiguousarray(np.asarray(a, dtype=np.float32))
    consts = _const_tables()
    sp = _pack_small(*(f32(inputs[k])[:depth] for k in ("g_norm1", "g_norm2", "conv_w", "conv_b", "conv_ln_g",
                                                         "conv_ln_b", "g_sub", "diff_lambda")))
    gfin = np.ascontiguousarray(np.broadcast_to(f32(inputs["g_final"])[None, :], (128, D)))
    shared = dict(consts)
    shared["smallp"] = sp
    shared["gfin"] = gfin
    for k in ("w_ada", "b_ada", "w_in", "w_out", "w_router", "w_gate", "w_up", "w_down"):
        a = inputs[k]
        shared[k] = np.asarray(a)[:depth] if depth != np.asarray(a).shape[0] else np.asarray(a)
    x = np.asarray(inputs["x"])
    ctx = np.asarray(inputs["ctx"])
    c = f32(inputs["c"])
    c_ctx = f32(inputs["c_ctx"])
    maps = []
    for i in range(n_cores):
        m = dict(shared)
        m["x"] = x[NSMP * i:NSMP * (i + 1)]
        m["ctx"] = ctx[NSMP * i:NSMP * (i + 1)]
        cv = np.stack([c[NSMP * i], c[NSMP * i + 1], c_ctx], axis=-1)
        m["cvec"] = np.ascontiguousarray(cv.reshape(16, 128, 3).transpose(1, 0, 2))
        maps.append(m)
    return maps


def kernel(**inputs):
    n_cores = 8
    depth = 4
    nc = build_program(depth=depth)
    maps = make_in_maps(inputs, n_cores, depth)
    res = run_bass_kernel_spmd(nc, maps, core_ids=list(range(n_cores)))
    out = np.concatenate([np.asarray(r["out"]) for r in res.results], axis=0)
    return out.astype(np.float32, copy=False)
```
